# Optimizing a Trainium2 kernel written in Bass

```python
import jax
import jax.numpy as jnp
from jax import lax
import numpy as np

D_MODEL = 4096
BATCH = 4
SEQ = 4096
DEPTH = 2
DEC_BATCH = 8
DEC_SEQ = 2048
PAST_LEN = 128

PLE_DIM = 256
CHUNK = 128
D_GMLP = D_MODEL // 2
GMLP_HEAD = 128
N_GMLP_HEADS = D_GMLP // GMLP_HEAD
D_RWKV = D_MODEL - D_GMLP
RWKV_HEAD = 64
N_RWKV_HEADS = D_RWKV // RWKV_HEAD
DECAY_LORA = 96
AAA_LORA = 96
MV_LORA = 64
GATE_LORA = 256
CONV_W = 3
RWKV_SIZES = (D_RWKV, D_RWKV, D_RWKV, DECAY_LORA, DECAY_LORA, AAA_LORA, AAA_LORA, GATE_LORA)
C_RWKV0 = sum(RWKV_SIZES)
C_RWKV = C_RWKV0 + MV_LORA
C_IN0 = 2 * D_GMLP + C_RWKV0
C_IN = 2 * D_GMLP + C_RWKV
D_FF = 7 * D_MODEL // 2
D_FF_E = D_MODEL // 2
N_EXPERTS = 8
TOP_K = 2
N_DENSE = (DEPTH + 1) // 2
N_MOE = DEPTH // 2
ALPHA = (2 * DEPTH) ** 0.25
BETA = (8 * DEPTH) ** -0.25
LN_EPS = 1e-5
GN_EPS = 64e-5
L2_EPS = 1e-12

kernel_name = 'hymba_gmlp_rwkv7_deepnorm_encoder'


def _layernorm(x, g, b, eps=LN_EPS):
    xf = x.astype(jnp.float32)
    mu = jnp.mean(xf, -1, keepdims=True)
    var = jnp.mean(jnp.square(xf - mu), -1, keepdims=True)
    y = (xf - mu) * lax.rsqrt(var + eps)
    return (y * g.astype(jnp.float32) + b.astype(jnp.float32)).astype(x.dtype)


def _split_cols(z, sizes):
    idx = [int(i) for i in np.cumsum(sizes)[:-1]]
    return jnp.split(z, idx, axis=-1)


def _centered_conv(z, w):
    half = CONV_W // 2
    s = z.shape[1]
    zp = jnp.pad(z, ((0, 0), (half, half), (0, 0)))
    return sum(zp[:, j:j + s] * w[j] for j in range(CONV_W))


def _spatial_gating(u, v, ln_g, ln_b, w_s, b_s):
    u = jax.nn.gelu(u)
    v = _layernorm(jax.nn.gelu(v), ln_g, ln_b)
    bsz, s, _ = v.shape
    vc = v.reshape(bsz, s // CHUNK, CHUNK, N_GMLP_HEADS, GMLP_HEAD)
    sv = jnp.einsum('hpq,bcqhd->bcphd', w_s, vc) + b_s.T[None, None, :, :, None]
    return u * sv.reshape(bsz, s, D_GMLP)


def _heads(t):
    return t.reshape(t.shape[0], t.shape[1], N_RWKV_HEADS, RWKV_HEAD)


def _wkv_scan(r, decay, k, v, kk, b, reverse):
    bsz, _, h, n = r.shape
    xs = tuple(jnp.moveaxis(t, 1, 0) for t in (r, decay, k, v, kk, b))

    def step(state, inp):
        r_t, w_t, k_t, v_t, kk_t, b_t = inp
        sa = jnp.einsum('bhij,bhj->bhi', state, kk_t)
        new = (state * w_t[:, :, None, :] - sa[..., None] * b_t[:, :, None, :]
               + v_t[..., None] * k_t[:, :, None, :])
        y = jnp.einsum('bhij,bhj->bhi', state if reverse else new, r_t)
        return new, y

    s0 = jnp.zeros((bsz, h, n, n), jnp.float32)
    _, ys = lax.scan(step, s0, xs, reverse=reverse)
    return jnp.moveaxis(ys, 0, 1)


def _rwkv_mix(zr, conv_w, w0, w2, a0, a2, g2, k_k, k_a, r_k, gn_g, gn_b, v_first, v0, v2):
    f32 = jnp.float32
    bsz, s, _ = zr.shape
    zr = _centered_conv(zr, conv_w).astype(f32)
    sizes = RWKV_SIZES if v0 is None else RWKV_SIZES + (MV_LORA,)
    parts = _split_cols(zr, sizes)
    r, k, v, wd_f, wd_b, ad_f, ad_b, gd = parts[:8]
    if v0 is None:
        v_first = v
    else:
        v = v + (v_first - v) * jax.nn.sigmoid(v0 + parts[8] @ v2)
    g = jax.nn.sigmoid(gd) @ g2
    kk = _heads(k * k_k)
    kk = kk * lax.rsqrt(jnp.sum(kk * kk, -1, keepdims=True) + L2_EPS)
    r_h, v_h = _heads(r), _heads(v)
    ys, k_dir = [], []
    for d, (wd, ad) in enumerate(((wd_f, ad_f), (wd_b, ad_b))):
        w_log = -jax.nn.softplus(-(w0[d] + jnp.tanh(wd) @ w2[d])) - 0.5
        decay = jnp.exp(-jnp.exp(w_log))
        a = jax.nn.sigmoid(a0[d] + ad @ a2[d])
        k_d = _heads(k * (1.0 + (a - 1.0) * k_a))
        ys.append(_wkv_scan(r_h, _heads(decay), k_d, v_h, kk, kk * _heads(a), reverse=(d == 1)))
        k_dir.append(k_d)
    y = ys[0] + ys[1]
    mu = jnp.mean(y, -1, keepdims=True)
    var = jnp.mean(jnp.square(y - mu), -1, keepdims=True)
    y = ((y - mu) * lax.rsqrt(var + GN_EPS)).reshape(bsz, s, D_RWKV) * gn_g + gn_b
    bonus = (jnp.sum(r_h * k_dir[0] * r_k, -1, keepdims=True) * v_h).reshape(bsz, s, D_RWKV)
    return (y + bonus) * g, v_first


def _swiglu(x, wg, wu, wd):
    h = jax.nn.silu(jnp.einsum('bsd,df->bsf', x, wg)) * jnp.einsum('bsd,df->bsf', x, wu)
    return jnp.einsum('bsf,fd->bsd', h, wd)


def _moe(x, router, we_gate, we_up, we_down):
    logits = jnp.einsum('bsd,de->bse', x, router).astype(jnp.float32)
    top_vals, top_idx = lax.top_k(logits, TOP_K)
    top_w = jax.nn.softmax(top_vals, axis=-1)
    gates = jnp.sum(jax.nn.one_hot(top_idx, N_EXPERTS, dtype=jnp.float32) * top_w[..., None], axis=-2)
    out = jnp.zeros_like(x)
    for e in range(N_EXPERTS):
        ye = _swiglu(x, we_gate[e], we_up[e], we_down[e])
        out = out + gates[..., e:e + 1].astype(x.dtype) * ye
    return out


def _trunk(x, p, P):
    v_first = None
    for l in range(DEPTH):
        if l == 0:
            w_in_l, conv_l, v0_l, v2_l = P['w_in0'], P['conv0'], None, None
        else:
            w_in_l, conv_l, v0_l, v2_l = P['w_in'][l - 1], P['conv'][l - 1], P['v0'][l - 1], P['v2'][l - 1]
        z = jnp.einsum('bsd,dc->bsc', x, w_in_l)
        u, vg, zr = z[..., :D_GMLP], z[..., D_GMLP:2 * D_GMLP], z[..., 2 * D_GMLP:]
        y_g = _spatial_gating(u, vg, P['sgu_ln_g'][l], P['sgu_ln_b'][l], P['w_s'][l], P['b_s'][l])
        y_r, v_first = _rwkv_mix(zr, conv_l, P['w0'][l], P['w2'][l], P['a0'][l], P['a2'][l], P['g2'][l],
                                 P['k_k'][l], P['k_a'][l], P['r_k'][l], P['gn_g'][l], P['gn_b'][l],
                                 v_first, v0_l, v2_l)
        mixed = jnp.concatenate([y_g, y_r.astype(y_g.dtype)], axis=-1)
        mix = jnp.einsum('bsc,cd->bsd', mixed, P['w_out'][l])
        x = _layernorm(ALPHA * x + mix, P['ln1_g'][l], P['ln1_b'][l])
        j = l // 2
        if l % 2 == 0:
            ff = _swiglu(x, P['w_ff_gate'][j], P['w_ff_up'][j], P['w_ff_down'][j])
        else:
            ff = _moe(x, P['router'][j], P['we_gate'][j], P['we_up'][j], P['we_down'][j])
        x = _layernorm(ALPHA * x + ff, P['ln2_g'][l], P['ln2_b'][l])
        gate = jax.nn.sigmoid(jnp.einsum('bsd,de->bse', x, P['w_pgate'][l]))
        x = x + gate * jnp.einsum('bsk,kd->bsd', p[l], P['w_pproj'][l])
    return x


def setup_inputs(seed: int = 0) -> dict:
    key = jax.random.key(seed)
    ks = iter(jax.random.split(key, 48))
    f32 = jnp.float32

    def nrm(shape, scale):
        return jax.random.normal(next(ks), shape, f32) * scale

    def gain(shape):
        return 1.0 + nrm(shape, 0.05)

    conv_base = jnp.array([0.25, 0.5, 0.25], f32)[:, None]
    return {
        'x_prompt': nrm((BATCH, SEQ, D_MODEL), 1.0),
        'x_sample': nrm((DEC_BATCH, DEC_SEQ, D_MODEL), 1.0),
        'p_prompt': nrm((DEPTH, BATCH, SEQ, PLE_DIM), 1.0),
        'p_sample': nrm((DEPTH, DEC_BATCH, DEC_SEQ, PLE_DIM), 1.0),
        'w_in0': nrm((D_MODEL, C_IN0), D_MODEL ** -0.5),
        'conv0': conv_base + nrm((CONV_W, C_RWKV0), 0.05),
        'w_in': nrm((DEPTH - 1, D_MODEL, C_IN), D_MODEL ** -0.5),
        'conv': conv_base + nrm((DEPTH - 1, CONV_W, C_RWKV), 0.05),
        'sgu_ln_g': gain((DEPTH, D_GMLP)),
        'sgu_ln_b': nrm((DEPTH, D_GMLP), 0.02),
        'w_s': nrm((DEPTH, N_GMLP_HEADS, CHUNK, CHUNK), CHUNK ** -0.5),
        'b_s': gain((DEPTH, N_GMLP_HEADS, CHUNK)),
        'w0': jax.random.uniform(next(ks), (DEPTH, 2, D_RWKV), f32, -6.0, 0.0),
        'w2': nrm((DEPTH, 2, DECAY_LORA, D_RWKV), 0.5 * DECAY_LORA ** -0.5),
        'a0': nrm((DEPTH, 2, D_RWKV), 0.5),
        'a2': nrm((DEPTH, 2, AAA_LORA, D_RWKV), 0.5 * AAA_LORA ** -0.5),
        'g2': nrm((DEPTH, GATE_LORA, D_RWKV), GATE_LORA ** -0.5),
        'k_k': 0.85 + nrm((DEPTH, D_RWKV), 0.05),
        'k_a': gain((DEPTH, D_RWKV)),
        'r_k': nrm((DEPTH, N_RWKV_HEADS, RWKV_HEAD), 0.1),
        'gn_g': gain((DEPTH, D_RWKV)),
        'gn_b': nrm((DEPTH, D_RWKV), 0.02),
        'v0': nrm((DEPTH - 1, D_RWKV), 0.5),
        'v2': nrm((DEPTH - 1, MV_LORA, D_RWKV), 0.5 * MV_LORA ** -0.5),
        'w_out': nrm((DEPTH, D_MODEL, D_MODEL), BETA * D_MODEL ** -0.5),
        'ln1_g': gain((DEPTH, D_MODEL)),
        'ln1_b': nrm((DEPTH, D_MODEL), 0.02),
        'ln2_g': gain((DEPTH, D_MODEL)),
        'ln2_b': nrm((DEPTH, D_MODEL), 0.02),
        'w_ff_gate': nrm((N_DENSE, D_MODEL, D_FF), D_MODEL ** -0.5),
        'w_ff_up': nrm((N_DENSE, D_MODEL, D_FF), D_MODEL ** -0.5),
        'w_ff_down': nrm((N_DENSE, D_FF, D_MODEL), BETA * D_FF ** -0.5),
        'router': nrm((N_MOE, D_MODEL, N_EXPERTS), D_MODEL ** -0.5),
        'we_gate': nrm((N_MOE, N_EXPERTS, D_MODEL, D_FF_E), D_MODEL ** -0.5),
        'we_up': nrm((N_MOE, N_EXPERTS, D_MODEL, D_FF_E), D_MODEL ** -0.5),
        'we_down': nrm((N_MOE, N_EXPERTS, D_FF_E, D_MODEL), BETA * D_FF_E ** -0.5),
        'w_pproj': nrm((DEPTH, PLE_DIM, D_MODEL), BETA * PLE_DIM ** -0.5),
        'w_pgate': nrm((DEPTH, D_MODEL, D_MODEL), D_MODEL ** -0.5),
    }


def reference(x_prompt, x_sample, p_prompt, p_sample, w_in0, conv0, w_in, conv, sgu_ln_g, sgu_ln_b,
              w_s, b_s, w0, w2, a0, a2, g2, k_k, k_a, r_k, gn_g, gn_b, v0, v2, w_out,
              ln1_g, ln1_b, ln2_g, ln2_b, w_ff_gate, w_ff_up, w_ff_down, router, we_gate, we_up,
              we_down, w_pproj, w_pgate):
    P = dict(w_in0=w_in0, conv0=conv0, w_in=w_in, conv=conv, sgu_ln_g=sgu_ln_g, sgu_ln_b=sgu_ln_b,
             w_s=w_s, b_s=b_s, w0=w0, w2=w2, a0=a0, a2=a2, g2=g2, k_k=k_k, k_a=k_a, r_k=r_k,
             gn_g=gn_g, gn_b=gn_b, v0=v0, v2=v2, w_out=w_out, ln1_g=ln1_g, ln1_b=ln1_b,
             ln2_g=ln2_g, ln2_b=ln2_b, w_ff_gate=w_ff_gate, w_ff_up=w_ff_up, w_ff_down=w_ff_down,
             router=router, we_gate=we_gate, we_up=we_up, we_down=we_down,
             w_pproj=w_pproj, w_pgate=w_pgate)
    y_prompt = _trunk(x_prompt, p_prompt, P)
    y_sample = _trunk(x_sample, p_sample, P)
    return (y_prompt, y_sample)
```

```python
import contextlib
import numpy as np
import concourse.bass as bass
import concourse.mybir as mybir
from concourse.bass_utils import run_bass_kernel_spmd

F32 = mybir.dt.float32
BF16 = mybir.dt.bfloat16
AF = mybir.ActivationFunctionType
ALU = mybir.AluOpType
AX = mybir.AxisListType

SEM_LIMIT = 28000


class Buf:
    __slots__ = ("name", "writers", "dma_w", "readers", "dma_r")

    def __init__(self, name=""):
        self.name = name
        self.writers = {}
        self.dma_w = []
        self.readers = {}
        self.dma_r = []


class Sched:
    ENGS = ("pe", "act", "dve", "pool", "sp")

    def __init__(self, nc, stack, n_sems=100, dma_ring=4):
        self.nc = nc
        self.eng = {"pe": nc.tensor, "act": nc.scalar, "dve": nc.vector, "pool": nc.gpsimd, "sp": nc.sync}
        self.count = {e: 0 for e in self.ENGS}
        self.seen = {e: {} for e in self.ENGS}
        self.free_sems = [stack.enter_context(nc.semaphore(f"s{i}")) for i in range(n_sems)]
        self.semmap = {}
        self.dma_ring = dma_ring
        self.dma_n = {q: 0 for q in ("sp", "pool", "act")}
        self.dma_slot_cnt = {}
        self.n_ops = 0
        self.n_waits = 0
        self.pending = {e: False for e in self.ENGS}

    def sem(self, key):
        s = self.semmap.get(key)
        if s is None:
            s = self.free_sems.pop()
            self.semmap[key] = s
        return s

    def _engpos(self, e, seq):
        return (("e", e, (seq - 1) // SEM_LIMIT), (seq - 1) % SEM_LIMIT + 1)

    def _need(self, e, key, val, waits):
        if self.seen[e].get(key, 0) < val:
            self.seen[e][key] = val
            waits.append((key, val))

    def _deps(self, e, r, w, dma_write=False):
        waits = []
        for b in r:
            for (pe_, seq) in b.writers.items():
                if pe_ == e:
                    if seq >= self.count[e] - 2:
                        k, v = self._engpos(pe_, seq)
                        self._need(e, k, v, waits)
                else:
                    k, v = self._engpos(pe_, seq)
                    self._need(e, k, v, waits)
            for (k, v) in b.dma_w:
                self._need(e, k, v, waits)
        for b in w:
            for (pe_, seq) in b.writers.items():
                if pe_ != e:
                    k, v = self._engpos(pe_, seq)
                    self._need(e, k, v, waits)
            if not dma_write:
                for (k, v) in b.dma_w:
                    self._need(e, k, v, waits)
            for (pe_, seq) in b.readers.items():
                if pe_ != e:
                    k, v = self._engpos(pe_, seq)
                    self._need(e, k, v, waits)
            for (k, v) in b.dma_r:
                self._need(e, k, v, waits)
        return waits

    def _emit_waits(self, e, waits):
        eng = self.eng[e]
        for (k, v) in waits:
            eng.wait_ge(self.sem(k), v)
        self.n_waits += len(waits)

    def op(self, e, fn, r=(), w=(), inc=True):
        waits = self._deps(e, r, w)
        self._emit_waits(e, waits)
        ins = fn(self.eng[e])
        self.pending[e] = not inc
        if inc:
            self.count[e] += 1
            seq = self.count[e]
            k, v = self._engpos(e, seq)
            ins.then_inc(self.sem(k), 1)
        else:
            seq = self.count[e] + 1
        for b in w:
            b.writers = {e: seq}
            b.dma_w = []
            b.readers = {}
            b.dma_r = []
        for b in r:
            b.readers[e] = seq
        self.n_ops += 1

    def pe(self, fn, r=(), w=(), inc=True):
        self.op("pe", fn, r, w, inc)

    def act(self, fn, r=(), w=()):
        self.op("act", fn, r, w)

    def dve(self, fn, r=(), w=()):
        self.op("dve", fn, r, w)

    def pool(self, fn, r=(), w=()):
        self.op("pool", fn, r, w)

    def dma(self, q, out, in_, r=(), w=()):
        waits = self._deps(q, r, w, dma_write=True)
        n = self.dma_n[q]
        self.dma_n[q] += 1
        slot = n % self.dma_ring
        cnt = self.dma_slot_cnt.get((q, slot), 0)
        if cnt > 0:
            pk = ("d", q, slot, (cnt - 1) * 16 // SEM_LIMIT)
            pv = ((cnt - 1) * 16) % SEM_LIMIT + 16
            self._need(q, pk, pv, waits)
        key = ("d", q, slot, cnt * 16 // SEM_LIMIT)
        val = (cnt * 16) % SEM_LIMIT + 16
        self.dma_slot_cnt[(q, slot)] = cnt + 1
        tok = (key, val)
        self._emit_waits(q, waits)
        self.eng[q].dma_start(out=out, in_=in_).then_inc(self.sem(key), 16)
        for b in w:
            b.dma_w.append(tok)
        for b in r:
            b.dma_r.append(tok)
        self.n_ops += 1
        return tok

    def barrier(self):
        targets = []
        assert not any(self.pending.values()), self.pending
        for e in self.ENGS:
            if self.count[e] > 0:
                targets.append(self._engpos(e, self.count[e]))
        for (q, slot), cnt in self.dma_slot_cnt.items():
            if cnt > 0:
                targets.append((("d", q, slot, (cnt - 1) * 16 // SEM_LIMIT), ((cnt - 1) * 16) % SEM_LIMIT + 16))
        for e in self.ENGS:
            waits = []
            for (k, v) in targets:
                if k[0] == "e" and k[1] == e:
                    continue
                self._need(e, k, v, waits)
            self._emit_waits(e, waits)


class Tile:
    def __init__(self, t, name):
        self.t = t
        self.b = Buf(name)

    def __getitem__(self, k):
        return self.t[k]


class Cfg:
    def __init__(self, D=4096, NT=4096, SEG=2048, DEPTH=2, TB=1024, n_cores=8, PB=None):
        self.PB = PB
        self.D = D
        self.NT = NT
        self.SEG = SEG
        self.DEPTH = DEPTH
        self.TB = TB
        self.n_cores = n_cores
        self.PLE = 256
        self.DG = D // 2
        self.NHG = self.DG // 128
        self.DR = D - self.DG
        self.NPAIR = self.DR // 128
        self.LD, self.LA, self.LMV, self.LG = 96, 96, 64, 256
        self.C_RW0 = 3 * self.DR + 2 * 96 + 2 * 96 + 256
        self.C_RW = self.C_RW0 + 64
        self.CIN0 = 2 * self.DG + self.C_RW0
        self.CIN = 2 * self.DG + self.C_RW
        self.DFF = 7 * D // 2
        self.DFFE = D // 2
        self.NE = 8
        self.NFT = D // 128
        self.NCH = NT // 64
        self.ALPHA = (2 * DEPTH) ** 0.25
        self.LN_EPS = 1e-5
        self.GN_EPS = 64e-5
        self.L2_EPS = 1e-12
        self.cols = {}
        self._build_cols()

    def _build_cols(self):
        n = 0

        def add(key):
            nonlocal n
            self.cols[key] = n
            n += 1
        P = self.NPAIR
        for nm in ("cv_r", "cv_k", "cv_v"):
            for p in range(P):
                for j in range(3):
                    add((nm, p, j))
        for nm in ("cv_wdf", "cv_wdb", "cv_adf", "cv_adb", "cv_gd0", "cv_gd1", "cv_mv"):
            for j in range(3):
                add((nm, j))
        for nm in ("k_k", "k_a", "r_k", "gn_g", "gn_b", "v0"):
            for p in range(P):
                add((nm, p))
        for nm in ("w0", "a0"):
            for d in range(2):
                for p in range(P):
                    add((nm, d, p))
        for nm in ("ln1_g", "ln1_b", "ln2_g", "ln2_b"):
            for t in range(self.NFT):
                add((nm, t))
        self.NCOLS = n


def pack_cols(cfg, l, W):
    C = np.zeros((128, cfg.NCOLS), np.float32)
    DR, P = cfg.DR, cfg.NPAIR
    conv = W["conv0"] if l == 0 else W["conv"][l - 1]

    def put(key, vec):
        C[:len(vec), cfg.cols[key]] = vec
    for i, nm in enumerate(("cv_r", "cv_k", "cv_v")):
        for p in range(P):
            for j in range(3):
                put((nm, p, j), conv[j, i * DR + 128 * p: i * DR + 128 * p + 128])
    base = 3 * DR
    offs = {"cv_wdf": (0, 96), "cv_wdb": (96, 96), "cv_adf": (192, 96), "cv_adb": (288, 96),
            "cv_gd0": (384, 128), "cv_gd1": (512, 128), "cv_mv": (640, 64)}
    for nm, (o, ln) in offs.items():
        if nm == "cv_mv" and l == 0:
            continue
        for j in range(3):
            put((nm, j), conv[j, base + o: base + o + ln])
    rk = W["r_k"][l].reshape(-1)
    vecs = {"k_k": W["k_k"][l], "k_a": W["k_a"][l], "r_k": rk, "gn_g": W["gn_g"][l], "gn_b": W["gn_b"][l]}
    if l > 0:
        vecs["v0"] = W["v0"][l - 1]
    for nm, v in vecs.items():
        for p in range(P):
            put((nm, p), v[128 * p:128 * p + 128])
    for nm in ("w0", "a0"):
        for d in range(2):
            for p in range(P):
                put((nm, d, p), W[nm][l, d, 128 * p:128 * p + 128])
    for nm in ("ln1_g", "ln1_b", "ln2_g", "ln2_b"):
        for t in range(cfg.NFT):
            put((nm, t), W[nm][l, 128 * t:128 * t + 128])
    return C


def build(cfg, debug_outs=()):
    c = cfg
    nc = bass.Bass("TRN2", target_bir_lowering=False)
    D, NT, TB, DG, DR, P, NHG, NFT, PLE = c.D, c.NT, c.TB, c.DG, c.DR, c.NPAIR, c.NHG, c.NFT, c.PLE
    DEPTH, NCH, SEG = c.DEPTH, c.NCH, c.SEG
    N_DENSE = (DEPTH + 1) // 2
    N_MOE = DEPTH // 2
    NE, DFF, DFFE = c.NE, c.DFF, c.DFFE
    I = {}

    def inp(name, shape):
        I[name] = nc.dram_tensor(name, list(shape), F32, kind="ExternalInput").ap()
        return I[name]
    inp("xT", [D, NT]); inp("pT", [DEPTH * PLE, NT]); inp("link", [128, 1]); inp("cols", [DEPTH * 128, c.NCOLS])
    inp("w_in0", [D, c.CIN0])
    if DEPTH > 1:
        inp("w_in", [(DEPTH - 1) * D, c.CIN])
        inp("v2", [(DEPTH - 1) * 64, DR])
    inp("w_out", [DEPTH * D, D]); inp("w_pgate", [DEPTH * D, D]); inp("w_pproj", [DEPTH * PLE, D])
    inp("w_ff_gate", [N_DENSE * D, DFF]); inp("w_ff_up", [N_DENSE * D, DFF]); inp("w_ff_down", [N_DENSE * DFF, D])
    if N_MOE:
        inp("router", [N_MOE * D, NE]); inp("we_gate", [N_MOE * NE * D, DFFE]); inp("we_up", [N_MOE * NE * D, DFFE])
        inp("we_down", [N_MOE * NE * DFFE, D])
    inp("sgu_ln_g", [DEPTH, DG]); inp("sgu_ln_b", [DEPTH, DG]); inp("b_s", [DEPTH, NHG * 128]); inp("w_sT", [DEPTH * 128, NHG * 128])
    inp("w2", [DEPTH * 2 * 96, DR]); inp("a2", [DEPTH * 2 * 96, DR]); inp("g2", [DEPTH * 256, DR])
    yT = nc.dram_tensor("yT", [D, NT], F32, kind="ExternalOutput").ap()

    dbg = {}

    def scr(name, shape, dt):
        if name in debug_outs:
            t = nc.dram_tensor(name, list(shape), dt, kind="ExternalOutput").ap()
            dbg[name] = t
            return t
        return nc.dram_tensor(name, list(shape), dt, kind="Internal").ap()

    XF1 = scr("XF1", [D, NT], F32); XB1 = scr("XB1", [D, NT], BF16)
    UG = scr("UG", [DG, NT], BF16); VG = scr("VG", [DG, NT], F32); ZR = scr("ZR", [c.C_RW, NT], F32)
    MIX = scr("MIX", [D, NT], BF16)
    SC = {}
    for d in range(2):
        for nm in ("RT", "KKT", "KH", "BH", "KEND", "NBEND"):
            SC[(nm, d)] = scr(f"{nm}{d}", [DR, NT], BF16)
        SC[("WL", d)] = scr(f"WL{d}", [DR, NCH], F32)
        SC[("Y", d)] = scr(f"Y{d}", [DR, NT], F32)
    VB = scr("VB", [DR, NT], BF16); VF = scr("VF", [DR, NT], F32); GG = scr("GG", [DR, NT], F32); BON = scr("BON", [DR, NT], F32)
    H1 = scr("H1", [D, NT], F32); X1F = scr("X1F", [D, NT], F32)
    HH = scr("HH", [max(DFF, NE * DFFE), NT], BF16)
    H2 = scr("H2", [D, NT], F32); X2F = scr("X2F", [D, NT], F32)

    WB = {}
    WCONV = {l_: [] for l_ in range(DEPTH)}

    def wconv(l_, key, src):
        dst = nc.dram_tensor(f"wb_{key}_{l_}", list(src.shape), BF16, kind="Internal").ap()
        WB[(key, l_)] = dst
        WCONV[l_].append((src, dst))
    for l_ in range(DEPTH):
        j_ = l_ // 2
        wconv(l_, "w_out", I["w_out"][l_ * D:(l_ + 1) * D, :])
        wconv(l_, "w_pgate", I["w_pgate"][l_ * D:(l_ + 1) * D, :])
        if l_ % 2 == 0:
            wconv(l_, "ffg", I["w_ff_gate"][j_ * D:(j_ + 1) * D, :])
            wconv(l_, "ffu", I["w_ff_up"][j_ * D:(j_ + 1) * D, :])
            wconv(l_, "ffd", I["w_ff_down"][j_ * DFF:(j_ + 1) * DFF, :])
        else:
            wconv(l_, "ffg", I["we_gate"][j_ * NE * D:(j_ + 1) * NE * D, :])
            wconv(l_, "ffu", I["we_up"][j_ * NE * D:(j_ + 1) * NE * D, :])
            wconv(l_, "ffd", I["we_down"][j_ * NE * DFFE:(j_ + 1) * NE * DFFE, :])
        if l_ + 1 < DEPTH:
            wconv(l_, "w_in_next", I["w_in"][l_ * D:(l_ + 1) * D, :])

    gst = contextlib.ExitStack()
    with gst:
        S = Sched(nc, gst)

        uid = [0]

        def sb(st, name, shape, dt=F32):
            uid[0] += 1
            nm = f"{name}_{uid[0]}"
            return Tile(st.enter_context(nc.sbuf_tensor(nm, list(shape), dt)), name)
        PS = [Tile(gst.enter_context(nc.psum_tensor(f"psb{i}", [128, 512], F32)), f"psb{i}") for i in range(8)]

        ident_f = sb(gst, "ident_f", [128, 128]); ident_b = sb(gst, "ident_b", [128, 128], BF16)
        ones_f = sb(gst, "ones_f", [128, 128]); onesbd = sb(gst, "onesbd", [128, 128])
        identbd_b = ident_b
        linkt = sb(gst, "linkt", [128, 1])
        M01 = sb(gst, "M01", [128, 512])
        S.dma("sp", linkt[:], I["link"], w=[linkt.b])
        S.pool(lambda e: e.memset(ident_f[:], 0.0), w=[ident_f.b])
        S.pool(lambda e: e.affine_select(out=ident_f[:], in_=ident_f[:], pattern=[[-1, 128]], compare_op=ALU.not_equal,
                                         fill=1.0, base=0, channel_multiplier=1), r=[ident_f.b], w=[ident_f.b])
        S.pool(lambda e: e.tensor_copy(out=ident_b[:], in_=ident_f[:]), r=[ident_f.b], w=[ident_b.b])
        S.pool(lambda e: e.memset(ones_f[:], 1.0), w=[ones_f.b])
        S.pool(lambda e: e.memset(onesbd[:], 0.0), w=[onesbd.b])
        S.pool(lambda e: e.memset(onesbd[0:64, 0:64], 1.0), w=[onesbd.b])
        S.pool(lambda e: e.memset(onesbd[64:128, 64:128], 1.0), w=[onesbd.b])
        S.pool(lambda e: e.memset(M01[:], 1.0), w=[M01.b])
        S.pool(lambda e: e.memset(M01[:].rearrange("p (c t) -> p c t", t=64)[:, :, 0:1], 0.0), w=[M01.b])
        Lst = sb(gst, "Lst", [128, 128]); Ust = sb(gst, "Ust", [128, 128]); Uin = sb(gst, "Uin", [128, 128])
        for (T_, op_, st_, cm_) in ((Lst, ALU.is_gt, -1, 1), (Ust, ALU.is_gt, 1, -1), (Uin, ALU.is_ge, 1, -1)):
            S.pool(lambda e, T_=T_: e.memset(T_[:], 1.0), w=[T_.b])
            S.pool(lambda e, T_=T_, op_=op_, st_=st_, cm_=cm_: e.affine_select(out=T_[:], in_=T_[:], pattern=[[st_, 128]], compare_op=op_,
                                                                               fill=0.0, base=0, channel_multiplier=cm_), r=[T_.b], w=[T_.b])
            S.pool(lambda e, T_=T_: e.memset(T_[64:128, 0:64], 0.0), w=[T_.b])
            S.pool(lambda e, T_=T_: e.memset(T_[0:64, 64:128], 0.0), w=[T_.b])
        MASK = {}
        mdefs = {0: {"X1": (Lst, -1.0), "X1t": (Ust, -1.0), "NAybT": (Uin, -1.0), "AukT": (Ust, 1.0), "AykT": (Uin, 1.0)},
                 1: {"X1": (Ust, -1.0), "X1t": (Lst, -1.0), "NAybT": (Lst, -1.0), "AukT": (Lst, 1.0), "AykT": (Lst, 1.0)}}
        negs = {}
        for T_ in (Lst, Ust, Uin):
            n_ = sb(gst, "neg_" + T_.b.name, [128, 128])
            S.pool(lambda e, T_=T_, n_=n_: e.tensor_scalar(out=n_[:], in0=T_[:], scalar1=-1.0, scalar2=None, op0=ALU.mult),
                   r=[T_.b], w=[n_.b])
            negs[T_.b.name] = n_
        for d in range(2):
            for k_, (T_, sg_) in mdefs[d].items():
                MASK[(k_, d)] = T_ if sg_ > 0 else negs[T_.b.name]

        colt = sb(gst, "colt", [128, c.NCOLS])
        negw0 = sb(gst, "negw0", [128, 2 * P]); omka = sb(gst, "omka", [128, P])
        nega0 = sb(gst, "nega0", [128, 2 * P]); negv0 = sb(gst, "negv0", [128, P])
        MEAN = sb(gst, "MEAN", [128, TB]); RSTD = sb(gst, "RSTD", [128, TB])
        S1 = sb(gst, "S1acc", [128, TB]); S2 = sb(gst, "S2acc", [128, TB])

        def col(key):
            i = c.cols[key]
            return colt[:, i:i + 1]

        def colrows(key, n):
            i = c.cols[key]
            return colt[0:n, i:i + 1]

        def gemm(st, name, panels, nk, rhs, TBw, epi, banks, nsets, NW=256, KP=8, WNB=6, PF=3):
            ntb = TBw // 512
            nwt = NW // 128
            per = nwt * ntb
            assert per * nsets <= len(banks)
            wring = [sb(st, f"{name}_w{i}", [128, KP, NW], BF16) for i in range(WNB)]
            pieces = []
            for pi, (wsrc, c0, ncols, tag) in enumerate(panels):
                for k0 in range(0, nk, KP):
                    pieces.append((pi, k0, min(KP, nk - k0)))
            loaded = 0

            def load(i):
                pi, k0, kn = pieces[i]
                wsrc, c0, ncols, tag = panels[pi]
                slot = wring[i % WNB]
                src = wsrc.rearrange("(kc p) n -> p kc n", p=128)[:, k0:k0 + kn, c0:c0 + ncols]
                S.dma("pool", slot[:, 0:kn, 0:ncols], src, w=[slot.b])
            for i, (pi, k0, kn) in enumerate(pieces):
                while loaded < min(len(pieces), i + PF + 1):
                    load(loaded)
                    loaded += 1
                wsrc, c0, ncols, tag = panels[pi]
                slot = wring[i % WNB]
                bset = banks[(pi % nsets) * per:(pi % nsets) * per + per]
                nots = (ncols + 127) // 128
                last_piece = (k0 + kn >= nk)
                for kc in range(kn):
                    for ot in range(nots):
                        m = min(128, ncols - ot * 128)
                        for tb in range(ntb):
                            bank = bset[ot * ntb + tb]
                            lastmm = (kc == kn - 1 and ot == nots - 1 and tb == ntb - 1)
                            S.pe(lambda e, bank=bank, slot=slot, kc=kc, ot=ot, m=m, tb=tb, k0=k0:
                                 e.matmul(bank[0:m, :], lhsT=slot[:, kc, ot * 128:ot * 128 + m],
                                          rhs=rhs[:, k0 + kc, tb * 512:(tb + 1) * 512],
                                          start=(k0 + kc == 0), stop=(k0 + kc == nk - 1)),
                                 r=[slot.b, rhs.b], w=[bank.b], inc=lastmm)
                if last_piece:
                    for ot in range(nots):
                        m = min(128, ncols - ot * 128)
                        for tb in range(ntb):
                            epi(tag, c0, ot, m, tb, bset[ot * ntb + tb])

        def wview(name, row0, K):
            return I[name][row0:row0 + K, :]

        def load_rhs(st, tname, src2d, nk, t0, TBw, cast=False):
            T_ = sb(st, tname, [128, nk, TBw], BF16)
            v = src2d.rearrange("(kc p) t -> p kc t", p=128)
            step = 8
            for k0 in range(0, nk, step):
                kn = min(step, nk - k0)
                S.dma("pool" if cast else "sp", T_[:, k0:k0 + kn, :], v[:, k0:k0 + kn, t0:t0 + TBw], w=[T_.b])
            return T_

        def ring(st, name, n, shape, dt=F32):
            tiles = [sb(st, f"{name}{i}", shape, dt) for i in range(n)]
            state = [0]

            def nxt():
                t = tiles[state[0] % n]
                state[0] += 1
                return t
            return nxt

        def layer_setup(l):
            S.dma("sp", colt[:], I["cols"][l * 128:(l + 1) * 128, :], w=[colt.b])
            for d in range(2):
                for p in range(P):
                    S.dve(lambda e, d=d, p=p: e.tensor_scalar(out=negw0[:, d * P + p:d * P + p + 1], in0=col(("w0", d, p)),
                                                              scalar1=-1.0, scalar2=None, op0=ALU.mult), r=[colt.b], w=[negw0.b])
            for d in range(2):
                for p in range(P):
                    S.dve(lambda e, d=d, p=p: e.tensor_scalar(out=nega0[:, d * P + p:d * P + p + 1], in0=col(("a0", d, p)),
                                                              scalar1=-1.0, scalar2=None, op0=ALU.mult), r=[colt.b], w=[nega0.b])
            for p in range(P):
                S.dve(lambda e, p=p: e.tensor_scalar(out=omka[:, p:p + 1], in0=col(("k_a", p)), scalar1=-1.0, scalar2=1.0,
                                                     op0=ALU.mult, op1=ALU.add), r=[colt.b], w=[omka.b])
                S.dve(lambda e, p=p: e.tensor_scalar(out=negv0[:, p:p + 1], in0=col(("v0", p)), scalar1=-1.0, scalar2=None,
                                                     op0=ALU.mult), r=[colt.b], w=[negv0.b])

        def phase_A(l):
            cin = c.CIN0 if l == 0 else c.CIN
            wsrc = I["w_in0"] if l == 0 else WB[("w_in_next", l - 1)]
            for t0 in range(0, NT, TB):
                with contextlib.ExitStack() as st:
                    if l == 0:
                        XT = load_rhs(st, "A_xt", I["xT"], NFT, t0, TB, cast=True)
                    else:
                        XT = load_rhs(st, "A_xt", XB1, NFT, t0, TB)
                    stg_f = ring(st, "A_sf", 4, [128, 512], F32)
                    stg_b = ring(st, "A_sb", 4, [128, 512], BF16)
                    panels = [(wsrc, c0, min(256, cin - c0), None) for c0 in range(0, cin, 256)]

                    def epi(tag, c0, ot, m, tb, bank):
                        cc = c0 + ot * 128
                        tok = slice(t0 + tb * 512, t0 + tb * 512 + 512)
                        if cc < DG:
                            o = stg_b()
                            S.act(lambda e: e.activation(out=o[0:m, :], in_=bank[0:m, :], func=AF.Gelu_apprx_tanh), r=[bank.b], w=[o.b])
                            S.dma("sp", UG[cc:cc + m, tok], o[0:m, :], r=[o.b])
                        elif cc < 2 * DG:
                            o = stg_f()
                            S.act(lambda e: e.activation(out=o[0:m, :], in_=bank[0:m, :], func=AF.Gelu_apprx_tanh), r=[bank.b], w=[o.b])
                            S.dma("sp", VG[cc - DG:cc - DG + m, tok], o[0:m, :], r=[o.b])
                        else:
                            o = stg_f()
                            S.dve(lambda e: e.tensor_copy(out=o[0:m, :], in_=bank[0:m, :]), r=[bank.b], w=[o.b])
                            S.dma("sp", ZR[cc - 2 * DG:cc - 2 * DG + m, tok], o[0:m, :], r=[o.b])
                    gemm(st, "A", panels, NFT, XT, TB, epi, PS, 2)
                    S.barrier()

        def phase_B1(l):
            with contextlib.ExitStack() as st:
                Gb = sb(st, "B1_g", [128, DG]); Bb = sb(st, "B1_b", [128, DG]); BSb = sb(st, "B1_bs", [128, NHG * 128])
                WST = sb(st, "B1_wst", [128, NHG * 128], BF16)
                S.dma("sp", Gb[:], I["sgu_ln_g"][l:l + 1, :].partition_broadcast(128), w=[Gb.b])
                S.dma("sp", Bb[:], I["sgu_ln_b"][l:l + 1, :].partition_broadcast(128), w=[Bb.b])
                S.dma("sp", BSb[:], I["b_s"][l:l + 1, :].partition_broadcast(128), w=[BSb.b])
                S.dma("pool", WST[:], I["w_sT"][l * 128:(l + 1) * 128, :], w=[WST.b])
                nb = (NHG + 3) // 4
                VGc_r = ring(st, "B1_vg", 2, [128, NHG, 128], F32)
                UGc_r = ring(st, "B1_ug", 2, [128, NHG, 128], BF16)
                VN = sb(st, "B1_vn", [128, DG]); VNB = sb(st, "B1_vnb", [128, DG], BF16)
                STt = sb(st, "B1_st", [128, nb, 6]); MV = sb(st, "B1_mv", [128, 2]); RS = sb(st, "B1_rs", [128, 1])
                TMP = sb(st, "B1_tmp", [128, NHG * 128]); YG_r = ring(st, "B1_yg", 2, [128, NHG, 128], BF16)
                for ch in range(NT // 128):
                    tok = slice(ch * 128, ch * 128 + 128)
                    VGc = VGc_r(); UGc = UGc_r(); YG = YG_r()
                    S.dma("sp", VGc[:], VG.rearrange("(h p) t -> p h t", p=128)[:, :, tok], w=[VGc.b])
                    S.dma("sp", UGc[:], UG.rearrange("(h p) t -> p h t", p=128)[:, :, tok], w=[UGc.b])
                    for h in range(NHG):
                        bank = PS[h // 4]
                        S.pe(lambda e, h=h, bank=bank: e.matmul(bank[:, (h % 4) * 128:(h % 4) * 128 + 128], lhsT=VGc[:, h, :], rhs=ident_f[:],
                                                                start=True, stop=True), r=[VGc.b, ident_f.b], w=[bank.b],
                             inc=(h % 4 == 3 or h == NHG - 1))
                    for b in range(nb):
                        w_ = min(512, DG - b * 512)
                        S.dve(lambda e, b=b, w_=w_: e.bn_stats(out=STt[:, b, :], in_=PS[b][:, 0:w_]), r=[PS[b].b], w=[STt.b])
                    S.dve(lambda e: e.bn_aggr(out=MV[:], in_=STt[:]), r=[STt.b], w=[MV.b])
                    S.act(lambda e: e.activation(out=RS[:], in_=MV[:, 1:2], func=AF.Sqrt, bias=c.LN_EPS, scale=1.0), r=[MV.b], w=[RS.b])
                    S.dve(lambda e: e.reciprocal(out=RS[:], in_=RS[:]), r=[RS.b], w=[RS.b])
                    for b in range(nb):
                        w_ = min(512, DG - b * 512)
                        S.dve(lambda e, b=b, w_=w_: e.tensor_scalar(out=VN[:, b * 512:b * 512 + w_], in0=PS[b][:, 0:w_], scalar1=MV[:, 0:1],
                                                                    scalar2=RS[:, 0:1], op0=ALU.subtract, op1=ALU.mult),
                              r=[PS[b].b, MV.b, RS.b], w=[VN.b])
                    S.pool(lambda e: e.tensor_tensor(out=VN[:], in0=VN[:], in1=Gb[:], op=ALU.mult), r=[VN.b, Gb.b], w=[VN.b])
                    S.pool(lambda e: e.tensor_tensor(out=VNB[:], in0=VN[:], in1=Bb[:], op=ALU.add), r=[VN.b, Bb.b], w=[VNB.b])
                    for h in range(NHG):
                        bank = PS[4 + h // 4]
                        S.pe(lambda e, h=h, bank=bank: e.matmul(bank[:, (h % 4) * 128:(h % 4) * 128 + 128], lhsT=VNB[:, h * 128:(h + 1) * 128],
                                                                rhs=WST[:, h * 128:(h + 1) * 128], start=True, stop=True),
                             r=[VNB.b, WST.b], w=[bank.b], inc=(h % 4 == 3 or h == NHG - 1))
                    for b in range(nb):
                        w_ = min(512, DG - b * 512)
                        S.dve(lambda e, b=b, w_=w_: e.tensor_tensor(out=TMP[:, b * 512:b * 512 + w_], in0=PS[4 + b][:, 0:w_],
                                                                    in1=BSb[:, b * 512:b * 512 + w_], op=ALU.add),
                              r=[PS[4 + b].b, BSb.b], w=[TMP.b])
                    S.pool(lambda e: e.tensor_tensor(out=YG[:].rearrange("p h t -> p (h t)"), in0=TMP[:],
                                                     in1=UGc[:].rearrange("p h t -> p (h t)"), op=ALU.mult), r=[TMP.b, UGc.b], w=[YG.b])
                    S.dma("sp", MIX[0:DG, :].rearrange("(h p) t -> p h t", p=128)[:, :, tok], YG[:], r=[YG.b])
                S.barrier()

        TN = 512

        def phase_B2(l):
            nchb = TN // 64
            with contextlib.ExitStack() as st:
                W2 = [sb(st, f"B2_w2{d}", [96, DR], BF16) for d in range(2)]
                A2 = [sb(st, f"B2_a2{d}", [96, DR], BF16) for d in range(2)]
                G2 = sb(st, "B2_g2", [128, 2, DR], BF16)
                for d in range(2):
                    S.dma("pool", W2[d][:], I["w2"][(l * 2 + d) * 96:(l * 2 + d + 1) * 96, :], w=[W2[d].b])
                    S.dma("pool", A2[d][:], I["a2"][(l * 2 + d) * 96:(l * 2 + d + 1) * 96, :], w=[A2[d].b])
                S.dma("pool", G2[:], I["g2"][l * 256:(l + 1) * 256, :].rearrange("(k p) n -> p k n", p=128), w=[G2.b])
                if l > 0:
                    V2 = sb(st, "B2_v2", [64, DR], BF16)
                    S.dma("pool", V2[:], I["v2"][(l - 1) * 64:l * 64, :], w=[V2.b])

                def fsb(name, dt=F32):
                    return sb(st, "B2_" + name, [128, TN], dt)
                TW = [fsb("tw0", BF16), fsb("tw1", BF16)]; AD = [fsb("ad0", BF16), fsb("ad1", BF16)]
                SG = [fsb("sg0", BF16), fsb("sg1", BF16)]; MVt = fsb("mv", BF16)
                OUTS = [ring(st, f"B2_o{i}", 2, [128, TN], BF16) for i in range(6)]
                psb = [0]

                def nbank():
                    b = PS[psb[0] % 8]
                    psb[0] += 1
                    return b

                class TS:
                    pass
                SETS = []
                for si in range(2):
                    t = TS()
                    for nm in ("R_", "K_", "V_", "KK", "EW", "A_", "T1", "KD", "B_", "E_", "DM", "F_", "G_", "XA", "XB_", "XC", "XD", "TMP", "E1"):
                        setattr(t, nm, fsb(f"{nm}{si}"))
                    t.VBt = fsb(f"vb{si}", BF16)
                    t.WLt = sb(st, f"B2_wl{si}", [128, nchb])
                    t.Zr = ring(st, f"B2_z{si}", 3, [128, TN + 2], F32)
                    SETS.append(t)

                def load_z(Zring, nrows, row0, t0):
                    Z = Zring()
                    lo = t0 - 1
                    hi = t0 + TN + 1
                    c_lo, c_hi = 0, TN + 2
                    if t0 == 0:
                        S.dve(lambda e: e.memset(Z[0:nrows, 0:1], 0.0), w=[Z.b])
                        lo, c_lo = 0, 1
                    if t0 + TN == NT:
                        S.dve(lambda e: e.memset(Z[0:nrows, TN + 1:TN + 2], 0.0), w=[Z.b])
                        hi, c_hi = NT, TN + 1
                    S.dma("sp", Z[0:nrows, c_lo:c_hi], ZR[row0:row0 + nrows, lo:hi], w=[Z.b])
                    if t0 == SEG:
                        S.dve(lambda e: e.tensor_scalar(out=Z[0:nrows, 0:1], in0=Z[0:nrows, 0:1], scalar1=linkt[0:nrows, 0:1], scalar2=None,
                                                        op0=ALU.mult), r=[Z.b, linkt.b], w=[Z.b])
                    if t0 + TN == SEG:
                        S.dve(lambda e: e.tensor_scalar(out=Z[0:nrows, TN + 1:TN + 2], in0=Z[0:nrows, TN + 1:TN + 2], scalar1=linkt[0:nrows, 0:1],
                                                        scalar2=None, op0=ALU.mult), r=[Z.b, linkt.b], w=[Z.b])
                    return Z

                def conv3(eng, Z, dst, TMP, nrows, cvkey):
                    w = [colt[0:nrows, c.cols[cvkey + (j,)]:c.cols[cvkey + (j,)] + 1] for j in range(3)]
                    eng(lambda e: e.tensor_scalar(out=TMP[0:nrows, :], in0=Z[0:nrows, 0:TN], scalar1=w[0], scalar2=None, op0=ALU.mult),
                        r=[Z.b, colt.b], w=[TMP.b])
                    S.dve(lambda e: e.scalar_tensor_tensor(out=TMP[0:nrows, :], in0=Z[0:nrows, 1:TN + 1], scalar=w[1], in1=TMP[0:nrows, :],
                                                           op0=ALU.mult, op1=ALU.add), r=[Z.b, colt.b, TMP.b], w=[TMP.b])
                    S.dve(lambda e: e.scalar_tensor_tensor(out=dst[0:nrows, :], in0=Z[0:nrows, 2:TN + 2], scalar=w[2], in1=TMP[0:nrows, :],
                                                           op0=ALU.mult, op1=ALU.add), r=[Z.b, colt.b, TMP.b], w=[dst.b])

                def pair_gen(p, t0, T):
                    tok = slice(t0, t0 + TN)
                    rows = slice(128 * p, 128 * p + 128)
                    R_, K_, V_, KK, EW, A_, T1, KD, B_, E_, DM, F_, G_, XA, XB_, XC, XD, TMP, E1 = (
                        T.R_, T.K_, T.V_, T.KK, T.EW, T.A_, T.T1, T.KD, T.B_, T.E_, T.DM, T.F_, T.G_, T.XA, T.XB_, T.XC, T.XD, T.TMP, T.E1)
                    Zr_ = load_z(T.Zr, 128, 128 * p, t0)
                    Zk_ = load_z(T.Zr, 128, DR + 128 * p, t0)
                    Zv_ = load_z(T.Zr, 128, 2 * DR + 128 * p, t0)
                    if l > 0:
                        S.dma("sp", XD[:], VF[rows, tok], w=[XD.b])
                    yield
                    conv3(S.dve, Zr_, R_, TMP, 128, ("cv_r", p))
                    conv3(S.pool, Zk_, K_, TMP, 128, ("cv_k", p))
                    yield
                    conv3(S.pool, Zv_, V_, TMP, 128, ("cv_v", p))
                    if l == 0:
                        S.dma("sp", VF[rows, tok], V_[:], r=[V_.b])
                    else:
                        bk = nbank()
                        S.pe(lambda e: e.matmul(bk[:, :], lhsT=V2[:, rows], rhs=MVt[0:64, :], start=True, stop=True), r=[V2.b, MVt.b], w=[bk.b])
                        S.act(lambda e: e.activation(out=XC[:], in_=bk[:, :], func=AF.Exp, bias=negv0[:, p:p + 1], scale=-1.0), r=[bk.b, negv0.b], w=[XC.b])
                        S.pool(lambda e: e.tensor_scalar(out=XC[:], in0=XC[:], scalar1=1.0, scalar2=None, op0=ALU.add), r=[XC.b], w=[XC.b])
                        S.dve(lambda e: e.reciprocal(out=XC[:], in_=XC[:]), r=[XC.b], w=[XC.b])
                        S.pool(lambda e: e.tensor_tensor(out=XD[:], in0=XD[:], in1=V_[:], op=ALU.subtract), r=[XD.b, V_.b], w=[XD.b])
                        S.pool(lambda e: e.tensor_tensor(out=XD[:], in0=XD[:], in1=XC[:], op=ALU.mult), r=[XD.b, XC.b], w=[XD.b])
                        S.pool(lambda e: e.tensor_tensor(out=V_[:], in0=V_[:], in1=XD[:], op=ALU.add), r=[V_.b, XD.b], w=[V_.b])
                    S.pool(lambda e: e.tensor_copy(out=T.VBt[:], in_=V_[:]), r=[V_.b], w=[T.VBt.b])
                    S.dma("sp", VB[rows, tok], T.VBt[:], r=[T.VBt.b])
                    yield
                    bk = nbank()
                    for k2 in range(2):
                        S.pe(lambda e, k2=k2: e.matmul(bk[:, :], lhsT=G2[:, k2, rows], rhs=SG[k2][:], start=(k2 == 0), stop=(k2 == 1)),
                             r=[G2.b, SG[k2].b], w=[bk.b], inc=(k2 == 1))
                    S.dve(lambda e: e.tensor_copy(out=F_[:], in_=bk[:, :]), r=[bk.b], w=[F_.b])
                    S.dma("sp", GG[rows, tok], F_[:], r=[F_.b])
                    S.dve(lambda e: e.tensor_scalar(out=KK[:], in0=K_[:], scalar1=col(("k_k", p)), scalar2=None, op0=ALU.mult),
                          r=[K_.b, colt.b], w=[KK.b])
                    S.pool(lambda e: e.tensor_tensor(out=XA[:], in0=KK[:], in1=KK[:], op=ALU.mult), r=[KK.b], w=[XA.b])
                    bk = nbank()
                    S.pe(lambda e: e.matmul(bk[:, :], lhsT=onesbd[:], rhs=XA[:], start=True, stop=True), r=[onesbd.b, XA.b], w=[bk.b])
                    S.act(lambda e: e.activation(out=XB_[:], in_=bk[:, :], func=AF.Ln, bias=c.L2_EPS, scale=1.0), r=[bk.b], w=[XB_.b])
                    S.act(lambda e: e.activation(out=XB_[:], in_=XB_[:], func=AF.Exp, scale=-0.5), r=[XB_.b], w=[XB_.b])
                    S.dve(lambda e: e.tensor_tensor(out=KK[:], in0=KK[:], in1=XB_[:], op=ALU.mult), r=[KK.b, XB_.b], w=[KK.b])
                    yield
                    for d in range(2):
                        bk = nbank()
                        S.pe(lambda e, d=d: e.matmul(bk[:, :], lhsT=W2[d][:, rows], rhs=TW[d][0:96, :], start=True, stop=True),
                             r=[W2[d].b, TW[d].b], w=[bk.b])
                        S.act(lambda e, d=d: e.activation(out=E1[:], in_=bk[:, :], func=AF.Exp, bias=negw0[:, d * P + p:d * P + p + 1], scale=-1.0),
                              r=[bk.b, negw0.b], w=[E1.b])
                        S.act(lambda e: e.activation(out=E1[:], in_=E1[:], func=AF.Ln, bias=1.0, scale=1.0), r=[E1.b], w=[E1.b])
                        S.act(lambda e: e.activation(out=EW[:], in_=E1[:], func=AF.Exp, bias=-0.5, scale=-1.0), r=[E1.b], w=[EW.b])
                        bk = nbank()
                        S.pe(lambda e, d=d: e.matmul(bk[:, :], lhsT=A2[d][:, rows], rhs=AD[d][0:96, :], start=True, stop=True),
                             r=[A2[d].b, AD[d].b], w=[bk.b])
                        S.act(lambda e, d=d: e.activation(out=A_[:], in_=bk[:, :], func=AF.Exp, bias=nega0[:, d * P + p:d * P + p + 1], scale=-1.0),
                              r=[bk.b, nega0.b], w=[A_.b])
                        S.pool(lambda e: e.tensor_scalar(out=A_[:], in0=A_[:], scalar1=1.0, scalar2=None, op0=ALU.add), r=[A_.b], w=[A_.b])
                        S.dve(lambda e: e.reciprocal(out=A_[:], in_=A_[:]), r=[A_.b], w=[A_.b])
                        yield
                        S.dve(lambda e: e.tensor_scalar(out=T1[:], in0=A_[:], scalar1=col(("k_a", p)), scalar2=omka[:, p:p + 1],
                                                        op0=ALU.mult, op1=ALU.add), r=[A_.b, colt.b, omka.b], w=[T1.b])
                        S.dve(lambda e: e.tensor_tensor(out=KD[:], in0=K_[:], in1=T1[:], op=ALU.mult), r=[K_.b, T1.b], w=[KD.b])
                        S.pool(lambda e: e.tensor_tensor(out=B_[:], in0=KK[:], in1=A_[:], op=ALU.mult), r=[KK.b, A_.b], w=[B_.b])
                        if d == 0:
                            S.dve(lambda e: e.scalar_tensor_tensor(out=T1[:], in0=R_[:], scalar=col(("r_k", p)), in1=KD[:],
                                                                   op0=ALU.mult, op1=ALU.mult), r=[R_.b, colt.b, KD.b], w=[T1.b])
                            bk = nbank()
                            S.pe(lambda e: e.matmul(bk[:, :], lhsT=onesbd[:], rhs=T1[:], start=True, stop=True), r=[onesbd.b, T1.b], w=[bk.b])
                            S.dve(lambda e: e.tensor_tensor(out=G_[:], in0=bk[:, :], in1=V_[:], op=ALU.mult), r=[bk.b, V_.b], w=[G_.b])
                            S.dma("sp", BON[rows, tok], G_[:], r=[G_.b])
                        S.dve(lambda e: e.tensor_tensor_scan(out=E_[:], data0=M01[:, 0:TN], data1=EW[:], initial=0.0, op0=ALU.mult, op1=ALU.add),
                              r=[M01.b, EW.b], w=[E_.b])
                        E3 = E_[:].rearrange("p (c t) -> p c t", t=64)
                        S.dve(lambda e: e.tensor_tensor(out=DM[:].rearrange("p (c t) -> p c t", t=64), in0=E3,
                                                        in1=E3[:, :, 63:64].to_broadcast([128, nchb, 64]), op=ALU.subtract), r=[E_.b], w=[DM.b])
                        S.pool(lambda e: e.tensor_tensor(out=F_[:], in0=EW[:], in1=E_[:], op=ALU.subtract), r=[EW.b, E_.b], w=[F_.b])
                        yield
                        S.act(lambda e: e.activation(out=T.WLt[:].rearrange("p (c o) -> p c o", o=1), in_=E3[:, :, 63:64], func=AF.Exp, scale=-1.0),
                              r=[E_.b], w=[T.WLt.b])
                        S.dma("sp", SC[("WL", d)][rows, t0 // 64:t0 // 64 + nchb], T.WLt[:], r=[T.WLt.b])
                        S.act(lambda e: e.activation(out=XB_[:], in_=F_[:], func=AF.Exp), r=[F_.b], w=[XB_.b])
                        S.act(lambda e: e.activation(out=XD[:], in_=DM[:], func=AF.Exp), r=[DM.b], w=[XD.b])
                        if d == 0:
                            S.act(lambda e: e.activation(out=XA[:], in_=E_[:], func=AF.Exp, scale=-1.0), r=[E_.b], w=[XA.b])
                            S.act(lambda e: e.activation(out=XC[:], in_=E_[:], func=AF.Exp), r=[E_.b], w=[XC.b])
                            prods = [("RT", R_, XA, 1), ("KKT", KK, XB_, 1), ("KH", KD, XC, 1), ("BH", B_, XC, 1),
                                     ("KEND", KD, XD, 1), ("NBEND", B_, XD, -1)]
                        else:
                            S.pool(lambda e: e.tensor_tensor(out=G_[:], in0=EW[:], in1=DM[:], op=ALU.subtract), r=[EW.b, DM.b], w=[G_.b])
                            S.act(lambda e: e.activation(out=XC[:], in_=G_[:], func=AF.Exp), r=[G_.b], w=[XC.b])
                            prods = [("RT", R_, XD, 1), ("KKT", KK, XD, 1), ("KH", KD, XC, 1), ("BH", B_, XC, 1),
                                     ("KEND", KD, XB_, 1), ("NBEND", B_, XB_, -1)]
                        yield
                        for i_, (nm, a_, x_, sgn) in enumerate(prods):
                            o = OUTS[i_]()
                            eng = S.dve if (i_ % 2 == 0 or sgn < 0) else S.pool
                            if sgn > 0:
                                eng(lambda e, o=o, a_=a_, x_=x_: e.tensor_tensor(out=o[:], in0=a_[:], in1=x_[:], op=ALU.mult),
                                    r=[a_.b, x_.b], w=[o.b])
                            else:
                                eng(lambda e, o=o, a_=a_, x_=x_: e.scalar_tensor_tensor(out=o[:], in0=a_[:], scalar=-1.0, in1=x_[:],
                                                                                        op0=ALU.mult, op1=ALU.mult), r=[a_.b, x_.b], w=[o.b])
                            S.dma("sp", SC[(nm, d)][rows, tok], o[:], r=[o.b])
                        yield

                E1g = SETS[0].E1
                TMPg = SETS[0].TMP
                Zg = SETS[0].Zr
                for t0 in range(0, NT, TN):
                    base = 3 * DR
                    for d, nm in enumerate(("cv_wdf", "cv_wdb")):
                        Z = load_z(Zg, 96, base + 96 * d, t0)
                        conv3(S.dve, Z, E1g, TMPg, 96, (nm,))
                        S.act(lambda e, d=d: e.activation(out=TW[d][0:96, :], in_=E1g[0:96, :], func=AF.Tanh), r=[E1g.b], w=[TW[d].b])
                    for k2, nm in enumerate(("cv_gd0", "cv_gd1")):
                        Z = load_z(Zg, 128, base + 384 + 128 * k2, t0)
                        conv3(S.dve, Z, E1g, TMPg, 128, (nm,))
                        S.act(lambda e, k2=k2: e.activation(out=SG[k2][:], in_=E1g[:], func=AF.Sigmoid), r=[E1g.b], w=[SG[k2].b])
                    for d, nm in enumerate(("cv_adf", "cv_adb")):
                        Z = load_z(Zg, 96, base + 192 + 96 * d, t0)
                        conv3(S.dve, Z, E1g, TMPg, 96, (nm,))
                        S.pool(lambda e, d=d: e.tensor_copy(out=AD[d][0:96, :], in_=E1g[0:96, :]), r=[E1g.b], w=[AD[d].b])
                    if l > 0:
                        Z = load_z(Zg, 64, base + 640, t0)
                        conv3(S.dve, Z, E1g, TMPg, 64, ("cv_mv",))
                        S.pool(lambda e: e.tensor_copy(out=MVt[0:64, :], in_=E1g[0:64, :]), r=[E1g.b], w=[MVt.b])
                    for p0 in range(0, P, 2):
                        interleave([pair_gen(p, t0, SETS[p - p0]) for p in range(p0, min(P, p0 + 2))])
                S.barrier()

        PB = c.PB if getattr(c, "PB", None) else min(4, P)
        GC = 2
        OPN = ("KKT", "RT", "BH", "KH", "KEND", "NBEND", "V")

        def interleave(gens):
            gens = list(gens)
            while gens:
                for g_ in list(gens):
                    try:
                        next(g_)
                    except StopIteration:
                        gens.remove(g_)

        def convert_weights(l):
            for (src, dst) in WCONV[l]:
                rows = src.shape[0]
                step = 512
                for r0 in range(0, rows, step):
                    r1 = min(rows, r0 + step)
                    S.dma("pool", dst[r0:r1, :], src[r0:r1, :])

        def phase_B3(l):
            with contextlib.ExitStack() as st:
                MR = {}
                for d in range(2):
                    for k_ in ("X1", "X1t", "NAybT", "AukT", "AykT"):
                        src = MASK[(k_, d)]
                        key = src.b.name
                        if key not in MR:
                            t_ = sb(st, "B3_m_" + key, [128, PB, 128])
                            for q in range(PB):
                                S.pool(lambda e, q=q, t_=t_, src=src: e.tensor_copy(out=t_[:, q, :], in_=src[:]), r=[src.b], w=[t_.b])
                            MR[key] = t_
                IDR = sb(st, "B3_idr", [128, PB, 128], BF16)
                for q in range(PB):
                    S.pool(lambda e, q=q: e.tensor_copy(out=IDR[:, q, :], in_=ident_b[:]), r=[ident_b.b], w=[IDR.b])
                WLa = sb(st, "B3_wl", [128, P, NCH])
                bank_i = [0]
                evac_i = [0]

                def nbank():
                    b = PS[bank_i[0] % 8]
                    bank_i[0] += 1
                    return b

                def stage(nq, mms):
                    bank = nbank()
                    for q in range(nq):
                        lst = mms(q)
                        for i, (l_, r_, bufs) in enumerate(lst):
                            S.pe(lambda e, l_=l_, r_=r_, q=q, i=i, n=len(lst): e.matmul(bank[:, q * 128:(q + 1) * 128], lhsT=l_, rhs=r_,
                                                                                     start=(i == 0), stop=(i == n - 1)),
                                 r=bufs, w=[bank.b], inc=(q == nq - 1 and i == len(lst) - 1))
                    return bank

                def evac_copy(bank, dst, nq):
                    evac_i[0] += 1
                    o = dst[:, 0:nq, :].rearrange("p q t -> p (q t)")
                    if evac_i[0] % 3 == 0:
                        S.dve(lambda e: e.tensor_copy(out=o, in_=bank[:, 0:nq * 128]), r=[bank.b], w=[dst.b])
                    else:
                        S.act(lambda e: e.copy(out=o, in_=bank[:, 0:nq * 128]), r=[bank.b], w=[dst.b])

                def evac_mask(bank, dst, nq, mask):
                    o = dst[:, 0:nq, :].rearrange("p q t -> p (q t)")
                    m_ = mask[:, 0:nq, :].rearrange("p q t -> p (q t)")
                    S.dve(lambda e: e.tensor_tensor(out=o, in0=bank[:, 0:nq * 128], in1=m_, op=ALU.mult), r=[bank.b, mask.b], w=[dst.b])

                class Res:
                    pass
                RES = []
                for ri in range(2):
                    R_ = Res()
                    R_.OPS = []
                    for i in range(2):
                        dct = {}
                        for nm in OPN:
                            t_ = sb(st, f"B3_op{ri}{i}_{nm}", [128, PB, GC, 128], BF16)
                            S.pool(lambda e, t_=t_: e.memset(t_[:], 0.0), w=[t_.b])
                            dct[nm] = t_
                        R_.OPS.append(dct)

                    def tb(name, dt=BF16, ri=ri):
                        return sb(st, f"B3_{name}{ri}", [128, PB, 128], dt)
                    R_.X = [tb("x0"), tb("x1")]; R_.Xt = [tb("xt0"), tb("xt1")]; R_.Tt = [tb("tt0"), tb("tt1")]
                    R_.KKTT = tb("kktt"); R_.KENDT = tb("kendt"); R_.NBENDT = tb("nbendt"); R_.VT = tb("vt")
                    R_.NAybT = tb("naybt"); R_.AukT = tb("aukt"); R_.AykT = tb("aykt"); R_.KTT = tb("ktt"); R_.AV = tb("av"); R_.U = tb("u")
                    R_.Hf = tb("hf", F32); R_.Hb = tb("hb")
                    R_.Ys = [tb("ys0", F32), tb("ys1", F32)]
                    RES.append(R_)

                def batch_gen(d, p0, nq, R_):
                    X, Xt, Tt = R_.X, R_.Xt, R_.Tt
                    KKTT, KENDT, NBENDT, VT = R_.KKTT, R_.KENDT, R_.NBENDT, R_.VT
                    NAybT, AukT, AykT, KTT, AV, U, Hf, Hb, Ys, OPS = R_.NAybT, R_.AukT, R_.AykT, R_.KTT, R_.AV, R_.U, R_.Hf, R_.Hb, R_.Ys, R_.OPS
                    Yd = SC[("Y", d)]
                    ngroups = NCH // GC
                    gorder = list(range(ngroups)) if d == 0 else list(range(ngroups - 1, -1, -1))

                    def load_ops(gi, slot):
                        g = gorder[gi]
                        for nm in OPN:
                            src = VB if nm == "V" else SC[(nm, d)]
                            t_ = OPS[slot][nm]
                            for q in range(nq):
                                for h in range(2):
                                    r0 = 128 * (p0 + q) + 64 * h
                                    S.dma("sp", t_[64 * h:64 * h + 64, q, :, 64 * h:64 * h + 64],
                                          src[r0:r0 + 64, g * GC * 64:(g + 1) * GC * 64].rearrange("p (c t) -> p c t", t=64), w=[t_.b])
                    load_ops(0, 0)
                    S.dve(lambda e: e.memset(Hf[:], 0.0), w=[Hf.b])
                    S.dve(lambda e: e.memset(Hb[:], 0.0), w=[Hb.b])
                    yield
                    for gi in range(ngroups):
                        if gi + 1 < ngroups:
                            load_ops(gi + 1, (gi + 1) % 2)
                        O = OPS[gi % 2]
                        g = gorder[gi]
                        corder = list(range(GC)) if d == 0 else list(range(GC - 1, -1, -1))
                        for cl in corder:
                            ch = g * GC + cl
                            if (d == 0 and ch * 64 == SEG) or (d == 1 and (ch + 1) * 64 == SEG):
                                S.dve(lambda e: e.tensor_scalar(out=Hf[:].rearrange("p q t -> p (q t)"), in0=Hf[:].rearrange("p q t -> p (q t)"),
                                                                scalar1=linkt[:, 0:1], scalar2=None, op0=ALU.mult), r=[Hf.b, linkt.b], w=[Hf.b])
                                S.act(lambda e: e.copy(out=Hb[:].rearrange("p q t -> p (q t)"), in_=Hf[:].rearrange("p q t -> p (q t)")),
                                      r=[Hf.b], w=[Hb.b])

                            def op(nm, q):
                                return O[nm][:, q, cl, :]
                            mk = lambda k_: MR[MASK[(k_, d)].b.name]
                            bk = stage(nq, lambda q: [(op("KKT", q), op("BH", q), [O["KKT"].b, O["BH"].b])])
                            evac_mask(bk, X[0], nq, mk("X1"))
                            bk = stage(nq, lambda q: [(op("BH", q), op("KKT", q), [O["KKT"].b, O["BH"].b])])
                            evac_mask(bk, Xt[0], nq, mk("X1t"))
                            yield
                            bk = stage(nq, lambda q: [(op("BH", q), op("RT", q), [O["RT"].b, O["BH"].b])])
                            evac_mask(bk, NAybT, nq, mk("NAybT"))
                            bk = stage(nq, lambda q: [(op("KH", q), op("KKT", q), [O["KKT"].b, O["KH"].b])])
                            evac_mask(bk, AukT, nq, mk("AukT"))
                            yield
                            bk = stage(nq, lambda q: [(op("KH", q), op("RT", q), [O["RT"].b, O["KH"].b])])
                            evac_mask(bk, AykT, nq, mk("AykT"))
                            for (nm, dst) in (("KKT", KKTT), ("KEND", KENDT), ("NBEND", NBENDT), ("V", VT)):
                                bk = stage(nq, lambda q, nm=nm: [(op(nm, q), ident_b[:], [O[nm].b, ident_b.b])])
                                evac_copy(bk, dst, nq)
                                yield
                            S.dve(lambda e: e.tensor_tensor(out=Tt[0][:, 0:nq, :], in0=Xt[0][:, 0:nq, :], in1=IDR[:, 0:nq, :], op=ALU.add),
                                  r=[Xt[0].b, IDR.b], w=[Tt[0].b])
                            cur = 0
                            for k in range(1, 6):
                                nx = 1 - cur
                                bk = stage(nq, lambda q: [(Xt[cur][:, q, :], X[cur][:, q, :], [Xt[cur].b, X[cur].b])])
                                evac_copy(bk, X[nx], nq)
                                if k < 5:
                                    bk = stage(nq, lambda q: [(X[cur][:, q, :], Xt[cur][:, q, :], [Xt[cur].b, X[cur].b])])
                                    evac_copy(bk, Xt[nx], nq)
                                yield
                                tcur = (k - 1) % 2
                                bk = stage(nq, lambda q: [(X[nx][:, q, :], Tt[tcur][:, q, :], [X[nx].b, Tt[tcur].b])])
                                S.dve(lambda e, bk=bk, tcur=tcur: e.tensor_tensor(out=Tt[1 - tcur][:, 0:nq, :].rearrange("p q t -> p (q t)"),
                                                                                  in0=bk[:, 0:nq * 128],
                                                                                  in1=Tt[tcur][:, 0:nq, :].rearrange("p q t -> p (q t)"), op=ALU.add),
                                      r=[bk.b, Tt[tcur].b], w=[Tt[1 - tcur].b])
                                cur = nx
                                yield
                            TT = Tt[1]
                            bk = stage(nq, lambda q: [(KKTT[:, q, :], TT[:, q, :], [KKTT.b, TT.b])])
                            evac_copy(bk, KTT, nq)
                            bk = stage(nq, lambda q: [(AukT[:, q, :], VT[:, q, :], [AukT.b, VT.b])])
                            evac_copy(bk, AV, nq)
                            yield
                            bk = stage(nq, lambda q: [(TT[:, q, :], AV[:, q, :], [TT.b, AV.b]), (KTT[:, q, :], Hb[:, q, :], [KTT.b, Hb.b])])
                            evac_copy(bk, U, nq)
                            yield
                            bk = stage(nq, lambda q: [(Hb[:, q, :], op("RT", q), [Hb.b, O["RT"].b]), (VT[:, q, :], AykT[:, q, :], [VT.b, AykT.b]),
                                                      (U[:, q, :], NAybT[:, q, :], [U.b, NAybT.b])])
                            Y_ = Ys[ch % 2]
                            evac_copy(bk, Y_, nq)
                            for h in range(2):
                                S.dma("sp", Yd.rearrange("(pp r) t -> r pp t", r=128)[64 * h:64 * h + 64, p0:p0 + nq, ch * 64:ch * 64 + 64],
                                      Y_[64 * h:64 * h + 64, 0:nq, 64 * h:64 * h + 64], r=[Y_.b])
                            bk = stage(nq, lambda q: [(KENDT[:, q, :], VT[:, q, :], [KENDT.b, VT.b]), (NBENDT[:, q, :], U[:, q, :], [NBENDT.b, U.b])])
                            for q in range(nq):
                                S.dve(lambda e, q=q, bk=bk: e.scalar_tensor_tensor(out=Hf[:, q, :], in0=Hf[:, q, :], scalar=WLa[:, p0 + q, ch:ch + 1],
                                                                                   in1=bk[:, q * 128:(q + 1) * 128], op0=ALU.mult, op1=ALU.add),
                                      r=[Hf.b, WLa.b, bk.b], w=[Hf.b])
                            S.act(lambda e: e.copy(out=Hb[:, 0:nq, :].rearrange("p q t -> p (q t)"), in_=Hf[:, 0:nq, :].rearrange("p q t -> p (q t)")),
                                  r=[Hf.b], w=[Hb.b])
                            yield

                convert_weights(l)
                for d in range(2):
                    S.dma("sp", WLa[:], SC[("WL", d)].rearrange("(pp p) c -> p pp c", p=128), w=[WLa.b])
                    batches = list(range(0, P, PB))
                    for bi in range(0, len(batches), 2):
                        gens = []
                        for k_, p0 in enumerate(batches[bi:bi + 2]):
                            gens.append(batch_gen(d, p0, min(PB, P - p0), RES[k_]))
                        interleave(gens)
                S.barrier()

        def phase_B4(l):
            with contextlib.ExitStack() as st:
                def fsb(name, n=2, dt=F32):
                    return ring(st, "B4_" + name, n, [128, TN], dt)
                YA = fsb("ya"); YB_ = fsb("yb"); SQ = fsb("sq", 1); MEANt = fsb("mean", 1); MSQ = fsb("msq", 1); VAR = fsb("var", 1)
                BONt = fsb("bon"); GGt = fsb("gg"); OUT = fsb("out", 2, BF16)
                for t0 in range(0, NT, TN):
                    tok = slice(t0, t0 + TN)
                    for p in range(P):
                        rows = slice(128 * p, 128 * p + 128)
                        ya = YA(); yb = YB_(); sq = SQ(); mean = MEANt(); msq = MSQ(); var = VAR(); bon = BONt(); gg = GGt(); out = OUT()
                        S.dma("sp", ya[:], SC[("Y", 0)][rows, tok], w=[ya.b])
                        S.dma("sp", yb[:], SC[("Y", 1)][rows, tok], w=[yb.b])
                        S.dma("sp", bon[:], BON[rows, tok], w=[bon.b])
                        S.dma("sp", gg[:], GG[rows, tok], w=[gg.b])
                        S.pool(lambda e: e.tensor_tensor(out=ya[:], in0=ya[:], in1=yb[:], op=ALU.add), r=[ya.b, yb.b], w=[ya.b])
                        S.act(lambda e: e.activation(out=sq[:], in_=ya[:], func=AF.Square), r=[ya.b], w=[sq.b])
                        S.pe(lambda e: e.matmul(PS[0][:, :], lhsT=onesbd[:], rhs=ya[:], start=True, stop=True), r=[onesbd.b, ya.b], w=[PS[0].b])
                        S.pe(lambda e: e.matmul(PS[1][:, :], lhsT=onesbd[:], rhs=sq[:], start=True, stop=True), r=[onesbd.b, sq.b], w=[PS[1].b])
                        S.act(lambda e: e.activation(out=mean[:], in_=PS[0][:, :], func=AF.Copy, scale=1.0 / 64.0), r=[PS[0].b], w=[mean.b])
                        S.dve(lambda e: e.tensor_tensor(out=msq[:], in0=mean[:], in1=mean[:], op=ALU.mult), r=[mean.b], w=[msq.b])
                        S.dve(lambda e: e.scalar_tensor_tensor(out=var[:], in0=PS[1][:, :], scalar=1.0 / 64.0, in1=msq[:], op0=ALU.mult, op1=ALU.subtract),
                              r=[PS[1].b, msq.b], w=[var.b])
                        S.act(lambda e: e.activation(out=var[:], in_=var[:], func=AF.Sqrt, bias=c.GN_EPS, scale=1.0), r=[var.b], w=[var.b])
                        S.dve(lambda e: e.reciprocal(out=var[:], in_=var[:]), r=[var.b], w=[var.b])
                        S.dve(lambda e: e.tensor_tensor(out=ya[:], in0=ya[:], in1=mean[:], op=ALU.subtract), r=[ya.b, mean.b], w=[ya.b])
                        S.dve(lambda e: e.tensor_tensor(out=ya[:], in0=ya[:], in1=var[:], op=ALU.mult), r=[ya.b, var.b], w=[ya.b])
                        S.dve(lambda e: e.tensor_scalar(out=ya[:], in0=ya[:], scalar1=col(("gn_g", p)), scalar2=col(("gn_b", p)), op0=ALU.mult, op1=ALU.add),
                              r=[ya.b, colt.b], w=[ya.b])
                        S.pool(lambda e: e.tensor_tensor(out=ya[:], in0=ya[:], in1=bon[:], op=ALU.add), r=[ya.b, bon.b], w=[ya.b])
                        S.pool(lambda e: e.tensor_tensor(out=out[:], in0=ya[:], in1=gg[:], op=ALU.mult), r=[ya.b, gg.b], w=[out.b])
                        S.dma("sp", MIX[DG + 128 * p:DG + 128 * p + 128, tok], out[:], r=[out.b])
                S.barrier()

        def ln_stats(st, TBw):
            msq = sb(st, "ln_msq", [128, TBw])
            for tb in range(TBw // 512):
                cs = slice(tb * 512, tb * 512 + 512)
                S.pe(lambda e: e.matmul(PS[0][:, :], lhsT=ones_f[:], rhs=S1[:, cs], start=True, stop=True), r=[ones_f.b, S1.b], w=[PS[0].b])
                S.pe(lambda e: e.matmul(PS[1][:, :], lhsT=ones_f[:], rhs=S2[:, cs], start=True, stop=True), r=[ones_f.b, S2.b], w=[PS[1].b])
                S.act(lambda e: e.activation(out=MEAN[:, cs], in_=PS[0][:, :], func=AF.Copy, scale=1.0 / D), r=[PS[0].b], w=[MEAN.b])
                S.dve(lambda e: e.tensor_tensor(out=msq[:, cs], in0=MEAN[:, cs], in1=MEAN[:, cs], op=ALU.mult), r=[MEAN.b], w=[msq.b])
                S.dve(lambda e: e.scalar_tensor_tensor(out=RSTD[:, cs], in0=PS[1][:, :], scalar=1.0 / D, in1=msq[:, cs], op0=ALU.mult, op1=ALU.subtract),
                      r=[PS[1].b, msq.b], w=[RSTD.b])
                S.act(lambda e: e.activation(out=RSTD[:, cs], in_=RSTD[:, cs], func=AF.Sqrt, bias=c.LN_EPS, scale=1.0), r=[RSTD.b], w=[RSTD.b])
                S.dve(lambda e: e.reciprocal(out=RSTD[:, cs], in_=RSTD[:, cs]), r=[RSTD.b], w=[RSTD.b])

        def resid_epi(st, name, xsrc, hdst, t0, cs_off=0):
            xr = ring(st, name + "_x", 2, [128, 512]); hr = ring(st, name + "_h", 2, [128, 512]); sqr = ring(st, name + "_sq", 1, [128, 512])

            def epi(tag, c0, ot, m, tb, bank):
                cc = c0 + ot * 128
                tok = slice(t0 + tb * 512, t0 + tb * 512 + 512)
                cs = slice(cs_off + tb * 512, cs_off + tb * 512 + 512)
                x = xr(); h = hr(); sq = sqr()
                S.dma("sp", x[:], xsrc[cc:cc + 128, tok], w=[x.b])
                S.dve(lambda e: e.scalar_tensor_tensor(out=h[:], in0=x[:], scalar=float(c.ALPHA), in1=bank[:, :], op0=ALU.mult, op1=ALU.add),
                      r=[x.b, bank.b], w=[h.b])
                S.dma("sp", hdst[cc:cc + 128, tok], h[:], r=[h.b])
                S.act(lambda e: e.activation(out=sq[:], in_=h[:], func=AF.Square), r=[h.b], w=[sq.b])
                S.pool(lambda e: e.tensor_tensor(out=S1[:, cs], in0=S1[:, cs], in1=h[:], op=ALU.add), r=[S1.b, h.b], w=[S1.b])
                S.pool(lambda e: e.tensor_tensor(out=S2[:, cs], in0=S2[:, cs], in1=sq[:], op=ALU.add), r=[S2.b, sq.b], w=[S2.b])
            return epi

        def zero_stats():
            S.pool(lambda e: e.memset(S1[:], 0.0), w=[S1.b])
            S.pool(lambda e: e.memset(S2[:], 0.0), w=[S2.b])

        def ln_apply(st, name, hsrc, t0, TBw, gkey, bkey, fdst, XT):
            hr = ring(st, name + "_h", 2, [128, TBw]); xr = ring(st, name + "_xo", 2, [128, TBw])
            for ft in range(NFT):
                rows = slice(128 * ft, 128 * ft + 128)
                h = hr(); x = xr()
                S.dma("sp", h[:], hsrc[rows, t0:t0 + TBw], w=[h.b])
                S.dve(lambda e: e.tensor_tensor(out=h[:], in0=h[:], in1=MEAN[:, 0:TBw], op=ALU.subtract), r=[h.b, MEAN.b], w=[h.b])
                S.dve(lambda e: e.tensor_tensor(out=h[:], in0=h[:], in1=RSTD[:, 0:TBw], op=ALU.mult), r=[h.b, RSTD.b], w=[h.b])
                S.act(lambda e: e.activation(out=x[:], in_=h[:], func=AF.Identity, bias=col((bkey, ft)), scale=col((gkey, ft))),
                      r=[h.b, colt.b], w=[x.b])
                S.dma("sp", fdst[rows, t0:t0 + TBw], x[:], r=[x.b])
                S.pool(lambda e: e.tensor_copy(out=XT[:, ft, :], in_=x[:]), r=[x.b], w=[XT.b])

        def phase_C(l, t0):
            xsrc = I["xT"] if l == 0 else XF1
            last = (l == DEPTH - 1)
            j = l // 2
            moe = (l % 2 == 1)
            with contextlib.ExitStack() as st:
                XT = load_rhs(st, "C1_xt", MIX, NFT, t0, TB)
                zero_stats()
                epi = resid_epi(st, "C1", xsrc, H1, t0)
                wsrc = WB[("w_out", l)]
                gemm(st, "C1", [(wsrc, c0, 256, None) for c0 in range(0, D, 256)], NFT, XT, TB, epi, PS, 2)
                ln_stats(st, TB)
                S.barrier()
            with contextlib.ExitStack() as st:
                XT = sb(st, "D1_xt", [128, NFT, TB], BF16)
                with contextlib.ExitStack() as st2:
                    ln_apply(st2, "C2", H1, t0, TB, "ln1_g", "ln1_b", X1F, XT)
                    S.barrier()
                GT = None
                if moe:
                    GT = sb(st, "D1_gt", [128, NE, TB])
                    st_outer = st
                    st = st_outer.enter_context(contextlib.ExitStack())
                    RT_ = sb(st, "D1_rt", [128, NFT, NE]); XF_r = ring(st, "D1_xf", 1, [128, NFT, 128])
                    LG = sb(st, "D1_lg", [128, NE]); M1 = sb(st, "D1_m1", [128, 1]); M2 = sb(st, "D1_m2", [128, 1])
                    EQ1 = sb(st, "D1_eq1", [128, NE]); EQ2 = sb(st, "D1_eq2", [128, NE]); LG2 = sb(st, "D1_lg2", [128, NE])
                    G1 = sb(st, "D1_g1", [128, 1]); G2_ = sb(st, "D1_g2", [128, 1]); GA = sb(st, "D1_ga", [128, NE])
                    S.dma("sp", RT_[:], I["router"][j * D:(j + 1) * D, :].rearrange("(k p) e -> p k e", p=128), w=[RT_.b])
                    for tt in range(TB // 128):
                        xf = XF_r()
                        S.dma("sp", xf[:], X1F.rearrange("(k p) t -> p k t", p=128)[:, :, t0 + tt * 128:t0 + tt * 128 + 128], w=[xf.b])
                        for k in range(NFT):
                            S.pe(lambda e, k=k: e.matmul(PS[0][:, 0:NE], lhsT=xf[:, k, :], rhs=RT_[:, k, :], start=(k == 0), stop=(k == NFT - 1)),
                                 r=[xf.b, RT_.b], w=[PS[0].b], inc=(k == NFT - 1))
                        S.dve(lambda e: e.tensor_copy(out=LG[:], in_=PS[0][:, 0:NE]), r=[PS[0].b], w=[LG.b])
                        S.dve(lambda e: e.tensor_reduce(out=M1[:], in_=LG[:], axis=AX.X, op=ALU.max), r=[LG.b], w=[M1.b])
                        S.dve(lambda e: e.tensor_scalar(out=EQ1[:], in0=LG[:], scalar1=M1[:, 0:1], scalar2=None, op0=ALU.is_equal), r=[LG.b, M1.b], w=[EQ1.b])
                        S.dve(lambda e: e.scalar_tensor_tensor(out=LG2[:], in0=EQ1[:], scalar=-1.0e30, in1=LG[:], op0=ALU.mult, op1=ALU.add),
                              r=[EQ1.b, LG.b], w=[LG2.b])
                        S.dve(lambda e: e.tensor_reduce(out=M2[:], in_=LG2[:], axis=AX.X, op=ALU.max), r=[LG2.b], w=[M2.b])
                        S.dve(lambda e: e.tensor_scalar(out=EQ2[:], in0=LG2[:], scalar1=M2[:, 0:1], scalar2=None, op0=ALU.is_equal), r=[LG2.b, M2.b], w=[EQ2.b])
                        S.dve(lambda e: e.tensor_tensor(out=G1[:], in0=M1[:], in1=M2[:], op=ALU.subtract), r=[M1.b, M2.b], w=[G1.b])
                        S.act(lambda e: e.activation(out=G1[:], in_=G1[:], func=AF.Sigmoid), r=[G1.b], w=[G1.b])
                        S.dve(lambda e: e.tensor_scalar(out=G2_[:], in0=G1[:], scalar1=-1.0, scalar2=1.0, op0=ALU.mult, op1=ALU.add), r=[G1.b], w=[G2_.b])
                        S.dve(lambda e: e.tensor_scalar(out=GA[:], in0=EQ1[:], scalar1=G1[:, 0:1], scalar2=None, op0=ALU.mult), r=[EQ1.b, G1.b], w=[GA.b])
                        S.dve(lambda e: e.scalar_tensor_tensor(out=GA[:], in0=EQ2[:], scalar=G2_[:, 0:1], in1=GA[:], op0=ALU.mult, op1=ALU.add),
                              r=[EQ2.b, G2_.b, GA.b], w=[GA.b])
                        for e_ in range(NE):
                            bank = PS[1 + e_ // 4]
                            S.pe(lambda e, e_=e_, bank=bank: e.matmul(bank[:, (e_ % 4) * 128:(e_ % 4) * 128 + 128], lhsT=GA[:, e_:e_ + 1].to_broadcast([128, 128]),
                                                                      rhs=ident_f[:], start=True, stop=True), r=[GA.b, ident_f.b], w=[bank.b],
                                 inc=(e_ % 4 == 3))
                        for hb_ in range(NE // 4):
                            S.act(lambda e, hb_=hb_: e.copy(out=GT[:, hb_ * 4:hb_ * 4 + 4, tt * 128:tt * 128 + 128],
                                                            in_=PS[1 + hb_][:, :].rearrange("p (a t) -> p a t", t=128)), r=[PS[1 + hb_].b], w=[GT.b])
                    S.barrier()
                    st.close()
                    st = st_outer
                Gbuf = sb(st, "D1_g", [128, 2, TB], BF16)
                hr = ring(st, "D1_h", 3, [128, 512], BF16)
                panels = []
                if not moe:
                    wg = WB[("ffg", l)]; wu = WB[("ffu", l)]
                    for f0 in range(0, DFF, 256):
                        panels.append((wg, f0, min(256, DFF - f0), ("g", f0, None)))
                        panels.append((wu, f0, min(256, DFF - f0), ("u", f0, None)))
                else:
                    for e_ in range(NE):
                        wg = WB[("ffg", l)][e_ * D:(e_ + 1) * D, :]; wu = WB[("ffu", l)][e_ * D:(e_ + 1) * D, :]
                        for f0 in range(0, DFFE, 256):
                            panels.append((wg, f0, min(256, DFFE - f0), ("g", e_ * DFFE + f0, e_)))
                            panels.append((wu, f0, min(256, DFFE - f0), ("u", e_ * DFFE + f0, e_)))

                def epi_ff(tag, c0, ot, m, tb, bank):
                    kind, hrow0, e_ = tag
                    cs = slice(tb * 512, tb * 512 + 512)
                    if kind == "g":
                        S.act(lambda e: e.activation(out=Gbuf[0:m, ot, cs], in_=bank[0:m, :], func=AF.Silu), r=[bank.b], w=[Gbuf.b])
                    else:
                        h = hr()
                        S.dve(lambda e: e.tensor_tensor(out=h[0:m, :], in0=bank[0:m, :], in1=Gbuf[0:m, ot, cs], op=ALU.mult), r=[bank.b, Gbuf.b], w=[h.b])
                        if e_ is not None:
                            S.pool(lambda e: e.tensor_tensor(out=h[0:m, :], in0=h[0:m, :], in1=GT[0:m, e_, cs], op=ALU.mult), r=[h.b, GT.b], w=[h.b])
                        r0 = hrow0 + ot * 128
                        S.dma("sp", HH[r0:r0 + m, t0 + tb * 512:t0 + tb * 512 + 512], h[0:m, :], r=[h.b])
                gemm(st, "D1", panels, NFT, XT, TB, epi_ff, PS, 2)
                S.barrier()
            kff = (DFF if not moe else NE * DFFE)
            nkf = kff // 128
            wd = WB[("ffd", l)]
            zero_stats()
            for sbk in range(TB // 512):
                with contextlib.ExitStack() as st:
                    t1 = t0 + sbk * 512
                    HT = load_rhs(st, "D2_ht", HH[0:kff, :], nkf, t1, 512)
                    epi2 = resid_epi(st, "D2", X1F, H2, t1, cs_off=sbk * 512)
                    gemm(st, "D2", [(wd, c0, 512, None) for c0 in range(0, D, 512)], nkf, HT, 512, epi2, PS, 2, NW=512, KP=4, WNB=4, PF=2)
                    S.barrier()
            with contextlib.ExitStack() as st:
                ln_stats(st, TB)
                XT = sb(st, "E_xt", [128, NFT, TB], BF16)
                with contextlib.ExitStack() as st2:
                    ln_apply(st2, "E1", H2, t0, TB, "ln2_g", "ln2_b", X2F, XT)
                    S.barrier()
                nkp = PLE // 128
                PTt = load_rhs(st, "E_pt", I["pT"][l * PLE:(l + 1) * PLE, :], nkp, t0, TB, cast=True)
                WPP = sb(st, "E_wpp", [128, nkp, D], BF16)
                S.dma("pool", WPP[:], I["w_pproj"][l * PLE:(l + 1) * PLE, :].rearrange("(k p) n -> p k n", p=128), w=[WPP.b])
                xr = ring(st, "E_x", 3, [128, 512]); sr = ring(st, "E_s", 2, [128, 512]); orr = ring(st, "E_o", 3, [128, 512])
                obr = ring(st, "E_ob", 3, [128, 512], BF16)
                ppb = [0]
                dstF = yT if last else XF1

                def epi_e(tag, c0, ot, m, tb, bank):
                    cc = c0 + ot * 128
                    tok = slice(t0 + tb * 512, t0 + tb * 512 + 512)
                    pb = PS[4 + ppb[0] % 4]
                    ppb[0] += 1
                    x = xr(); s = sr(); o = orr()
                    S.dma("sp", x[:], X2F[cc:cc + 128, tok], w=[x.b])
                    for k in range(nkp):
                        S.pe(lambda e, k=k: e.matmul(pb[:, :], lhsT=WPP[:, k, cc:cc + 128], rhs=PTt[:, k, tb * 512:tb * 512 + 512], start=(k == 0), stop=(k == nkp - 1)),
                             r=[WPP.b, PTt.b], w=[pb.b], inc=(k == nkp - 1))
                    S.act(lambda e: e.activation(out=s[:], in_=bank[:, :], func=AF.Sigmoid), r=[bank.b], w=[s.b])
                    S.dve(lambda e: e.tensor_tensor(out=s[:], in0=s[:], in1=pb[:, :], op=ALU.mult), r=[s.b, pb.b], w=[s.b])
                    S.dve(lambda e: e.tensor_tensor(out=o[:], in0=s[:], in1=x[:], op=ALU.add), r=[s.b, x.b], w=[o.b])
                    S.dma("sp", dstF[cc:cc + 128, tok], o[:], r=[o.b])
                    if not last:
                        ob = obr()
                        S.pool(lambda e: e.tensor_copy(out=ob[:], in_=o[:]), r=[o.b], w=[ob.b])
                        S.dma("sp", XB1[cc:cc + 128, tok], ob[:], r=[ob.b])
                wsrc = WB[("w_pgate", l)]
                gemm(st, "E", [(wsrc, c0, 256, None) for c0 in range(0, D, 256)], NFT, XT, TB, epi_e, PS[0:4], 1)
                S.barrier()

        for l in range(DEPTH):
            layer_setup(l)
            S.barrier()
            phase_A(l)
            phase_B1(l)
            phase_B2(l)
            phase_B3(l)
            phase_B4(l)
            for t0 in range(0, NT, TB):
                phase_C(l, t0)
        S.barrier()
        nc._sched_stats = (S.n_ops, S.n_waits, dict(S.count), dict(S.dma_n), len(S.semmap))
    return nc, dbg


def shared_inputs(cfg, W):
    c = cfg
    DEPTH, D = c.DEPTH, c.D
    f = lambda a: np.ascontiguousarray(a, dtype=np.float32)
    sh = {}
    sh["w_in0"] = f(W["w_in0"])
    if DEPTH > 1:
        sh["w_in"] = f(W["w_in"]).reshape((DEPTH - 1) * D, c.CIN)
        sh["v2"] = f(W["v2"]).reshape((DEPTH - 1) * 64, c.DR)
    sh["w_out"] = f(W["w_out"]).reshape(DEPTH * D, D)
    sh["w_pgate"] = f(W["w_pgate"]).reshape(DEPTH * D, D)
    sh["w_pproj"] = f(W["w_pproj"]).reshape(DEPTH * c.PLE, D)
    sh["w_ff_gate"] = f(W["w_ff_gate"]).reshape(-1, c.DFF)
    sh["w_ff_up"] = f(W["w_ff_up"]).reshape(-1, c.DFF)
    sh["w_ff_down"] = f(W["w_ff_down"]).reshape(-1, D)
    if DEPTH // 2:
        sh["router"] = f(W["router"]).reshape(-1, c.NE)
        sh["we_gate"] = f(W["we_gate"]).reshape(-1, c.DFFE)
        sh["we_up"] = f(W["we_up"]).reshape(-1, c.DFFE)
        sh["we_down"] = f(W["we_down"]).reshape(-1, D)
    sh["sgu_ln_g"] = f(W["sgu_ln_g"]); sh["sgu_ln_b"] = f(W["sgu_ln_b"])
    sh["b_s"] = f(W["b_s"]).reshape(DEPTH, c.NHG * 128)
    sh["w_sT"] = f(np.transpose(np.asarray(W["w_s"]), (0, 3, 1, 2))).reshape(DEPTH * 128, c.NHG * 128)
    sh["w2"] = f(W["w2"]).reshape(DEPTH * 2 * 96, c.DR)
    sh["a2"] = f(W["a2"]).reshape(DEPTH * 2 * 96, c.DR)
    sh["g2"] = f(W["g2"]).reshape(DEPTH * 256, c.DR)
    Wn = {k: np.asarray(v) for k, v in W.items()}
    sh["cols"] = np.concatenate([pack_cols(c, l, Wn) for l in range(DEPTH)], axis=0)
    return sh


def core_tokens(cfg, x_prompt, x_sample, p_prompt, p_sample, core):
    nb = x_prompt.shape[0]
    if core < nb:
        return x_prompt[core], p_prompt[:, core], 1.0
    k = core - nb
    x = np.concatenate([x_sample[2 * k], x_sample[2 * k + 1]], axis=0)
    p = np.concatenate([p_sample[:, 2 * k], p_sample[:, 2 * k + 1]], axis=1)
    return x, p, 0.0


def run(cfg, inputs, debug_outs=()):
    c = cfg
    nc, dbg = build(c, debug_outs)
    W = {k: v for k, v in inputs.items() if k not in ("x_prompt", "x_sample", "p_prompt", "p_sample")}
    sh = shared_inputs(c, W)
    xp, xs = np.asarray(inputs["x_prompt"]), np.asarray(inputs["x_sample"])
    pp, psm = np.asarray(inputs["p_prompt"]), np.asarray(inputs["p_sample"])
    in_maps = []
    for core in range(c.n_cores):
        x, p, link = core_tokens(c, xp, xs, pp, psm, core)
        m = dict(sh)
        m["xT"] = np.ascontiguousarray(x.T, dtype=np.float32)
        m["pT"] = np.ascontiguousarray(np.transpose(p, (0, 2, 1)), dtype=np.float32).reshape(c.DEPTH * c.PLE, c.NT)
        m["link"] = np.full((128, 1), link, np.float32)
        in_maps.append(m)
    res = run_bass_kernel_spmd(nc, in_maps, core_ids=list(range(c.n_cores)))
    nb = xp.shape[0]
    y_prompt = np.empty(xp.shape, np.float32)
    y_sample = np.empty(xs.shape, np.float32)
    for core in range(c.n_cores):
        y = np.ascontiguousarray(res.results[core]["yT"].T)
        if core < nb:
            y_prompt[core] = y
        else:
            k = core - nb
            y_sample[2 * k] = y[:c.SEG]
            y_sample[2 * k + 1] = y[c.SEG:]
    return (y_prompt, y_sample), res


def kernel(x_prompt, x_sample, p_prompt, p_sample, w_in0, conv0, w_in, conv, sgu_ln_g, sgu_ln_b,
           w_s, b_s, w0, w2, a0, a2, g2, k_k, k_a, r_k, gn_g, gn_b, v0, v2, w_out,
           ln1_g, ln1_b, ln2_g, ln2_b, w_ff_gate, w_ff_up, w_ff_down, router, we_gate, we_up,
           we_down, w_pproj, w_pgate):
    inputs = dict(x_prompt=x_prompt, x_sample=x_sample, p_prompt=p_prompt, p_sample=p_sample, w_in0=w_in0, conv0=conv0,
                  w_in=w_in, conv=conv, sgu_ln_g=sgu_ln_g, sgu_ln_b=sgu_ln_b, w_s=w_s, b_s=b_s, w0=w0, w2=w2, a0=a0, a2=a2,
                  g2=g2, k_k=k_k, k_a=k_a, r_k=r_k, gn_g=gn_g, gn_b=gn_b, v0=v0, v2=v2, w_out=w_out, ln1_g=ln1_g, ln1_b=ln1_b,
                  ln2_g=ln2_g, ln2_b=ln2_b, w_ff_gate=w_ff_gate, w_ff_up=w_ff_up, w_ff_down=w_ff_down, router=router,
                  we_gate=we_gate, we_up=we_up, we_down=we_down, w_pproj=w_pproj, w_pgate=w_pgate)
    cfg = Cfg()
    (y_prompt, y_sample), _ = run(cfg, inputs)
    return (y_prompt, y_sample)
```

```python
import contextlib
import numpy as np
import concourse.bass as bass
import concourse.mybir as mybir
from concourse.bass_utils import run_bass_kernel_spmd

F32 = mybir.dt.float32
BF16 = mybir.dt.bfloat16
AF = mybir.ActivationFunctionType
ALU = mybir.AluOpType
AX = mybir.AxisListType

SEM_LIMIT = 28000


class Buf:
    __slots__ = ("name", "writers", "dma_w", "readers", "dma_r")

    def __init__(self, name=""):
        self.name = name
        self.writers = {}
        self.dma_w = []
        self.readers = {}
        self.dma_r = []


class Sched:
    ENGS = ("pe", "act", "dve", "pool", "sp")

    def __init__(self, nc, stack, n_sems=100, dma_ring=4):
        self.nc = nc
        self.eng = {"pe": nc.tensor, "act": nc.scalar, "dve": nc.vector, "pool": nc.gpsimd, "sp": nc.sync}
        self.count = {e: 0 for e in self.ENGS}
        self.seen = {e: {} for e in self.ENGS}
        self.free_sems = [stack.enter_context(nc.semaphore(f"s{i}")) for i in range(n_sems)]
        self.semmap = {}
        self.dma_ring = dma_ring
        self.dma_n = {q: 0 for q in ("sp", "pool", "act")}
        self.dma_slot_cnt = {}
        self.n_ops = 0
        self.n_waits = 0
        self.pending = {e: False for e in self.ENGS}

    def sem(self, key):
        s = self.semmap.get(key)
        if s is None:
            s = self.free_sems.pop()
            self.semmap[key] = s
        return s

    def _engpos(self, e, seq):
        return (("e", e, (seq - 1) // SEM_LIMIT), (seq - 1) % SEM_LIMIT + 1)

    def _need(self, e, key, val, waits):
        if self.seen[e].get(key, 0) < val:
            self.seen[e][key] = val
            waits.append((key, val))

    def _deps(self, e, r, w, dma_write=False):
        waits = []
        for b in r:
            for (pe_, seq) in b.writers.items():
                if pe_ == e:
                    if seq >= self.count[e] - 2:
                        k, v = self._engpos(pe_, seq)
                        self._need(e, k, v, waits)
                else:
                    k, v = self._engpos(pe_, seq)
                    self._need(e, k, v, waits)
            for (k, v) in b.dma_w:
                self._need(e, k, v, waits)
        for b in w:
            for (pe_, seq) in b.writers.items():
                if pe_ != e:
                    k, v = self._engpos(pe_, seq)
                    self._need(e, k, v, waits)
            if not dma_write:
                for (k, v) in b.dma_w:
                    self._need(e, k, v, waits)
            for (pe_, seq) in b.readers.items():
                if pe_ != e:
                    k, v = self._engpos(pe_, seq)
                    self._need(e, k, v, waits)
            for (k, v) in b.dma_r:
                self._need(e, k, v, waits)
        return waits

    def _emit_waits(self, e, waits):
        eng = self.eng[e]
        for (k, v) in waits:
            eng.wait_ge(self.sem(k), v)
        self.n_waits += len(waits)

    def op(self, e, fn, r=(), w=(), inc=True):
        waits = self._deps(e, r, w)
        self._emit_waits(e, waits)
        ins = fn(self.eng[e])
        self.pending[e] = not inc
        if inc:
            self.count[e] += 1
            seq = self.count[e]
            k, v = self._engpos(e, seq)
            ins.then_inc(self.sem(k), 1)
        else:
            seq = self.count[e] + 1
        for b in w:
            b.writers = {e: seq}
            b.dma_w = []
            b.readers = {}
            b.dma_r = []
        for b in r:
            b.readers[e] = seq
        self.n_ops += 1

    def pe(self, fn, r=(), w=(), inc=True):
        self.op("pe", fn, r, w, inc)

    def act(self, fn, r=(), w=()):
        self.op("act", fn, r, w)

    def dve(self, fn, r=(), w=()):
        self.op("dve", fn, r, w)

    def pool(self, fn, r=(), w=()):
        self.op("pool", fn, r, w)

    def dma(self, q, out, in_, r=(), w=()):
        waits = self._deps(q, r, w, dma_write=True)
        n = self.dma_n[q]
        self.dma_n[q] += 1
        slot = n % self.dma_ring
        cnt = self.dma_slot_cnt.get((q, slot), 0)
        if cnt > 0:
            pk = ("d", q, slot, (cnt - 1) * 16 // SEM_LIMIT)
            pv = ((cnt - 1) * 16) % SEM_LIMIT + 16
            self._need(q, pk, pv, waits)
        key = ("d", q, slot, cnt * 16 // SEM_LIMIT)
        val = (cnt * 16) % SEM_LIMIT + 16
        self.dma_slot_cnt[(q, slot)] = cnt + 1
        tok = (key, val)
        self._emit_waits(q, waits)
        self.eng[q].dma_start(out=out, in_=in_).then_inc(self.sem(key), 16)
        for b in w:
            b.dma_w.append(tok)
        for b in r:
            b.dma_r.append(tok)
        self.n_ops += 1
        return tok

    def barrier(self):
        targets = []
        assert not any(self.pending.values()), self.pending
        for e in self.ENGS:
            if self.count[e] > 0:
                targets.append(self._engpos(e, self.count[e]))
        for (q, slot), cnt in self.dma_slot_cnt.items():
            if cnt > 0:
                targets.append((("d", q, slot, (cnt - 1) * 16 // SEM_LIMIT), ((cnt - 1) * 16) % SEM_LIMIT + 16))
        for e in self.ENGS:
            waits = []
            for (k, v) in targets:
                if k[0] == "e" and k[1] == e:
                    continue
                self._need(e, k, v, waits)
            self._emit_waits(e, waits)


class Tile:
    def __init__(self, t, name):
        self.t = t
        self.b = Buf(name)

    def __getitem__(self, k):
        return self.t[k]


class Cfg:
    def __init__(self, D=4096, NT=4096, SEG=2048, DEPTH=2, TB=1024, n_cores=8, PB=None):
        self.PB = PB
        self.D = D
        self.NT = NT
        self.SEG = SEG
        self.DEPTH = DEPTH
        self.TB = TB
        self.n_cores = n_cores
        self.PLE = 256
        self.DG = D // 2
        self.NHG = self.DG // 128
        self.DR = D - self.DG
        self.NPAIR = self.DR // 128
        self.LD, self.LA, self.LMV, self.LG = 96, 96, 64, 256
        self.C_RW0 = 3 * self.DR + 2 * 96 + 2 * 96 + 256
        self.C_RW = self.C_RW0 + 64
        self.CIN0 = 2 * self.DG + self.C_RW0
        self.CIN = 2 * self.DG + self.C_RW
        self.DFF = 7 * D // 2
        self.DFFE = D // 2
        self.NE = 8
        self.NFT = D // 128
        self.NCH = NT // 64
        self.ALPHA = (2 * DEPTH) ** 0.25
        self.LN_EPS = 1e-5
        self.GN_EPS = 64e-5
        self.L2_EPS = 1e-12
        self.cols = {}
        self._build_cols()

    def _build_cols(self):
        n = 0

        def add(key):
            nonlocal n
            self.cols[key] = n
            n += 1
        P = self.NPAIR
        for nm in ("cv_r", "cv_k", "cv_v"):
            for p in range(P):
                for j in range(3):
                    add((nm, p, j))
        for nm in ("cv_wdf", "cv_wdb", "cv_adf", "cv_adb", "cv_gd0", "cv_gd1", "cv_mv"):
            for j in range(3):
                add((nm, j))
        for nm in ("k_k", "k_a", "r_k", "gn_g", "gn_b", "v0"):
            for p in range(P):
                add((nm, p))
        for nm in ("w0", "a0"):
            for d in range(2):
                for p in range(P):
                    add((nm, d, p))
        for nm in ("ln1_g", "ln1_b", "ln2_g", "ln2_b"):
            for t in range(self.NFT):
                add((nm, t))
        self.NCOLS = n


def pack_cols(cfg, l, W):
    C = np.zeros((128, cfg.NCOLS), np.float32)
    DR, P = cfg.DR, cfg.NPAIR
    conv = W["conv0"] if l == 0 else W["conv"][l - 1]

    def put(key, vec):
        C[:len(vec), cfg.cols[key]] = vec
    for i, nm in enumerate(("cv_r", "cv_k", "cv_v")):
        for p in range(P):
            for j in range(3):
                put((nm, p, j), conv[j, i * DR + 128 * p: i * DR + 128 * p + 128])
    base = 3 * DR
    offs = {"cv_wdf": (0, 96), "cv_wdb": (96, 96), "cv_adf": (192, 96), "cv_adb": (288, 96),
            "cv_gd0": (384, 128), "cv_gd1": (512, 128), "cv_mv": (640, 64)}
    for nm, (o, ln) in offs.items():
        if nm == "cv_mv" and l == 0:
            continue
        for j in range(3):
            put((nm, j), conv[j, base + o: base + o + ln])
    rk = W["r_k"][l].reshape(-1)
    vecs = {"k_k": W["k_k"][l], "k_a": W["k_a"][l], "r_k": rk, "gn_g": W["gn_g"][l], "gn_b": W["gn_b"][l]}
    if l > 0:
        vecs["v0"] = W["v0"][l - 1]
    for nm, v in vecs.items():
        for p in range(P):
            put((nm, p), v[128 * p:128 * p + 128])
    for nm in ("w0", "a0"):
        for d in range(2):
            for p in range(P):
                put((nm, d, p), W[nm][l, d, 128 * p:128 * p + 128])
    for nm in ("ln1_g", "ln1_b", "ln2_g", "ln2_b"):
        for t in range(cfg.NFT):
            put((nm, t), W[nm][l, 128 * t:128 * t + 128])
    return C


def build(cfg, debug_outs=()):
    c = cfg
    nc = bass.Bass("TRN2", target_bir_lowering=False)
    D, NT, TB, DG, DR, P, NHG, NFT, PLE = c.D, c.NT, c.TB, c.DG, c.DR, c.NPAIR, c.NHG, c.NFT, c.PLE
    DEPTH, NCH, SEG = c.DEPTH, c.NCH, c.SEG
    N_DENSE = (DEPTH + 1) // 2
    N_MOE = DEPTH // 2
    NE, DFF, DFFE = c.NE, c.DFF, c.DFFE
    I = {}

    def inp(name, shape):
        I[name] = nc.dram_tensor(name, list(shape), F32, kind="ExternalInput").ap()
        return I[name]
    inp("xT", [D, NT]); inp("pT", [DEPTH * PLE, NT]); inp("link", [128, 1]); inp("cols", [DEPTH * 128, c.NCOLS])
    inp("w_in0", [D, c.CIN0])
    if DEPTH > 1:
        inp("w_in", [(DEPTH - 1) * D, c.CIN])
        inp("v2", [(DEPTH - 1) * 64, DR])
    inp("w_out", [DEPTH * D, D]); inp("w_pgate", [DEPTH * D, D]); inp("w_pproj", [DEPTH * PLE, D])
    inp("w_ff_gate", [N_DENSE * D, DFF]); inp("w_ff_up", [N_DENSE * D, DFF]); inp("w_ff_down", [N_DENSE * DFF, D])
    if N_MOE:
        inp("router", [N_MOE * D, NE]); inp("we_gate", [N_MOE * NE * D, DFFE]); inp("we_up", [N_MOE * NE * D, DFFE])
        inp("we_down", [N_MOE * NE * DFFE, D])
    inp("sgu_ln_g", [DEPTH, DG]); inp("sgu_ln_b", [DEPTH, DG]); inp("b_s", [DEPTH, NHG * 128]); inp("w_sT", [DEPTH * 128, NHG * 128])
    inp("w2", [DEPTH * 2 * 96, DR]); inp("a2", [DEPTH * 2 * 96, DR]); inp("g2", [DEPTH * 256, DR])
    yT = nc.dram_tensor("yT", [D, NT], F32, kind="ExternalOutput").ap()

    dbg = {}

    def scr(name, shape, dt):
        if name in debug_outs:
            t = nc.dram_tensor(name, list(shape), dt, kind="ExternalOutput").ap()
            dbg[name] = t
            return t
        return nc.dram_tensor(name, list(shape), dt, kind="Internal").ap()

    XF1 = scr("XF1", [D, NT], F32); XB1 = scr("XB1", [D, NT], BF16)
    UG = scr("UG", [DG, NT], BF16); VG = scr("VG", [DG, NT], F32); ZR = scr("ZR", [c.C_RW, NT], F32)
    MIX = scr("MIX", [D, NT], BF16)
    SC = {}
    for d in range(2):
        for nm in ("RT", "KKT", "KH", "BH", "KEND", "NBEND"):
            SC[(nm, d)] = scr(f"{nm}{d}", [DR, NT], BF16)
        SC[("WL", d)] = scr(f"WL{d}", [DR, NCH], F32)
        SC[("Y", d)] = scr(f"Y{d}", [DR, NT], F32)
    VB = scr("VB", [DR, NT], BF16); VF = scr("VF", [DR, NT], F32); GG = scr("GG", [DR, NT], F32); BON = scr("BON", [DR, NT], F32)
    H1 = scr("H1", [D, NT], F32); X1F = scr("X1F", [D, NT], F32)
    HH = scr("HH", [max(DFF, NE * DFFE), NT], BF16)
    H2 = scr("H2", [D, NT], F32); X2F = scr("X2F", [D, NT], F32)

    WB = {}
    WCONV = {l_: [] for l_ in range(DEPTH)}

    def wconv(l_, key, src, NW=256, KPp=8, blk=None):
        K_, N_ = src.shape
        blk = blk or K_
        nblk = K_ // blk
        npan = (N_ + NW - 1) // NW
        npc = (blk // 128 + KPp - 1) // KPp
        dst = nc.dram_tensor(f"wb_{key}_{l_}", [nblk * npan * npc * 128, KPp * NW], BF16, kind="Internal").ap()
        dv = dst.rearrange("(i p) (k n) -> i p k n", p=128, n=NW)

        def getter(b_):
            def g(k0, kn, c0, ncols):
                idx = (b_ * npan + c0 // NW) * npc + k0 // KPp
                return dv[idx][:, 0:kn, 0:ncols]
            return g
        WB[(key, l_)] = [getter(b_) for b_ in range(nblk)]
        for b_ in range(nblk):
            sv = src[b_ * blk:(b_ + 1) * blk, :].rearrange("(kc p) n -> p kc n", p=128)
            for pi in range(npan):
                c0 = pi * NW
                ncols = min(NW, N_ - c0)
                for qi in range(npc):
                    k0 = qi * KPp
                    kn = min(KPp, blk // 128 - k0)
                    idx = (b_ * npan + pi) * npc + qi
                    WCONV[l_].append((sv[:, k0:k0 + kn, c0:c0 + ncols], dv[idx][:, 0:kn, 0:ncols]))
    for l_ in range(DEPTH):
        j_ = l_ // 2
        wconv(l_, "w_out", I["w_out"][l_ * D:(l_ + 1) * D, :])
        wconv(l_, "w_pgate", I["w_pgate"][l_ * D:(l_ + 1) * D, :])
        if l_ % 2 == 0:
            wconv(l_, "ffg", I["w_ff_gate"][j_ * D:(j_ + 1) * D, :])
            wconv(l_, "ffu", I["w_ff_up"][j_ * D:(j_ + 1) * D, :])
            wconv(l_, "ffd", I["w_ff_down"][j_ * DFF:(j_ + 1) * DFF, :], NW=512, KPp=4)
        else:
            wconv(l_, "ffg", I["we_gate"][j_ * NE * D:(j_ + 1) * NE * D, :], blk=D)
            wconv(l_, "ffu", I["we_up"][j_ * NE * D:(j_ + 1) * NE * D, :], blk=D)
            wconv(l_, "ffd", I["we_down"][j_ * NE * DFFE:(j_ + 1) * NE * DFFE, :], NW=512, KPp=4)
        if l_ + 1 < DEPTH:
            wconv(l_, "w_in_next", I["w_in"][l_ * D:(l_ + 1) * D, :])

    gst = contextlib.ExitStack()
    with gst:
        S = Sched(nc, gst)

        uid = [0]

        def sb(st, name, shape, dt=F32):
            uid[0] += 1
            nm = f"{name}_{uid[0]}"
            return Tile(st.enter_context(nc.sbuf_tensor(nm, list(shape), dt)), name)
        PS = [Tile(gst.enter_context(nc.psum_tensor(f"psb{i}", [128, 512], F32)), f"psb{i}") for i in range(8)]

        ident_f = sb(gst, "ident_f", [128, 128]); ident_b = sb(gst, "ident_b", [128, 128], BF16)
        ones_f = sb(gst, "ones_f", [128, 128]); onesbd = sb(gst, "onesbd", [128, 128])
        identbd_b = ident_b
        linkt = sb(gst, "linkt", [128, 1])
        M01 = sb(gst, "M01", [128, 512])
        S.dma("sp", linkt[:], I["link"], w=[linkt.b])
        S.pool(lambda e: e.memset(ident_f[:], 0.0), w=[ident_f.b])
        S.pool(lambda e: e.affine_select(out=ident_f[:], in_=ident_f[:], pattern=[[-1, 128]], compare_op=ALU.not_equal,
                                         fill=1.0, base=0, channel_multiplier=1), r=[ident_f.b], w=[ident_f.b])
        S.pool(lambda e: e.tensor_copy(out=ident_b[:], in_=ident_f[:]), r=[ident_f.b], w=[ident_b.b])
        S.pool(lambda e: e.memset(ones_f[:], 1.0), w=[ones_f.b])
        S.pool(lambda e: e.memset(onesbd[:], 0.0), w=[onesbd.b])
        S.pool(lambda e: e.memset(onesbd[0:64, 0:64], 1.0), w=[onesbd.b])
        S.pool(lambda e: e.memset(onesbd[64:128, 64:128], 1.0), w=[onesbd.b])
        S.pool(lambda e: e.memset(M01[:], 1.0), w=[M01.b])
        S.pool(lambda e: e.memset(M01[:].rearrange("p (c t) -> p c t", t=64)[:, :, 0:1], 0.0), w=[M01.b])
        Lst = sb(gst, "Lst", [128, 128]); Ust = sb(gst, "Ust", [128, 128]); Uin = sb(gst, "Uin", [128, 128])
        for (T_, op_, st_, cm_) in ((Lst, ALU.is_gt, -1, 1), (Ust, ALU.is_gt, 1, -1), (Uin, ALU.is_ge, 1, -1)):
            S.pool(lambda e, T_=T_: e.memset(T_[:], 1.0), w=[T_.b])
            S.pool(lambda e, T_=T_, op_=op_, st_=st_, cm_=cm_: e.affine_select(out=T_[:], in_=T_[:], pattern=[[st_, 128]], compare_op=op_,
                                                                               fill=0.0, base=0, channel_multiplier=cm_), r=[T_.b], w=[T_.b])
            S.pool(lambda e, T_=T_: e.memset(T_[64:128, 0:64], 0.0), w=[T_.b])
            S.pool(lambda e, T_=T_: e.memset(T_[0:64, 64:128], 0.0), w=[T_.b])
        MASK = {}
        mdefs = {0: {"X1": (Lst, -1.0), "X1t": (Ust, -1.0), "NAybT": (Uin, -1.0), "AukT": (Ust, 1.0), "AykT": (Uin, 1.0)},
                 1: {"X1": (Ust, -1.0), "X1t": (Lst, -1.0), "NAybT": (Lst, -1.0), "AukT": (Lst, 1.0), "AykT": (Lst, 1.0)}}
        negs = {}
        for T_ in (Lst, Ust, Uin):
            n_ = sb(gst, "neg_" + T_.b.name, [128, 128])
            S.pool(lambda e, T_=T_, n_=n_: e.tensor_scalar(out=n_[:], in0=T_[:], scalar1=-1.0, scalar2=None, op0=ALU.mult),
                   r=[T_.b], w=[n_.b])
            negs[T_.b.name] = n_
        for d in range(2):
            for k_, (T_, sg_) in mdefs[d].items():
                MASK[(k_, d)] = T_ if sg_ > 0 else negs[T_.b.name]

        colt = sb(gst, "colt", [128, c.NCOLS])
        negw0 = sb(gst, "negw0", [128, 2 * P]); omka = sb(gst, "omka", [128, P])
        nega0 = sb(gst, "nega0", [128, 2 * P]); negv0 = sb(gst, "negv0", [128, P])
        MEAN = sb(gst, "MEAN", [128, TB]); RSTD = sb(gst, "RSTD", [128, TB])
        S1 = sb(gst, "S1acc", [128, TB]); S2 = sb(gst, "S2acc", [128, TB])

        def col(key):
            i = c.cols[key]
            return colt[:, i:i + 1]

        def colrows(key, n):
            i = c.cols[key]
            return colt[0:n, i:i + 1]

        def gemm(st, name, panels, nk, rhs, TBw, epi, banks, nsets, NW=256, KP=8, WNB=6, PF=3):
            ntb = TBw // 512
            nwt = NW // 128
            per = nwt * ntb
            assert per * nsets <= len(banks)
            wring = [sb(st, f"{name}_w{i}", [128, KP, NW], BF16) for i in range(WNB)]
            pieces = []
            for pi, (wsrc, c0, ncols, tag) in enumerate(panels):
                for k0 in range(0, nk, KP):
                    pieces.append((pi, k0, min(KP, nk - k0)))
            loaded = 0

            def load(i):
                pi, k0, kn = pieces[i]
                wsrc, c0, ncols, tag = panels[pi]
                slot = wring[i % WNB]
                if callable(wsrc):
                    src = wsrc(k0, kn, c0, ncols)
                else:
                    src = wsrc.rearrange("(kc p) n -> p kc n", p=128)[:, k0:k0 + kn, c0:c0 + ncols]
                S.dma("pool", slot[:, 0:kn, 0:ncols], src, w=[slot.b])
            for i, (pi, k0, kn) in enumerate(pieces):
                while loaded < min(len(pieces), i + PF + 1):
                    load(loaded)
                    loaded += 1
                wsrc, c0, ncols, tag = panels[pi]
                slot = wring[i % WNB]
                bset = banks[(pi % nsets) * per:(pi % nsets) * per + per]
                nots = (ncols + 127) // 128
                last_piece = (k0 + kn >= nk)
                for kc in range(kn):
                    for ot in range(nots):
                        m = min(128, ncols - ot * 128)
                        for tb in range(ntb):
                            bank = bset[ot * ntb + tb]
                            lastmm = (kc == kn - 1 and ot == nots - 1 and tb == ntb - 1)
                            S.pe(lambda e, bank=bank, slot=slot, kc=kc, ot=ot, m=m, tb=tb, k0=k0:
                                 e.matmul(bank[0:m, :], lhsT=slot[:, kc, ot * 128:ot * 128 + m],
                                          rhs=rhs[:, k0 + kc, tb * 512:(tb + 1) * 512],
                                          start=(k0 + kc == 0), stop=(k0 + kc == nk - 1)),
                                 r=[slot.b, rhs.b], w=[bank.b], inc=lastmm)
                if last_piece:
                    for ot in range(nots):
                        m = min(128, ncols - ot * 128)
                        for tb in range(ntb):
                            epi(tag, c0, ot, m, tb, bset[ot * ntb + tb])

        def wview(name, row0, K):
            return I[name][row0:row0 + K, :]

        def load_rhs(st, tname, src2d, nk, t0, TBw, cast=False):
            T_ = sb(st, tname, [128, nk, TBw], BF16)
            v = src2d.rearrange("(kc p) t -> p kc t", p=128)
            step = 8
            for k0 in range(0, nk, step):
                kn = min(step, nk - k0)
                S.dma("pool" if cast else "sp", T_[:, k0:k0 + kn, :], v[:, k0:k0 + kn, t0:t0 + TBw], w=[T_.b])
            return T_

        def ring(st, name, n, shape, dt=F32):
            tiles = [sb(st, f"{name}{i}", shape, dt) for i in range(n)]
            state = [0]

            def nxt():
                t = tiles[state[0] % n]
                state[0] += 1
                return t
            return nxt

        def layer_setup(l):
            S.dma("sp", colt[:], I["cols"][l * 128:(l + 1) * 128, :], w=[colt.b])
            for d in range(2):
                for p in range(P):
                    S.dve(lambda e, d=d, p=p: e.tensor_scalar(out=negw0[:, d * P + p:d * P + p + 1], in0=col(("w0", d, p)),
                                                              scalar1=-1.0, scalar2=None, op0=ALU.mult), r=[colt.b], w=[negw0.b])
            for d in range(2):
                for p in range(P):
                    S.dve(lambda e, d=d, p=p: e.tensor_scalar(out=nega0[:, d * P + p:d * P + p + 1], in0=col(("a0", d, p)),
                                                              scalar1=-1.0, scalar2=None, op0=ALU.mult), r=[colt.b], w=[nega0.b])
            for p in range(P):
                S.dve(lambda e, p=p: e.tensor_scalar(out=omka[:, p:p + 1], in0=col(("k_a", p)), scalar1=-1.0, scalar2=1.0,
                                                     op0=ALU.mult, op1=ALU.add), r=[colt.b], w=[omka.b])
                S.dve(lambda e, p=p: e.tensor_scalar(out=negv0[:, p:p + 1], in0=col(("v0", p)), scalar1=-1.0, scalar2=None,
                                                     op0=ALU.mult), r=[colt.b], w=[negv0.b])

        def phase_A(l):
            cin = c.CIN0 if l == 0 else c.CIN
            wsrc = I["w_in0"] if l == 0 else WB[("w_in_next", l - 1)][0]
            for t0 in range(0, NT, TB):
                with contextlib.ExitStack() as st:
                    if l == 0:
                        XT = load_rhs(st, "A_xt", I["xT"], NFT, t0, TB, cast=True)
                    else:
                        XT = load_rhs(st, "A_xt", XB1, NFT, t0, TB)
                    stg_f = ring(st, "A_sf", 4, [128, 512], F32)
                    stg_b = ring(st, "A_sb", 4, [128, 512], BF16)
                    panels = [(wsrc, c0, min(256, cin - c0), None) for c0 in range(0, cin, 256)]

                    def epi(tag, c0, ot, m, tb, bank):
                        cc = c0 + ot * 128
                        tok = slice(t0 + tb * 512, t0 + tb * 512 + 512)
                        if cc < DG:
                            o = stg_b()
                            S.act(lambda e: e.activation(out=o[0:m, :], in_=bank[0:m, :], func=AF.Gelu_apprx_tanh), r=[bank.b], w=[o.b])
                            S.dma("sp", UG[cc:cc + m, tok], o[0:m, :], r=[o.b])
                        elif cc < 2 * DG:
                            o = stg_f()
                            S.act(lambda e: e.activation(out=o[0:m, :], in_=bank[0:m, :], func=AF.Gelu_apprx_tanh), r=[bank.b], w=[o.b])
                            S.dma("sp", VG[cc - DG:cc - DG + m, tok], o[0:m, :], r=[o.b])
                        else:
                            o = stg_f()
                            S.dve(lambda e: e.tensor_copy(out=o[0:m, :], in_=bank[0:m, :]), r=[bank.b], w=[o.b])
                            S.dma("sp", ZR[cc - 2 * DG:cc - 2 * DG + m, tok], o[0:m, :], r=[o.b])
                    gemm(st, "A", panels, NFT, XT, TB, epi, PS, 2)
                    S.barrier()

        def phase_B1(l):
            with contextlib.ExitStack() as st:
                Gb = sb(st, "B1_g", [128, DG]); Bb = sb(st, "B1_b", [128, DG]); BSb = sb(st, "B1_bs", [128, NHG * 128])
                WST = sb(st, "B1_wst", [128, NHG * 128], BF16)
                S.dma("sp", Gb[:], I["sgu_ln_g"][l:l + 1, :].partition_broadcast(128), w=[Gb.b])
                S.dma("sp", Bb[:], I["sgu_ln_b"][l:l + 1, :].partition_broadcast(128), w=[Bb.b])
                S.dma("sp", BSb[:], I["b_s"][l:l + 1, :].partition_broadcast(128), w=[BSb.b])
                S.dma("pool", WST[:], I["w_sT"][l * 128:(l + 1) * 128, :], w=[WST.b])
                nb = (NHG + 3) // 4
                VGc_r = ring(st, "B1_vg", 2, [128, NHG, 128], F32)
                UGc_r = ring(st, "B1_ug", 2, [128, NHG, 128], BF16)
                VN = sb(st, "B1_vn", [128, DG]); VNB = sb(st, "B1_vnb", [128, DG], BF16)
                STt = sb(st, "B1_st", [128, nb, 6]); MV = sb(st, "B1_mv", [128, 2]); RS = sb(st, "B1_rs", [128, 1])
                TMP = sb(st, "B1_tmp", [128, NHG * 128]); YG_r = ring(st, "B1_yg", 2, [128, NHG, 128], BF16)
                for ch in range(NT // 128):
                    tok = slice(ch * 128, ch * 128 + 128)
                    VGc = VGc_r(); UGc = UGc_r(); YG = YG_r()
                    S.dma("sp", VGc[:], VG.rearrange("(h p) t -> p h t", p=128)[:, :, tok], w=[VGc.b])
                    S.dma("sp", UGc[:], UG.rearrange("(h p) t -> p h t", p=128)[:, :, tok], w=[UGc.b])
                    for h in range(NHG):
                        bank = PS[h // 4]
                        S.pe(lambda e, h=h, bank=bank: e.matmul(bank[:, (h % 4) * 128:(h % 4) * 128 + 128], lhsT=VGc[:, h, :], rhs=ident_f[:],
                                                                start=True, stop=True), r=[VGc.b, ident_f.b], w=[bank.b],
                             inc=(h % 4 == 3 or h == NHG - 1))
                    for b in range(nb):
                        w_ = min(512, DG - b * 512)
                        S.dve(lambda e, b=b, w_=w_: e.bn_stats(out=STt[:, b, :], in_=PS[b][:, 0:w_]), r=[PS[b].b], w=[STt.b])
                    S.dve(lambda e: e.bn_aggr(out=MV[:], in_=STt[:]), r=[STt.b], w=[MV.b])
                    S.act(lambda e: e.activation(out=RS[:], in_=MV[:, 1:2], func=AF.Sqrt, bias=c.LN_EPS, scale=1.0), r=[MV.b], w=[RS.b])
                    S.dve(lambda e: e.reciprocal(out=RS[:], in_=RS[:]), r=[RS.b], w=[RS.b])
                    for b in range(nb):
                        w_ = min(512, DG - b * 512)
                        S.dve(lambda e, b=b, w_=w_: e.tensor_scalar(out=VN[:, b * 512:b * 512 + w_], in0=PS[b][:, 0:w_], scalar1=MV[:, 0:1],
                                                                    scalar2=RS[:, 0:1], op0=ALU.subtract, op1=ALU.mult),
                              r=[PS[b].b, MV.b, RS.b], w=[VN.b])
                    S.pool(lambda e: e.tensor_tensor(out=VN[:], in0=VN[:], in1=Gb[:], op=ALU.mult), r=[VN.b, Gb.b], w=[VN.b])
                    S.pool(lambda e: e.tensor_tensor(out=VNB[:], in0=VN[:], in1=Bb[:], op=ALU.add), r=[VN.b, Bb.b], w=[VNB.b])
                    for h in range(NHG):
                        bank = PS[4 + h // 4]
                        S.pe(lambda e, h=h, bank=bank: e.matmul(bank[:, (h % 4) * 128:(h % 4) * 128 + 128], lhsT=VNB[:, h * 128:(h + 1) * 128],
                                                                rhs=WST[:, h * 128:(h + 1) * 128], start=True, stop=True),
                             r=[VNB.b, WST.b], w=[bank.b], inc=(h % 4 == 3 or h == NHG - 1))
                    for b in range(nb):
                        w_ = min(512, DG - b * 512)
                        S.dve(lambda e, b=b, w_=w_: e.tensor_tensor(out=TMP[:, b * 512:b * 512 + w_], in0=PS[4 + b][:, 0:w_],
                                                                    in1=BSb[:, b * 512:b * 512 + w_], op=ALU.add),
                              r=[PS[4 + b].b, BSb.b], w=[TMP.b])
                    S.pool(lambda e: e.tensor_tensor(out=YG[:].rearrange("p h t -> p (h t)"), in0=TMP[:],
                                                     in1=UGc[:].rearrange("p h t -> p (h t)"), op=ALU.mult), r=[TMP.b, UGc.b], w=[YG.b])
                    S.dma("sp", MIX[0:DG, :].rearrange("(h p) t -> p h t", p=128)[:, :, tok], YG[:], r=[YG.b])
                S.barrier()

        TN = 512

        def phase_B2(l):
            nchb = TN // 64
            with contextlib.ExitStack() as st:
                W2 = [sb(st, f"B2_w2{d}", [96, DR], BF16) for d in range(2)]
                A2 = [sb(st, f"B2_a2{d}", [96, DR], BF16) for d in range(2)]
                G2 = sb(st, "B2_g2", [128, 2, DR], BF16)
                for d in range(2):
                    S.dma("pool", W2[d][:], I["w2"][(l * 2 + d) * 96:(l * 2 + d + 1) * 96, :], w=[W2[d].b])
                    S.dma("pool", A2[d][:], I["a2"][(l * 2 + d) * 96:(l * 2 + d + 1) * 96, :], w=[A2[d].b])
                S.dma("pool", G2[:], I["g2"][l * 256:(l + 1) * 256, :].rearrange("(k p) n -> p k n", p=128), w=[G2.b])
                if l > 0:
                    V2 = sb(st, "B2_v2", [64, DR], BF16)
                    S.dma("pool", V2[:], I["v2"][(l - 1) * 64:l * 64, :], w=[V2.b])

                def fsb(name, dt=F32):
                    return sb(st, "B2_" + name, [128, TN], dt)
                TW = [fsb("tw0", BF16), fsb("tw1", BF16)]; AD = [fsb("ad0", BF16), fsb("ad1", BF16)]
                SG = [fsb("sg0", BF16), fsb("sg1", BF16)]; MVt = fsb("mv", BF16)
                OUTS = [ring(st, f"B2_o{i}", 2, [128, TN], BF16) for i in range(6)]
                psb = [0]

                def nbank():
                    b = PS[psb[0] % 8]
                    psb[0] += 1
                    return b

                class TS:
                    pass
                SETS = []
                for si in range(2):
                    t = TS()
                    for nm in ("R_", "K_", "V_", "KK", "EW", "A_", "T1", "KD", "B_", "E_", "DM", "F_", "G_", "XA", "XB_", "XC", "XD", "TMP", "E1"):
                        setattr(t, nm, fsb(f"{nm}{si}"))
                    t.VBt = fsb(f"vb{si}", BF16)
                    t.WLt = sb(st, f"B2_wl{si}", [128, nchb])
                    t.Zr = ring(st, f"B2_z{si}", 3, [128, TN + 2], F32)
                    SETS.append(t)

                def load_z(Zring, nrows, row0, t0):
                    Z = Zring()
                    lo = t0 - 1
                    hi = t0 + TN + 1
                    c_lo, c_hi = 0, TN + 2
                    if t0 == 0:
                        S.dve(lambda e: e.memset(Z[0:nrows, 0:1], 0.0), w=[Z.b])
                        lo, c_lo = 0, 1
                    if t0 + TN == NT:
                        S.dve(lambda e: e.memset(Z[0:nrows, TN + 1:TN + 2], 0.0), w=[Z.b])
                        hi, c_hi = NT, TN + 1
                    S.dma("sp", Z[0:nrows, c_lo:c_hi], ZR[row0:row0 + nrows, lo:hi], w=[Z.b])
                    if t0 == SEG:
                        S.dve(lambda e: e.tensor_scalar(out=Z[0:nrows, 0:1], in0=Z[0:nrows, 0:1], scalar1=linkt[0:nrows, 0:1], scalar2=None,
                                                        op0=ALU.mult), r=[Z.b, linkt.b], w=[Z.b])
                    if t0 + TN == SEG:
                        S.dve(lambda e: e.tensor_scalar(out=Z[0:nrows, TN + 1:TN + 2], in0=Z[0:nrows, TN + 1:TN + 2], scalar1=linkt[0:nrows, 0:1],
                                                        scalar2=None, op0=ALU.mult), r=[Z.b, linkt.b], w=[Z.b])
                    return Z

                def conv3(eng, Z, dst, TMP, nrows, cvkey):
                    w = [colt[0:nrows, c.cols[cvkey + (j,)]:c.cols[cvkey + (j,)] + 1] for j in range(3)]
                    eng(lambda e: e.tensor_scalar(out=TMP[0:nrows, :], in0=Z[0:nrows, 0:TN], scalar1=w[0], scalar2=None, op0=ALU.mult),
                        r=[Z.b, colt.b], w=[TMP.b])
                    S.dve(lambda e: e.scalar_tensor_tensor(out=TMP[0:nrows, :], in0=Z[0:nrows, 1:TN + 1], scalar=w[1], in1=TMP[0:nrows, :],
                                                           op0=ALU.mult, op1=ALU.add), r=[Z.b, colt.b, TMP.b], w=[TMP.b])
                    S.dve(lambda e: e.scalar_tensor_tensor(out=dst[0:nrows, :], in0=Z[0:nrows, 2:TN + 2], scalar=w[2], in1=TMP[0:nrows, :],
                                                           op0=ALU.mult, op1=ALU.add), r=[Z.b, colt.b, TMP.b], w=[dst.b])

                def pair_gen(p, t0, T):
                    tok = slice(t0, t0 + TN)
                    rows = slice(128 * p, 128 * p + 128)
                    R_, K_, V_, KK, EW, A_, T1, KD, B_, E_, DM, F_, G_, XA, XB_, XC, XD, TMP, E1 = (
                        T.R_, T.K_, T.V_, T.KK, T.EW, T.A_, T.T1, T.KD, T.B_, T.E_, T.DM, T.F_, T.G_, T.XA, T.XB_, T.XC, T.XD, T.TMP, T.E1)
                    Zr_ = load_z(T.Zr, 128, 128 * p, t0)
                    Zk_ = load_z(T.Zr, 128, DR + 128 * p, t0)
                    Zv_ = load_z(T.Zr, 128, 2 * DR + 128 * p, t0)
                    if l > 0:
                        S.dma("sp", XD[:], VF[rows, tok], w=[XD.b])
                    yield
                    conv3(S.dve, Zr_, R_, TMP, 128, ("cv_r", p))
                    conv3(S.dve, Zk_, K_, TMP, 128, ("cv_k", p))
                    yield
                    conv3(S.dve, Zv_, V_, TMP, 128, ("cv_v", p))
                    if l == 0:
                        S.dma("sp", VF[rows, tok], V_[:], r=[V_.b])
                    else:
                        bk = nbank()
                        S.pe(lambda e: e.matmul(bk[:, :], lhsT=V2[:, rows], rhs=MVt[0:64, :], start=True, stop=True), r=[V2.b, MVt.b], w=[bk.b])
                        S.act(lambda e: e.activation(out=XC[:], in_=bk[:, :], func=AF.Exp, bias=negv0[:, p:p + 1], scale=-1.0), r=[bk.b, negv0.b], w=[XC.b])
                        S.act(lambda e: e.activation(out=XC[:], in_=XC[:], func=AF.Ln, bias=1.0, scale=1.0), r=[XC.b], w=[XC.b])
                        S.act(lambda e: e.activation(out=XC[:], in_=XC[:], func=AF.Exp, scale=-1.0), r=[XC.b], w=[XC.b])
                        S.dve(lambda e: e.tensor_tensor(out=XD[:], in0=XD[:], in1=V_[:], op=ALU.subtract), r=[XD.b, V_.b], w=[XD.b])
                        S.dve(lambda e: e.tensor_tensor(out=XD[:], in0=XD[:], in1=XC[:], op=ALU.mult), r=[XD.b, XC.b], w=[XD.b])
                        S.dve(lambda e: e.tensor_tensor(out=V_[:], in0=V_[:], in1=XD[:], op=ALU.add), r=[V_.b, XD.b], w=[V_.b])
                    S.act(lambda e: e.copy(out=T.VBt[:], in_=V_[:]), r=[V_.b], w=[T.VBt.b])
                    S.dma("sp", VB[rows, tok], T.VBt[:], r=[T.VBt.b])
                    yield
                    bk = nbank()
                    for k2 in range(2):
                        S.pe(lambda e, k2=k2: e.matmul(bk[:, :], lhsT=G2[:, k2, rows], rhs=SG[k2][:], start=(k2 == 0), stop=(k2 == 1)),
                             r=[G2.b, SG[k2].b], w=[bk.b], inc=(k2 == 1))
                    S.dve(lambda e: e.tensor_copy(out=F_[:], in_=bk[:, :]), r=[bk.b], w=[F_.b])
                    S.dma("sp", GG[rows, tok], F_[:], r=[F_.b])
                    S.dve(lambda e: e.tensor_scalar(out=KK[:], in0=K_[:], scalar1=col(("k_k", p)), scalar2=None, op0=ALU.mult),
                          r=[K_.b, colt.b], w=[KK.b])
                    S.dve(lambda e: e.tensor_tensor(out=XA[:], in0=KK[:], in1=KK[:], op=ALU.mult), r=[KK.b], w=[XA.b])
                    bk = nbank()
                    S.pe(lambda e: e.matmul(bk[:, :], lhsT=onesbd[:], rhs=XA[:], start=True, stop=True), r=[onesbd.b, XA.b], w=[bk.b])
                    S.act(lambda e: e.activation(out=XB_[:], in_=bk[:, :], func=AF.Ln, bias=c.L2_EPS, scale=1.0), r=[bk.b], w=[XB_.b])
                    S.act(lambda e: e.activation(out=XB_[:], in_=XB_[:], func=AF.Exp, scale=-0.5), r=[XB_.b], w=[XB_.b])
                    S.dve(lambda e: e.tensor_tensor(out=KK[:], in0=KK[:], in1=XB_[:], op=ALU.mult), r=[KK.b, XB_.b], w=[KK.b])
                    yield
                    for d in range(2):
                        bk = nbank()
                        S.pe(lambda e, d=d: e.matmul(bk[:, :], lhsT=W2[d][:, rows], rhs=TW[d][0:96, :], start=True, stop=True),
                             r=[W2[d].b, TW[d].b], w=[bk.b])
                        S.act(lambda e, d=d: e.activation(out=E1[:], in_=bk[:, :], func=AF.Exp, bias=negw0[:, d * P + p:d * P + p + 1], scale=-1.0),
                              r=[bk.b, negw0.b], w=[E1.b])
                        S.act(lambda e: e.activation(out=E1[:], in_=E1[:], func=AF.Ln, bias=1.0, scale=1.0), r=[E1.b], w=[E1.b])
                        S.act(lambda e: e.activation(out=EW[:], in_=E1[:], func=AF.Exp, bias=-0.5, scale=-1.0), r=[E1.b], w=[EW.b])
                        bk = nbank()
                        S.pe(lambda e, d=d: e.matmul(bk[:, :], lhsT=A2[d][:, rows], rhs=AD[d][0:96, :], start=True, stop=True),
                             r=[A2[d].b, AD[d].b], w=[bk.b])
                        S.act(lambda e, d=d: e.activation(out=A_[:], in_=bk[:, :], func=AF.Exp, bias=nega0[:, d * P + p:d * P + p + 1], scale=-1.0),
                              r=[bk.b, nega0.b], w=[A_.b])
                        S.act(lambda e: e.activation(out=A_[:], in_=A_[:], func=AF.Ln, bias=1.0, scale=1.0), r=[A_.b], w=[A_.b])
                        S.act(lambda e: e.activation(out=A_[:], in_=A_[:], func=AF.Exp, scale=-1.0), r=[A_.b], w=[A_.b])
                        yield
                        S.dve(lambda e: e.tensor_scalar(out=T1[:], in0=A_[:], scalar1=col(("k_a", p)), scalar2=omka[:, p:p + 1],
                                                        op0=ALU.mult, op1=ALU.add), r=[A_.b, colt.b, omka.b], w=[T1.b])
                        S.dve(lambda e: e.tensor_tensor(out=KD[:], in0=K_[:], in1=T1[:], op=ALU.mult), r=[K_.b, T1.b], w=[KD.b])
                        S.dve(lambda e: e.tensor_tensor(out=B_[:], in0=KK[:], in1=A_[:], op=ALU.mult), r=[KK.b, A_.b], w=[B_.b])
                        if d == 0:
                            S.dve(lambda e: e.scalar_tensor_tensor(out=T1[:], in0=R_[:], scalar=col(("r_k", p)), in1=KD[:],
                                                                   op0=ALU.mult, op1=ALU.mult), r=[R_.b, colt.b, KD.b], w=[T1.b])
                            bk = nbank()
                            S.pe(lambda e: e.matmul(bk[:, :], lhsT=onesbd[:], rhs=T1[:], start=True, stop=True), r=[onesbd.b, T1.b], w=[bk.b])
                            S.dve(lambda e: e.tensor_tensor(out=G_[:], in0=bk[:, :], in1=V_[:], op=ALU.mult), r=[bk.b, V_.b], w=[G_.b])
                            S.dma("sp", BON[rows, tok], G_[:], r=[G_.b])
                        S.dve(lambda e: e.tensor_tensor_scan(out=E_[:], data0=M01[:, 0:TN], data1=EW[:], initial=0.0, op0=ALU.mult, op1=ALU.add),
                              r=[M01.b, EW.b], w=[E_.b])
                        E3 = E_[:].rearrange("p (c t) -> p c t", t=64)
                        S.dve(lambda e: e.tensor_tensor(out=DM[:].rearrange("p (c t) -> p c t", t=64), in0=E3,
                                                        in1=E3[:, :, 63:64].to_broadcast([128, nchb, 64]), op=ALU.subtract), r=[E_.b], w=[DM.b])
                        S.dve(lambda e: e.tensor_tensor(out=F_[:], in0=EW[:], in1=E_[:], op=ALU.subtract), r=[EW.b, E_.b], w=[F_.b])
                        yield
                        S.act(lambda e: e.activation(out=T.WLt[:].rearrange("p (c o) -> p c o", o=1), in_=E3[:, :, 63:64], func=AF.Exp, scale=-1.0),
                              r=[E_.b], w=[T.WLt.b])
                        S.dma("sp", SC[("WL", d)][rows, t0 // 64:t0 // 64 + nchb], T.WLt[:], r=[T.WLt.b])
                        S.act(lambda e: e.activation(out=XB_[:], in_=F_[:], func=AF.Exp), r=[F_.b], w=[XB_.b])
                        S.act(lambda e: e.activation(out=XD[:], in_=DM[:], func=AF.Exp), r=[DM.b], w=[XD.b])
                        if d == 0:
                            S.act(lambda e: e.activation(out=XA[:], in_=E_[:], func=AF.Exp, scale=-1.0), r=[E_.b], w=[XA.b])
                            S.act(lambda e: e.activation(out=XC[:], in_=E_[:], func=AF.Exp), r=[E_.b], w=[XC.b])
                            prods = [("RT", R_, XA, 1), ("KKT", KK, XB_, 1), ("KH", KD, XC, 1), ("BH", B_, XC, 1),
                                     ("KEND", KD, XD, 1), ("NBEND", B_, XD, -1)]
                        else:
                            S.dve(lambda e: e.tensor_tensor(out=G_[:], in0=EW[:], in1=DM[:], op=ALU.subtract), r=[EW.b, DM.b], w=[G_.b])
                            S.act(lambda e: e.activation(out=XC[:], in_=G_[:], func=AF.Exp), r=[G_.b], w=[XC.b])
                            prods = [("RT", R_, XD, 1), ("KKT", KK, XD, 1), ("KH", KD, XC, 1), ("BH", B_, XC, 1),
                                     ("KEND", KD, XB_, 1), ("NBEND", B_, XB_, -1)]
                        yield
                        for i_, (nm, a_, x_, sgn) in enumerate(prods):
                            o = OUTS[i_]()
                            eng = S.dve
                            if sgn > 0:
                                eng(lambda e, o=o, a_=a_, x_=x_: e.tensor_tensor(out=o[:], in0=a_[:], in1=x_[:], op=ALU.mult),
                                    r=[a_.b, x_.b], w=[o.b])
                            else:
                                eng(lambda e, o=o, a_=a_, x_=x_: e.scalar_tensor_tensor(out=o[:], in0=a_[:], scalar=-1.0, in1=x_[:],
                                                                                        op0=ALU.mult, op1=ALU.mult), r=[a_.b, x_.b], w=[o.b])
                            S.dma("sp", SC[(nm, d)][rows, tok], o[:], r=[o.b])
                        yield

                E1g = SETS[0].E1
                TMPg = SETS[0].TMP
                Zg = SETS[0].Zr
                for t0 in range(0, NT, TN):
                    base = 3 * DR
                    for d, nm in enumerate(("cv_wdf", "cv_wdb")):
                        Z = load_z(Zg, 96, base + 96 * d, t0)
                        conv3(S.dve, Z, E1g, TMPg, 96, (nm,))
                        S.act(lambda e, d=d: e.activation(out=TW[d][0:96, :], in_=E1g[0:96, :], func=AF.Tanh), r=[E1g.b], w=[TW[d].b])
                    for k2, nm in enumerate(("cv_gd0", "cv_gd1")):
                        Z = load_z(Zg, 128, base + 384 + 128 * k2, t0)
                        conv3(S.dve, Z, E1g, TMPg, 128, (nm,))
                        S.act(lambda e, k2=k2: e.activation(out=SG[k2][:], in_=E1g[:], func=AF.Sigmoid), r=[E1g.b], w=[SG[k2].b])
                    for d, nm in enumerate(("cv_adf", "cv_adb")):
                        Z = load_z(Zg, 96, base + 192 + 96 * d, t0)
                        conv3(S.dve, Z, E1g, TMPg, 96, (nm,))
                        S.dve(lambda e, d=d: e.tensor_copy(out=AD[d][0:96, :], in_=E1g[0:96, :]), r=[E1g.b], w=[AD[d].b])
                    if l > 0:
                        Z = load_z(Zg, 64, base + 640, t0)
                        conv3(S.dve, Z, E1g, TMPg, 64, ("cv_mv",))
                        S.dve(lambda e: e.tensor_copy(out=MVt[0:64, :], in_=E1g[0:64, :]), r=[E1g.b], w=[MVt.b])
                    for p0 in range(0, P, 2):
                        interleave([pair_gen(p, t0, SETS[p - p0]) for p in range(p0, min(P, p0 + 2))])
                S.barrier()

        PB = c.PB if getattr(c, "PB", None) else min(4, P)
        GC = 2
        OPN = ("KKT", "RT", "BH", "KH", "KEND", "NBEND", "V")

        def interleave(gens):
            gens = list(gens)
            while gens:
                for g_ in list(gens):
                    try:
                        next(g_)
                    except StopIteration:
                        gens.remove(g_)

        def convert_weights(l):
            for (src, dst) in WCONV[l]:
                S.dma("pool", dst, src)

        def phase_B3(l):
            with contextlib.ExitStack() as st:
                MR = {}
                for d in range(2):
                    for k_ in ("X1", "X1t", "NAybT", "AukT", "AykT"):
                        src = MASK[(k_, d)]
                        key = src.b.name
                        if key not in MR:
                            t_ = sb(st, "B3_m_" + key, [128, PB, 128])
                            for q in range(PB):
                                S.pool(lambda e, q=q, t_=t_, src=src: e.tensor_copy(out=t_[:, q, :], in_=src[:]), r=[src.b], w=[t_.b])
                            MR[key] = t_
                IDR = sb(st, "B3_idr", [128, PB, 128], BF16)
                for q in range(PB):
                    S.pool(lambda e, q=q: e.tensor_copy(out=IDR[:, q, :], in_=ident_b[:]), r=[ident_b.b], w=[IDR.b])
                WLa = sb(st, "B3_wl", [128, P, NCH])
                bank_i = [0]
                evac_i = [0]

                def nbank():
                    b = PS[bank_i[0] % 8]
                    bank_i[0] += 1
                    return b

                def stage(nq, mms):
                    bank = nbank()
                    for q in range(nq):
                        lst = mms(q)
                        for i, (l_, r_, bufs) in enumerate(lst):
                            S.pe(lambda e, l_=l_, r_=r_, q=q, i=i, n=len(lst): e.matmul(bank[:, q * 128:(q + 1) * 128], lhsT=l_, rhs=r_,
                                                                                     start=(i == 0), stop=(i == n - 1)),
                                 r=bufs, w=[bank.b], inc=(q == nq - 1 and i == len(lst) - 1))
                    return bank

                def evac_copy(bank, dst, nq):
                    evac_i[0] += 1
                    o = dst[:, 0:nq, :].rearrange("p q t -> p (q t)")
                    if evac_i[0] % 3 == 0:
                        S.dve(lambda e: e.tensor_copy(out=o, in_=bank[:, 0:nq * 128]), r=[bank.b], w=[dst.b])
                    else:
                        S.act(lambda e: e.copy(out=o, in_=bank[:, 0:nq * 128]), r=[bank.b], w=[dst.b])

                def evac_mask(bank, dst, nq, mask):
                    o = dst[:, 0:nq, :].rearrange("p q t -> p (q t)")
                    m_ = mask[:, 0:nq, :].rearrange("p q t -> p (q t)")
                    S.dve(lambda e: e.tensor_tensor(out=o, in0=bank[:, 0:nq * 128], in1=m_, op=ALU.mult), r=[bank.b, mask.b], w=[dst.b])

                class Res:
                    pass
                RES = []
                for ri in range(2):
                    R_ = Res()
                    R_.OPS = []
                    for i in range(2):
                        dct = {}
                        for nm in OPN:
                            t_ = sb(st, f"B3_op{ri}{i}_{nm}", [128, PB, GC, 128], BF16)
                            S.pool(lambda e, t_=t_: e.memset(t_[:], 0.0), w=[t_.b])
                            dct[nm] = t_
                        R_.OPS.append(dct)

                    def tb(name, dt=BF16, ri=ri):
                        return sb(st, f"B3_{name}{ri}", [128, PB, 128], dt)
                    R_.X = [tb("x0"), tb("x1")]; R_.Xt = [tb("xt0"), tb("xt1")]; R_.Tt = [tb("tt0"), tb("tt1")]
                    R_.KKTT = tb("kktt"); R_.KENDT = tb("kendt"); R_.NBENDT = tb("nbendt"); R_.VT = tb("vt")
                    R_.NAybT = tb("naybt"); R_.AukT = tb("aukt"); R_.AykT = tb("aykt"); R_.KTT = tb("ktt"); R_.AV = tb("av"); R_.U = tb("u")
                    R_.Hf = tb("hf", F32); R_.Hb = tb("hb")
                    R_.Ys = [tb("ys0", F32), tb("ys1", F32)]
                    RES.append(R_)

                def batch_gen(d, p0, nq, R_):
                    X, Xt, Tt = R_.X, R_.Xt, R_.Tt
                    KKTT, KENDT, NBENDT, VT = R_.KKTT, R_.KENDT, R_.NBENDT, R_.VT
                    NAybT, AukT, AykT, KTT, AV, U, Hf, Hb, Ys, OPS = R_.NAybT, R_.AukT, R_.AykT, R_.KTT, R_.AV, R_.U, R_.Hf, R_.Hb, R_.Ys, R_.OPS
                    Yd = SC[("Y", d)]
                    ngroups = NCH // GC
                    gorder = list(range(ngroups)) if d == 0 else list(range(ngroups - 1, -1, -1))

                    def load_ops(gi, slot):
                        g = gorder[gi]
                        for nm in OPN:
                            src = VB if nm == "V" else SC[(nm, d)]
                            t_ = OPS[slot][nm]
                            for q in range(nq):
                                for h in range(2):
                                    r0 = 128 * (p0 + q) + 64 * h
                                    S.dma("sp", t_[64 * h:64 * h + 64, q, :, 64 * h:64 * h + 64],
                                          src[r0:r0 + 64, g * GC * 64:(g + 1) * GC * 64].rearrange("p (c t) -> p c t", t=64), w=[t_.b])
                    load_ops(0, 0)
                    S.dve(lambda e: e.memset(Hf[:], 0.0), w=[Hf.b])
                    S.dve(lambda e: e.memset(Hb[:], 0.0), w=[Hb.b])
                    yield
                    for gi in range(ngroups):
                        if gi + 1 < ngroups:
                            load_ops(gi + 1, (gi + 1) % 2)
                        O = OPS[gi % 2]
                        g = gorder[gi]
                        corder = list(range(GC)) if d == 0 else list(range(GC - 1, -1, -1))
                        for cl in corder:
                            ch = g * GC + cl
                            if (d == 0 and ch * 64 == SEG) or (d == 1 and (ch + 1) * 64 == SEG):
                                S.dve(lambda e: e.tensor_scalar(out=Hf[:].rearrange("p q t -> p (q t)"), in0=Hf[:].rearrange("p q t -> p (q t)"),
                                                                scalar1=linkt[:, 0:1], scalar2=None, op0=ALU.mult), r=[Hf.b, linkt.b], w=[Hf.b])
                                S.act(lambda e: e.copy(out=Hb[:].rearrange("p q t -> p (q t)"), in_=Hf[:].rearrange("p q t -> p (q t)")),
                                      r=[Hf.b], w=[Hb.b])

                            def op(nm, q):
                                return O[nm][:, q, cl, :]
                            mk = lambda k_: MR[MASK[(k_, d)].b.name]
                            bk = stage(nq, lambda q: [(op("KKT", q), op("BH", q), [O["KKT"].b, O["BH"].b])])
                            evac_mask(bk, X[0], nq, mk("X1"))
                            bk = stage(nq, lambda q: [(op("BH", q), op("KKT", q), [O["KKT"].b, O["BH"].b])])
                            evac_mask(bk, Xt[0], nq, mk("X1t"))
                            yield
                            bk = stage(nq, lambda q: [(op("BH", q), op("RT", q), [O["RT"].b, O["BH"].b])])
                            evac_mask(bk, NAybT, nq, mk("NAybT"))
                            bk = stage(nq, lambda q: [(op("KH", q), op("KKT", q), [O["KKT"].b, O["KH"].b])])
                            evac_mask(bk, AukT, nq, mk("AukT"))
                            yield
                            bk = stage(nq, lambda q: [(op("KH", q), op("RT", q), [O["RT"].b, O["KH"].b])])
                            evac_mask(bk, AykT, nq, mk("AykT"))
                            for (nm, dst) in (("KKT", KKTT), ("KEND", KENDT), ("NBEND", NBENDT), ("V", VT)):
                                bk = stage(nq, lambda q, nm=nm: [(op(nm, q), ident_b[:], [O[nm].b, ident_b.b])])
                                evac_copy(bk, dst, nq)
                                yield
                            S.dve(lambda e: e.tensor_tensor(out=Tt[0][:, 0:nq, :], in0=Xt[0][:, 0:nq, :], in1=IDR[:, 0:nq, :], op=ALU.add),
                                  r=[Xt[0].b, IDR.b], w=[Tt[0].b])
                            cur = 0
                            for k in range(1, 6):
                                nx = 1 - cur
                                bk = stage(nq, lambda q: [(Xt[cur][:, q, :], X[cur][:, q, :], [Xt[cur].b, X[cur].b])])
                                evac_copy(bk, X[nx], nq)
                                if k < 5:
                                    bk = stage(nq, lambda q: [(X[cur][:, q, :], Xt[cur][:, q, :], [Xt[cur].b, X[cur].b])])
                                    evac_copy(bk, Xt[nx], nq)
                                yield
                                tcur = (k - 1) % 2
                                bk = stage(nq, lambda q: [(X[nx][:, q, :], Tt[tcur][:, q, :], [X[nx].b, Tt[tcur].b])])
                                S.dve(lambda e, bk=bk, tcur=tcur: e.tensor_tensor(out=Tt[1 - tcur][:, 0:nq, :].rearrange("p q t -> p (q t)"),
                                                                                  in0=bk[:, 0:nq * 128],
                                                                                  in1=Tt[tcur][:, 0:nq, :].rearrange("p q t -> p (q t)"), op=ALU.add),
                                      r=[bk.b, Tt[tcur].b], w=[Tt[1 - tcur].b])
                                cur = nx
                                yield
                            TT = Tt[1]
                            bk = stage(nq, lambda q: [(KKTT[:, q, :], TT[:, q, :], [KKTT.b, TT.b])])
                            evac_copy(bk, KTT, nq)
                            bk = stage(nq, lambda q: [(AukT[:, q, :], VT[:, q, :], [AukT.b, VT.b])])
                            evac_copy(bk, AV, nq)
                            yield
                            bk = stage(nq, lambda q: [(TT[:, q, :], AV[:, q, :], [TT.b, AV.b]), (KTT[:, q, :], Hb[:, q, :], [KTT.b, Hb.b])])
                            evac_copy(bk, U, nq)
                            yield
                            bk = stage(nq, lambda q: [(Hb[:, q, :], op("RT", q), [Hb.b, O["RT"].b]), (VT[:, q, :], AykT[:, q, :], [VT.b, AykT.b]),
                                                      (U[:, q, :], NAybT[:, q, :], [U.b, NAybT.b])])
                            Y_ = Ys[ch % 2]
                            evac_copy(bk, Y_, nq)
                            for h in range(2):
                                S.dma("sp", Yd.rearrange("(pp r) t -> r pp t", r=128)[64 * h:64 * h + 64, p0:p0 + nq, ch * 64:ch * 64 + 64],
                                      Y_[64 * h:64 * h + 64, 0:nq, 64 * h:64 * h + 64], r=[Y_.b])
                            bk = stage(nq, lambda q: [(KENDT[:, q, :], VT[:, q, :], [KENDT.b, VT.b]), (NBENDT[:, q, :], U[:, q, :], [NBENDT.b, U.b])])
                            for q in range(nq):
                                S.dve(lambda e, q=q, bk=bk: e.scalar_tensor_tensor(out=Hf[:, q, :], in0=Hf[:, q, :], scalar=WLa[:, p0 + q, ch:ch + 1],
                                                                                   in1=bk[:, q * 128:(q + 1) * 128], op0=ALU.mult, op1=ALU.add),
                                      r=[Hf.b, WLa.b, bk.b], w=[Hf.b])
                            S.act(lambda e: e.copy(out=Hb[:, 0:nq, :].rearrange("p q t -> p (q t)"), in_=Hf[:, 0:nq, :].rearrange("p q t -> p (q t)")),
                                  r=[Hf.b], w=[Hb.b])
                            yield

                convert_weights(l)
                for d in range(2):
                    S.dma("sp", WLa[:], SC[("WL", d)].rearrange("(pp p) c -> p pp c", p=128), w=[WLa.b])
                    batches = list(range(0, P, PB))
                    for bi in range(0, len(batches), 2):
                        gens = []
                        for k_, p0 in enumerate(batches[bi:bi + 2]):
                            gens.append(batch_gen(d, p0, min(PB, P - p0), RES[k_]))
                        interleave(gens)
                S.barrier()

        def phase_B4(l):
            with contextlib.ExitStack() as st:
                def fsb(name, n=2, dt=F32):
                    return ring(st, "B4_" + name, n, [128, TN], dt)
                YA = fsb("ya"); YB_ = fsb("yb"); SQ = fsb("sq", 1); MEANt = fsb("mean", 1); MSQ = fsb("msq", 1); VAR = fsb("var", 1)
                BONt = fsb("bon"); GGt = fsb("gg"); OUT = fsb("out", 2, BF16)
                for t0 in range(0, NT, TN):
                    tok = slice(t0, t0 + TN)
                    for p in range(P):
                        rows = slice(128 * p, 128 * p + 128)
                        ya = YA(); yb = YB_(); sq = SQ(); mean = MEANt(); msq = MSQ(); var = VAR(); bon = BONt(); gg = GGt(); out = OUT()
                        S.dma("sp", ya[:], SC[("Y", 0)][rows, tok], w=[ya.b])
                        S.dma("sp", yb[:], SC[("Y", 1)][rows, tok], w=[yb.b])
                        S.dma("sp", bon[:], BON[rows, tok], w=[bon.b])
                        S.dma("sp", gg[:], GG[rows, tok], w=[gg.b])
                        S.pool(lambda e: e.tensor_tensor(out=ya[:], in0=ya[:], in1=yb[:], op=ALU.add), r=[ya.b, yb.b], w=[ya.b])
                        S.act(lambda e: e.activation(out=sq[:], in_=ya[:], func=AF.Square), r=[ya.b], w=[sq.b])
                        S.pe(lambda e: e.matmul(PS[0][:, :], lhsT=onesbd[:], rhs=ya[:], start=True, stop=True), r=[onesbd.b, ya.b], w=[PS[0].b])
                        S.pe(lambda e: e.matmul(PS[1][:, :], lhsT=onesbd[:], rhs=sq[:], start=True, stop=True), r=[onesbd.b, sq.b], w=[PS[1].b])
                        S.act(lambda e: e.activation(out=mean[:], in_=PS[0][:, :], func=AF.Copy, scale=1.0 / 64.0), r=[PS[0].b], w=[mean.b])
                        S.dve(lambda e: e.tensor_tensor(out=msq[:], in0=mean[:], in1=mean[:], op=ALU.mult), r=[mean.b], w=[msq.b])
                        S.dve(lambda e: e.scalar_tensor_tensor(out=var[:], in0=PS[1][:, :], scalar=1.0 / 64.0, in1=msq[:], op0=ALU.mult, op1=ALU.subtract),
                              r=[PS[1].b, msq.b], w=[var.b])
                        S.act(lambda e: e.activation(out=var[:], in_=var[:], func=AF.Sqrt, bias=c.GN_EPS, scale=1.0), r=[var.b], w=[var.b])
                        S.dve(lambda e: e.reciprocal(out=var[:], in_=var[:]), r=[var.b], w=[var.b])
                        S.dve(lambda e: e.tensor_tensor(out=ya[:], in0=ya[:], in1=mean[:], op=ALU.subtract), r=[ya.b, mean.b], w=[ya.b])
                        S.dve(lambda e: e.tensor_tensor(out=ya[:], in0=ya[:], in1=var[:], op=ALU.mult), r=[ya.b, var.b], w=[ya.b])
                        S.dve(lambda e: e.tensor_scalar(out=ya[:], in0=ya[:], scalar1=col(("gn_g", p)), scalar2=col(("gn_b", p)), op0=ALU.mult, op1=ALU.add),
                              r=[ya.b, colt.b], w=[ya.b])
                        S.pool(lambda e: e.tensor_tensor(out=ya[:], in0=ya[:], in1=bon[:], op=ALU.add), r=[ya.b, bon.b], w=[ya.b])
                        S.pool(lambda e: e.tensor_tensor(out=out[:], in0=ya[:], in1=gg[:], op=ALU.mult), r=[ya.b, gg.b], w=[out.b])
                        S.dma("sp", MIX[DG + 128 * p:DG + 128 * p + 128, tok], out[:], r=[out.b])
                S.barrier()

        def ln_stats(st, TBw):
            msq = sb(st, "ln_msq", [128, TBw])
            for tb in range(TBw // 512):
                cs = slice(tb * 512, tb * 512 + 512)
                S.pe(lambda e: e.matmul(PS[0][:, :], lhsT=ones_f[:], rhs=S1[:, cs], start=True, stop=True), r=[ones_f.b, S1.b], w=[PS[0].b])
                S.pe(lambda e: e.matmul(PS[1][:, :], lhsT=ones_f[:], rhs=S2[:, cs], start=True, stop=True), r=[ones_f.b, S2.b], w=[PS[1].b])
                S.act(lambda e: e.activation(out=MEAN[:, cs], in_=PS[0][:, :], func=AF.Copy, scale=1.0 / D), r=[PS[0].b], w=[MEAN.b])
                S.dve(lambda e: e.tensor_tensor(out=msq[:, cs], in0=MEAN[:, cs], in1=MEAN[:, cs], op=ALU.mult), r=[MEAN.b], w=[msq.b])
                S.dve(lambda e: e.scalar_tensor_tensor(out=RSTD[:, cs], in0=PS[1][:, :], scalar=1.0 / D, in1=msq[:, cs], op0=ALU.mult, op1=ALU.subtract),
                      r=[PS[1].b, msq.b], w=[RSTD.b])
                S.act(lambda e: e.activation(out=RSTD[:, cs], in_=RSTD[:, cs], func=AF.Sqrt, bias=c.LN_EPS, scale=1.0), r=[RSTD.b], w=[RSTD.b])
                S.dve(lambda e: e.reciprocal(out=RSTD[:, cs], in_=RSTD[:, cs]), r=[RSTD.b], w=[RSTD.b])

        def resid_epi(st, name, xsrc, hdst, t0, cs_off=0):
            xr = ring(st, name + "_x", 2, [128, 512]); hr = ring(st, name + "_h", 2, [128, 512]); sqr = ring(st, name + "_sq", 1, [128, 512])

            def epi(tag, c0, ot, m, tb, bank):
                cc = c0 + ot * 128
                tok = slice(t0 + tb * 512, t0 + tb * 512 + 512)
                cs = slice(cs_off + tb * 512, cs_off + tb * 512 + 512)
                x = xr(); h = hr(); sq = sqr()
                S.dma("sp", x[:], xsrc[cc:cc + 128, tok], w=[x.b])
                S.dve(lambda e: e.scalar_tensor_tensor(out=h[:], in0=x[:], scalar=float(c.ALPHA), in1=bank[:, :], op0=ALU.mult, op1=ALU.add),
                      r=[x.b, bank.b], w=[h.b])
                S.dma("sp", hdst[cc:cc + 128, tok], h[:], r=[h.b])
                S.act(lambda e: e.activation(out=sq[:], in_=h[:], func=AF.Square), r=[h.b], w=[sq.b])
                S.pool(lambda e: e.tensor_tensor(out=S1[:, cs], in0=S1[:, cs], in1=h[:], op=ALU.add), r=[S1.b, h.b], w=[S1.b])
                S.pool(lambda e: e.tensor_tensor(out=S2[:, cs], in0=S2[:, cs], in1=sq[:], op=ALU.add), r=[S2.b, sq.b], w=[S2.b])
            return epi

        def zero_stats():
            S.pool(lambda e: e.memset(S1[:], 0.0), w=[S1.b])
            S.pool(lambda e: e.memset(S2[:], 0.0), w=[S2.b])

        def ln_apply(st, name, hsrc, t0, TBw, gkey, bkey, fdst, XT):
            hr = ring(st, name + "_h", 2, [128, TBw]); xr = ring(st, name + "_xo", 2, [128, TBw])
            for ft in range(NFT):
                rows = slice(128 * ft, 128 * ft + 128)
                h = hr(); x = xr()
                S.dma("sp", h[:], hsrc[rows, t0:t0 + TBw], w=[h.b])
                S.dve(lambda e: e.tensor_tensor(out=h[:], in0=h[:], in1=MEAN[:, 0:TBw], op=ALU.subtract), r=[h.b, MEAN.b], w=[h.b])
                S.dve(lambda e: e.tensor_tensor(out=h[:], in0=h[:], in1=RSTD[:, 0:TBw], op=ALU.mult), r=[h.b, RSTD.b], w=[h.b])
                S.act(lambda e: e.activation(out=x[:], in_=h[:], func=AF.Identity, bias=col((bkey, ft)), scale=col((gkey, ft))),
                      r=[h.b, colt.b], w=[x.b])
                S.dma("sp", fdst[rows, t0:t0 + TBw], x[:], r=[x.b])
                S.pool(lambda e: e.tensor_copy(out=XT[:, ft, :], in_=x[:]), r=[x.b], w=[XT.b])

        def phase_C(l, t0):
            xsrc = I["xT"] if l == 0 else XF1
            last = (l == DEPTH - 1)
            j = l // 2
            moe = (l % 2 == 1)
            with contextlib.ExitStack() as st:
                XT = load_rhs(st, "C1_xt", MIX, NFT, t0, TB)
                zero_stats()
                epi = resid_epi(st, "C1", xsrc, H1, t0)
                wsrc = WB[("w_out", l)][0]
                gemm(st, "C1", [(wsrc, c0, 256, None) for c0 in range(0, D, 256)], NFT, XT, TB, epi, PS, 2)
                ln_stats(st, TB)
                S.barrier()
            with contextlib.ExitStack() as st:
                XT = sb(st, "D1_xt", [128, NFT, TB], BF16)
                with contextlib.ExitStack() as st2:
                    ln_apply(st2, "C2", H1, t0, TB, "ln1_g", "ln1_b", X1F, XT)
                    S.barrier()
                GT = None
                if moe:
                    GT = sb(st, "D1_gt", [128, NE, TB])
                    st_outer = st
                    st = st_outer.enter_context(contextlib.ExitStack())
                    RT_ = sb(st, "D1_rt", [128, NFT, NE]); XF_r = ring(st, "D1_xf", 1, [128, NFT, 128])
                    LG = sb(st, "D1_lg", [128, NE]); M1 = sb(st, "D1_m1", [128, 1]); M2 = sb(st, "D1_m2", [128, 1])
                    EQ1 = sb(st, "D1_eq1", [128, NE]); EQ2 = sb(st, "D1_eq2", [128, NE]); LG2 = sb(st, "D1_lg2", [128, NE])
                    G1 = sb(st, "D1_g1", [128, 1]); G2_ = sb(st, "D1_g2", [128, 1]); GA = sb(st, "D1_ga", [128, NE])
                    S.dma("sp", RT_[:], I["router"][j * D:(j + 1) * D, :].rearrange("(k p) e -> p k e", p=128), w=[RT_.b])
                    for tt in range(TB // 128):
                        xf = XF_r()
                        S.dma("sp", xf[:], X1F.rearrange("(k p) t -> p k t", p=128)[:, :, t0 + tt * 128:t0 + tt * 128 + 128], w=[xf.b])
                        for k in range(NFT):
                            S.pe(lambda e, k=k: e.matmul(PS[0][:, 0:NE], lhsT=xf[:, k, :], rhs=RT_[:, k, :], start=(k == 0), stop=(k == NFT - 1)),
                                 r=[xf.b, RT_.b], w=[PS[0].b], inc=(k == NFT - 1))
                        S.dve(lambda e: e.tensor_copy(out=LG[:], in_=PS[0][:, 0:NE]), r=[PS[0].b], w=[LG.b])
                        S.dve(lambda e: e.tensor_reduce(out=M1[:], in_=LG[:], axis=AX.X, op=ALU.max), r=[LG.b], w=[M1.b])
                        S.dve(lambda e: e.tensor_scalar(out=EQ1[:], in0=LG[:], scalar1=M1[:, 0:1], scalar2=None, op0=ALU.is_equal), r=[LG.b, M1.b], w=[EQ1.b])
                        S.dve(lambda e: e.scalar_tensor_tensor(out=LG2[:], in0=EQ1[:], scalar=-1.0e30, in1=LG[:], op0=ALU.mult, op1=ALU.add),
                              r=[EQ1.b, LG.b], w=[LG2.b])
                        S.dve(lambda e: e.tensor_reduce(out=M2[:], in_=LG2[:], axis=AX.X, op=ALU.max), r=[LG2.b], w=[M2.b])
                        S.dve(lambda e: e.tensor_scalar(out=EQ2[:], in0=LG2[:], scalar1=M2[:, 0:1], scalar2=None, op0=ALU.is_equal), r=[LG2.b, M2.b], w=[EQ2.b])
                        S.dve(lambda e: e.tensor_tensor(out=G1[:], in0=M1[:], in1=M2[:], op=ALU.subtract), r=[M1.b, M2.b], w=[G1.b])
                        S.act(lambda e: e.activation(out=G1[:], in_=G1[:], func=AF.Sigmoid), r=[G1.b], w=[G1.b])
                        S.dve(lambda e: e.tensor_scalar(out=G2_[:], in0=G1[:], scalar1=-1.0, scalar2=1.0, op0=ALU.mult, op1=ALU.add), r=[G1.b], w=[G2_.b])
                        S.dve(lambda e: e.tensor_scalar(out=GA[:], in0=EQ1[:], scalar1=G1[:, 0:1], scalar2=None, op0=ALU.mult), r=[EQ1.b, G1.b], w=[GA.b])
                        S.dve(lambda e: e.scalar_tensor_tensor(out=GA[:], in0=EQ2[:], scalar=G2_[:, 0:1], in1=GA[:], op0=ALU.mult, op1=ALU.add),
                              r=[EQ2.b, G2_.b, GA.b], w=[GA.b])
                        for e_ in range(NE):
                            bank = PS[1 + e_ // 4]
                            S.pe(lambda e, e_=e_, bank=bank: e.matmul(bank[:, (e_ % 4) * 128:(e_ % 4) * 128 + 128], lhsT=GA[:, e_:e_ + 1].to_broadcast([128, 128]),
                                                                      rhs=ident_f[:], start=True, stop=True), r=[GA.b, ident_f.b], w=[bank.b],
                                 inc=(e_ % 4 == 3))
                        for hb_ in range(NE // 4):
                            S.act(lambda e, hb_=hb_: e.copy(out=GT[:, hb_ * 4:hb_ * 4 + 4, tt * 128:tt * 128 + 128],
                                                            in_=PS[1 + hb_][:, :].rearrange("p (a t) -> p a t", t=128)), r=[PS[1 + hb_].b], w=[GT.b])
                    S.barrier()
                    st.close()
                    st = st_outer
                Gbuf = sb(st, "D1_g", [128, 2, TB], BF16)
                hr = ring(st, "D1_h", 3, [128, 512], BF16)
                panels = []
                if not moe:
                    wg = WB[("ffg", l)][0]; wu = WB[("ffu", l)][0]
                    for f0 in range(0, DFF, 256):
                        panels.append((wg, f0, min(256, DFF - f0), ("g", f0, None)))
                        panels.append((wu, f0, min(256, DFF - f0), ("u", f0, None)))
                else:
                    for e_ in range(NE):
                        wg = WB[("ffg", l)][e_]; wu = WB[("ffu", l)][e_]
                        for f0 in range(0, DFFE, 256):
                            panels.append((wg, f0, min(256, DFFE - f0), ("g", e_ * DFFE + f0, e_)))
                            panels.append((wu, f0, min(256, DFFE - f0), ("u", e_ * DFFE + f0, e_)))

                def epi_ff(tag, c0, ot, m, tb, bank):
                    kind, hrow0, e_ = tag
                    cs = slice(tb * 512, tb * 512 + 512)
                    if kind == "g":
                        S.act(lambda e: e.activation(out=Gbuf[0:m, ot, cs], in_=bank[0:m, :], func=AF.Silu), r=[bank.b], w=[Gbuf.b])
                    else:
                        h = hr()
                        S.dve(lambda e: e.tensor_tensor(out=h[0:m, :], in0=bank[0:m, :], in1=Gbuf[0:m, ot, cs], op=ALU.mult), r=[bank.b, Gbuf.b], w=[h.b])
                        if e_ is not None:
                            S.pool(lambda e: e.tensor_tensor(out=h[0:m, :], in0=h[0:m, :], in1=GT[0:m, e_, cs], op=ALU.mult), r=[h.b, GT.b], w=[h.b])
                        r0 = hrow0 + ot * 128
                        S.dma("sp", HH[r0:r0 + m, t0 + tb * 512:t0 + tb * 512 + 512], h[0:m, :], r=[h.b])
                gemm(st, "D1", panels, NFT, XT, TB, epi_ff, PS, 2)
                S.barrier()
            kff = (DFF if not moe else NE * DFFE)
            nkf = kff // 128
            wd = WB[("ffd", l)][0]
            zero_stats()
            for sbk in range(TB // 512):
                with contextlib.ExitStack() as st:
                    t1 = t0 + sbk * 512
                    HT = load_rhs(st, "D2_ht", HH[0:kff, :], nkf, t1, 512)
                    epi2 = resid_epi(st, "D2", X1F, H2, t1, cs_off=sbk * 512)
                    gemm(st, "D2", [(wd, c0, 512, None) for c0 in range(0, D, 512)], nkf, HT, 512, epi2, PS, 2, NW=512, KP=4, WNB=4, PF=2)
                    S.barrier()
            with contextlib.ExitStack() as st:
                ln_stats(st, TB)
                XT = sb(st, "E_xt", [128, NFT, TB], BF16)
                with contextlib.ExitStack() as st2:
                    ln_apply(st2, "E1", H2, t0, TB, "ln2_g", "ln2_b", X2F, XT)
                    S.barrier()
                nkp = PLE // 128
                PTt = load_rhs(st, "E_pt", I["pT"][l * PLE:(l + 1) * PLE, :], nkp, t0, TB, cast=True)
                WPP = sb(st, "E_wpp", [128, nkp, D], BF16)
                S.dma("pool", WPP[:], I["w_pproj"][l * PLE:(l + 1) * PLE, :].rearrange("(k p) n -> p k n", p=128), w=[WPP.b])
                xr = ring(st, "E_x", 3, [128, 512]); sr = ring(st, "E_s", 2, [128, 512]); orr = ring(st, "E_o", 3, [128, 512])
                obr = ring(st, "E_ob", 3, [128, 512], BF16)
                ppb = [0]
                dstF = yT if last else XF1

                def epi_e(tag, c0, ot, m, tb, bank):
                    cc = c0 + ot * 128
                    tok = slice(t0 + tb * 512, t0 + tb * 512 + 512)
                    pb = PS[4 + ppb[0] % 4]
                    ppb[0] += 1
                    x = xr(); s = sr(); o = orr()
                    S.dma("sp", x[:], X2F[cc:cc + 128, tok], w=[x.b])
                    for k in range(nkp):
                        S.pe(lambda e, k=k: e.matmul(pb[:, :], lhsT=WPP[:, k, cc:cc + 128], rhs=PTt[:, k, tb * 512:tb * 512 + 512], start=(k == 0), stop=(k == nkp - 1)),
                             r=[WPP.b, PTt.b], w=[pb.b], inc=(k == nkp - 1))
                    S.act(lambda e: e.activation(out=s[:], in_=bank[:, :], func=AF.Sigmoid), r=[bank.b], w=[s.b])
                    S.dve(lambda e: e.tensor_tensor(out=s[:], in0=s[:], in1=pb[:, :], op=ALU.mult), r=[s.b, pb.b], w=[s.b])
                    S.dve(lambda e: e.tensor_tensor(out=o[:], in0=s[:], in1=x[:], op=ALU.add), r=[s.b, x.b], w=[o.b])
                    S.dma("sp", dstF[cc:cc + 128, tok], o[:], r=[o.b])
                    if not last:
                        ob = obr()
                        S.pool(lambda e: e.tensor_copy(out=ob[:], in_=o[:]), r=[o.b], w=[ob.b])
                        S.dma("sp", XB1[cc:cc + 128, tok], ob[:], r=[ob.b])
                wsrc = WB[("w_pgate", l)][0]
                gemm(st, "E", [(wsrc, c0, 256, None) for c0 in range(0, D, 256)], NFT, XT, TB, epi_e, PS[0:4], 1)
                S.barrier()

        for l in range(DEPTH):
            layer_setup(l)
            S.barrier()
            phase_A(l)
            phase_B1(l)
            phase_B2(l)
            phase_B3(l)
            phase_B4(l)
            for t0 in range(0, NT, TB):
                phase_C(l, t0)
        S.barrier()
        nc._sched_stats = (S.n_ops, S.n_waits, dict(S.count), dict(S.dma_n), len(S.semmap))
    return nc, dbg


def shared_inputs(cfg, W):
    c = cfg
    DEPTH, D = c.DEPTH, c.D
    f = lambda a: np.ascontiguousarray(a, dtype=np.float32)
    sh = {}
    sh["w_in0"] = f(W["w_in0"])
    if DEPTH > 1:
        sh["w_in"] = f(W["w_in"]).reshape((DEPTH - 1) * D, c.CIN)
        sh["v2"] = f(W["v2"]).reshape((DEPTH - 1) * 64, c.DR)
    sh["w_out"] = f(W["w_out"]).reshape(DEPTH * D, D)
    sh["w_pgate"] = f(W["w_pgate"]).reshape(DEPTH * D, D)
    sh["w_pproj"] = f(W["w_pproj"]).reshape(DEPTH * c.PLE, D)
    sh["w_ff_gate"] = f(W["w_ff_gate"]).reshape(-1, c.DFF)
    sh["w_ff_up"] = f(W["w_ff_up"]).reshape(-1, c.DFF)
    sh["w_ff_down"] = f(W["w_ff_down"]).reshape(-1, D)
    if DEPTH // 2:
        sh["router"] = f(W["router"]).reshape(-1, c.NE)
        sh["we_gate"] = f(W["we_gate"]).reshape(-1, c.DFFE)
        sh["we_up"] = f(W["we_up"]).reshape(-1, c.DFFE)
        sh["we_down"] = f(W["we_down"]).reshape(-1, D)
    sh["sgu_ln_g"] = f(W["sgu_ln_g"]); sh["sgu_ln_b"] = f(W["sgu_ln_b"])
    sh["b_s"] = f(W["b_s"]).reshape(DEPTH, c.NHG * 128)
    sh["w_sT"] = f(np.transpose(np.asarray(W["w_s"]), (0, 3, 1, 2))).reshape(DEPTH * 128, c.NHG * 128)
    sh["w2"] = f(W["w2"]).reshape(DEPTH * 2 * 96, c.DR)
    sh["a2"] = f(W["a2"]).reshape(DEPTH * 2 * 96, c.DR)
    sh["g2"] = f(W["g2"]).reshape(DEPTH * 256, c.DR)
    Wn = {k: np.asarray(v) for k, v in W.items()}
    sh["cols"] = np.concatenate([pack_cols(c, l, Wn) for l in range(DEPTH)], axis=0)
    return sh


def core_tokens(cfg, x_prompt, x_sample, p_prompt, p_sample, core):
    nb = x_prompt.shape[0]
    if core < nb:
        return x_prompt[core], p_prompt[:, core], 1.0
    k = core - nb
    x = np.concatenate([x_sample[2 * k], x_sample[2 * k + 1]], axis=0)
    p = np.concatenate([p_sample[:, 2 * k], p_sample[:, 2 * k + 1]], axis=1)
    return x, p, 0.0


def run(cfg, inputs, debug_outs=()):
    c = cfg
    nc, dbg = build(c, debug_outs)
    W = {k: v for k, v in inputs.items() if k not in ("x_prompt", "x_sample", "p_prompt", "p_sample")}
    sh = shared_inputs(c, W)
    xp, xs = np.asarray(inputs["x_prompt"]), np.asarray(inputs["x_sample"])
    pp, psm = np.asarray(inputs["p_prompt"]), np.asarray(inputs["p_sample"])
    in_maps = []
    for core in range(c.n_cores):
        x, p, link = core_tokens(c, xp, xs, pp, psm, core)
        m = dict(sh)
        m["xT"] = np.ascontiguousarray(x.T, dtype=np.float32)
        m["pT"] = np.ascontiguousarray(np.transpose(p, (0, 2, 1)), dtype=np.float32).reshape(c.DEPTH * c.PLE, c.NT)
        m["link"] = np.full((128, 1), link, np.float32)
        in_maps.append(m)
    res = run_bass_kernel_spmd(nc, in_maps, core_ids=list(range(c.n_cores)))
    nb = xp.shape[0]
    y_prompt = np.empty(xp.shape, np.float32)
    y_sample = np.empty(xs.shape, np.float32)
    for core in range(c.n_cores):
        y = np.ascontiguousarray(res.results[core]["yT"].T)
        if core < nb:
            y_prompt[core] = y
        else:
            k = core - nb
            y_sample[2 * k] = y[:c.SEG]
            y_sample[2 * k + 1] = y[c.SEG:]
    return (y_prompt, y_sample), res


def kernel(x_prompt, x_sample, p_prompt, p_sample, w_in0, conv0, w_in, conv, sgu_ln_g, sgu_ln_b,
           w_s, b_s, w0, w2, a0, a2, g2, k_k, k_a, r_k, gn_g, gn_b, v0, v2, w_out,
           ln1_g, ln1_b, ln2_g, ln2_b, w_ff_gate, w_ff_up, w_ff_down, router, we_gate, we_up,
           we_down, w_pproj, w_pgate):
    inputs = dict(x_prompt=x_prompt, x_sample=x_sample, p_prompt=p_prompt, p_sample=p_sample, w_in0=w_in0, conv0=conv0,
                  w_in=w_in, conv=conv, sgu_ln_g=sgu_ln_g, sgu_ln_b=sgu_ln_b, w_s=w_s, b_s=b_s, w0=w0, w2=w2, a0=a0, a2=a2,
                  g2=g2, k_k=k_k, k_a=k_a, r_k=r_k, gn_g=gn_g, gn_b=gn_b, v0=v0, v2=v2, w_out=w_out, ln1_g=ln1_g, ln1_b=ln1_b,
                  ln2_g=ln2_g, ln2_b=ln2_b, w_ff_gate=w_ff_gate, w_ff_up=w_ff_up, w_ff_down=w_ff_down, router=router,
                  we_gate=we_gate, we_up=we_up, we_down=we_down, w_pproj=w_pproj, w_pgate=w_pgate)
    cfg = Cfg()
    (y_prompt, y_sample), _ = run(cfg, inputs)
    return (y_prompt, y_sample)
```

```python
import contextlib
import numpy as np
import concourse.bass as bass
import concourse.mybir as mybir
from concourse.bass_utils import run_bass_kernel_spmd

F32 = mybir.dt.float32
BF16 = mybir.dt.bfloat16
AF = mybir.ActivationFunctionType
ALU = mybir.AluOpType
AX = mybir.AxisListType

SEM_LIMIT = 28000


class Buf:
    __slots__ = ("name", "writers", "dma_w", "readers", "dma_r")

    def __init__(self, name=""):
        self.name = name
        self.writers = {}
        self.dma_w = []
        self.readers = {}
        self.dma_r = []


class Sched:
    ENGS = ("pe", "act", "dve", "pool", "sp")

    def __init__(self, nc, stack, n_sems=100, dma_ring=4):
        self.nc = nc
        self.eng = {"pe": nc.tensor, "act": nc.scalar, "dve": nc.vector, "pool": nc.gpsimd, "sp": nc.sync}
        self.count = {e: 0 for e in self.ENGS}
        self.seen = {e: {} for e in self.ENGS}
        self.free_sems = [stack.enter_context(nc.semaphore(f"s{i}")) for i in range(n_sems)]
        self.semmap = {}
        self.dma_ring = dma_ring
        self.dma_n = {q: 0 for q in ("sp", "pool", "act")}
        self.dma_slot_cnt = {}
        self.n_ops = 0
        self.n_waits = 0
        self.pending = {e: False for e in self.ENGS}

    def sem(self, key):
        s = self.semmap.get(key)
        if s is None:
            s = self.free_sems.pop()
            self.semmap[key] = s
        return s

    def _engpos(self, e, seq):
        return (("e", e, (seq - 1) // SEM_LIMIT), (seq - 1) % SEM_LIMIT + 1)

    def _need(self, e, key, val, waits):
        if self.seen[e].get(key, 0) < val:
            self.seen[e][key] = val
            waits.append((key, val))

    def _deps(self, e, r, w, dma_write=False):
        waits = []
        for b in r:
            for (pe_, seq) in b.writers.items():
                if pe_ == e:
                    if seq >= self.count[e] - 2:
                        k, v = self._engpos(pe_, seq)
                        self._need(e, k, v, waits)
                else:
                    k, v = self._engpos(pe_, seq)
                    self._need(e, k, v, waits)
            for (k, v) in b.dma_w:
                self._need(e, k, v, waits)
        for b in w:
            for (pe_, seq) in b.writers.items():
                if pe_ != e:
                    k, v = self._engpos(pe_, seq)
                    self._need(e, k, v, waits)
            if not dma_write:
                for (k, v) in b.dma_w:
                    self._need(e, k, v, waits)
            for (pe_, seq) in b.readers.items():
                if pe_ != e:
                    k, v = self._engpos(pe_, seq)
                    self._need(e, k, v, waits)
            for (k, v) in b.dma_r:
                self._need(e, k, v, waits)
        return waits

    def _emit_waits(self, e, waits):
        eng = self.eng[e]
        for (k, v) in waits:
            eng.wait_ge(self.sem(k), v)
        self.n_waits += len(waits)

    def op(self, e, fn, r=(), w=(), inc=True):
        waits = self._deps(e, r, w)
        self._emit_waits(e, waits)
        ins = fn(self.eng[e])
        self.pending[e] = not inc
        if inc:
            self.count[e] += 1
            seq = self.count[e]
            k, v = self._engpos(e, seq)
            ins.then_inc(self.sem(k), 1)
        else:
            seq = self.count[e] + 1
        for b in w:
            b.writers = {e: seq}
            b.dma_w = []
            b.readers = {}
            b.dma_r = []
        for b in r:
            b.readers[e] = seq
        self.n_ops += 1

    def pe(self, fn, r=(), w=(), inc=True):
        self.op("pe", fn, r, w, inc)

    def act(self, fn, r=(), w=()):
        self.op("act", fn, r, w)

    def dve(self, fn, r=(), w=()):
        self.op("dve", fn, r, w)

    def pool(self, fn, r=(), w=()):
        self.op("pool", fn, r, w)

    def dma(self, q, out, in_, r=(), w=()):
        waits = self._deps(q, r, w, dma_write=True)
        n = self.dma_n[q]
        self.dma_n[q] += 1
        slot = n % self.dma_ring
        cnt = self.dma_slot_cnt.get((q, slot), 0)
        if cnt > 0:
            pk = ("d", q, slot, (cnt - 1) * 16 // SEM_LIMIT)
            pv = ((cnt - 1) * 16) % SEM_LIMIT + 16
            self._need(q, pk, pv, waits)
        key = ("d", q, slot, cnt * 16 // SEM_LIMIT)
        val = (cnt * 16) % SEM_LIMIT + 16
        self.dma_slot_cnt[(q, slot)] = cnt + 1
        tok = (key, val)
        self._emit_waits(q, waits)
        self.eng[q].dma_start(out=out, in_=in_).then_inc(self.sem(key), 16)
        for b in w:
            b.dma_w.append(tok)
        for b in r:
            b.dma_r.append(tok)
        self.n_ops += 1
        return tok

    def barrier(self):
        targets = []
        assert not any(self.pending.values()), self.pending
        for e in self.ENGS:
            if self.count[e] > 0:
                targets.append(self._engpos(e, self.count[e]))
        for (q, slot), cnt in self.dma_slot_cnt.items():
            if cnt > 0:
                targets.append((("d", q, slot, (cnt - 1) * 16 // SEM_LIMIT), ((cnt - 1) * 16) % SEM_LIMIT + 16))
        for e in self.ENGS:
            waits = []
            for (k, v) in targets:
                if k[0] == "e" and k[1] == e:
                    continue
                self._need(e, k, v, waits)
            self._emit_waits(e, waits)


class Tile:
    def __init__(self, t, name):
        self.t = t
        self.b = Buf(name)

    def __getitem__(self, k):
        return self.t[k]


class Cfg:
    def __init__(self, D=4096, NT=4096, SEG=2048, DEPTH=2, TB=1024, n_cores=8, PB=None):
        self.PB = PB
        self.D = D
        self.NT = NT
        self.SEG = SEG
        self.DEPTH = DEPTH
        self.TB = TB
        self.n_cores = n_cores
        self.PLE = 256
        self.DG = D // 2
        self.NHG = self.DG // 128
        self.DR = D - self.DG
        self.NPAIR = self.DR // 128
        self.LD, self.LA, self.LMV, self.LG = 96, 96, 64, 256
        self.C_RW0 = 3 * self.DR + 2 * 96 + 2 * 96 + 256
        self.C_RW = self.C_RW0 + 64
        self.CIN0 = 2 * self.DG + self.C_RW0
        self.CIN = 2 * self.DG + self.C_RW
        self.DFF = 7 * D // 2
        self.DFFE = D // 2
        self.NE = 8
        self.NFT = D // 128
        self.NCH = NT // 64
        self.ALPHA = (2 * DEPTH) ** 0.25
        self.LN_EPS = 1e-5
        self.GN_EPS = 64e-5
        self.L2_EPS = 1e-12
        self.cols = {}
        self._build_cols()

    def _build_cols(self):
        n = 0

        def add(key):
            nonlocal n
            self.cols[key] = n
            n += 1
        P = self.NPAIR
        for nm in ("cv_r", "cv_k", "cv_v"):
            for p in range(P):
                for j in range(3):
                    add((nm, p, j))
        for nm in ("cv_wdf", "cv_wdb", "cv_adf", "cv_adb", "cv_gd0", "cv_gd1", "cv_mv"):
            for j in range(3):
                add((nm, j))
        for nm in ("k_k", "k_a", "r_k", "gn_g", "gn_b", "v0"):
            for p in range(P):
                add((nm, p))
        for nm in ("w0", "a0"):
            for d in range(2):
                for p in range(P):
                    add((nm, d, p))
        for nm in ("ln1_g", "ln1_b", "ln2_g", "ln2_b"):
            for t in range(self.NFT):
                add((nm, t))
        self.NCOLS = n


def pack_cols(cfg, l, W):
    C = np.zeros((128, cfg.NCOLS), np.float32)
    DR, P = cfg.DR, cfg.NPAIR
    conv = W["conv0"] if l == 0 else W["conv"][l - 1]

    def put(key, vec):
        C[:len(vec), cfg.cols[key]] = vec
    for i, nm in enumerate(("cv_r", "cv_k", "cv_v")):
        for p in range(P):
            for j in range(3):
                put((nm, p, j), conv[j, i * DR + 128 * p: i * DR + 128 * p + 128])
    base = 3 * DR
    offs = {"cv_wdf": (0, 96), "cv_wdb": (96, 96), "cv_adf": (192, 96), "cv_adb": (288, 96),
            "cv_gd0": (384, 128), "cv_gd1": (512, 128), "cv_mv": (640, 64)}
    for nm, (o, ln) in offs.items():
        if nm == "cv_mv" and l == 0:
            continue
        for j in range(3):
            put((nm, j), conv[j, base + o: base + o + ln])
    rk = W["r_k"][l].reshape(-1)
    vecs = {"k_k": W["k_k"][l], "k_a": W["k_a"][l], "r_k": rk, "gn_g": W["gn_g"][l], "gn_b": W["gn_b"][l]}
    if l > 0:
        vecs["v0"] = W["v0"][l - 1]
    for nm, v in vecs.items():
        for p in range(P):
            put((nm, p), v[128 * p:128 * p + 128])
    for nm in ("w0", "a0"):
        for d in range(2):
            for p in range(P):
                put((nm, d, p), W[nm][l, d, 128 * p:128 * p + 128])
    for nm in ("ln1_g", "ln1_b", "ln2_g", "ln2_b"):
        for t in range(cfg.NFT):
            put((nm, t), W[nm][l, 128 * t:128 * t + 128])
    return C


def build(cfg, debug_outs=()):
    c = cfg
    nc = bass.Bass("TRN2", target_bir_lowering=False)
    D, NT, TB, DG, DR, P, NHG, NFT, PLE = c.D, c.NT, c.TB, c.DG, c.DR, c.NPAIR, c.NHG, c.NFT, c.PLE
    DEPTH, NCH, SEG = c.DEPTH, c.NCH, c.SEG
    N_DENSE = (DEPTH + 1) // 2
    N_MOE = DEPTH // 2
    NE, DFF, DFFE = c.NE, c.DFF, c.DFFE
    I = {}

    def inp(name, shape):
        I[name] = nc.dram_tensor(name, list(shape), F32, kind="ExternalInput").ap()
        return I[name]
    inp("xT", [D, NT]); inp("pT", [DEPTH * PLE, NT]); inp("link", [128, 1]); inp("cols", [DEPTH * 128, c.NCOLS])
    inp("w_in0", [D, c.CIN0])
    if DEPTH > 1:
        inp("w_in", [(DEPTH - 1) * D, c.CIN])
        inp("v2", [(DEPTH - 1) * 64, DR])
    inp("w_out", [DEPTH * D, D]); inp("w_pgate", [DEPTH * D, D]); inp("w_pproj", [DEPTH * PLE, D])
    inp("w_ff_gate", [N_DENSE * D, DFF]); inp("w_ff_up", [N_DENSE * D, DFF]); inp("w_ff_down", [N_DENSE * DFF, D])
    if N_MOE:
        inp("router", [N_MOE * D, NE]); inp("we_gate", [N_MOE * NE * D, DFFE]); inp("we_up", [N_MOE * NE * D, DFFE])
        inp("we_down", [N_MOE * NE * DFFE, D])
    inp("sgu_ln_g", [DEPTH, DG]); inp("sgu_ln_b", [DEPTH, DG]); inp("b_s", [DEPTH, NHG * 128]); inp("w_sT", [DEPTH * 128, NHG * 128])
    inp("w2", [DEPTH * 2 * 96, DR]); inp("a2", [DEPTH * 2 * 96, DR]); inp("g2", [DEPTH * 256, DR])
    yT = nc.dram_tensor("yT", [D, NT], F32, kind="ExternalOutput").ap()

    dbg = {}

    def scr(name, shape, dt):
        if name in debug_outs:
            t = nc.dram_tensor(name, list(shape), dt, kind="ExternalOutput").ap()
            dbg[name] = t
            return t
        return nc.dram_tensor(name, list(shape), dt, kind="Internal").ap()

    XF1 = scr("XF1", [D, NT], F32); XB1 = scr("XB1", [D, NT], BF16)
    UG = scr("UG", [DG, NT], BF16); VG = scr("VG", [DG, NT], F32); ZR = scr("ZR", [c.C_RW, NT], F32)
    MIX = scr("MIX", [D, NT], BF16)
    SC = {}
    for d in range(2):
        for nm in ("RT", "KKT", "KH", "BH", "KEND", "NBEND"):
            SC[(nm, d)] = scr(f"{nm}{d}", [DR, NT], BF16)
        SC[("WL", d)] = scr(f"WL{d}", [DR, NCH], F32)
        SC[("Y", d)] = scr(f"Y{d}", [DR, NT], F32)
    VB = scr("VB", [DR, NT], BF16); VF = scr("VF", [DR, NT], F32); GG = scr("GG", [DR, NT], F32); BON = scr("BON", [DR, NT], F32)
    H1 = scr("H1", [D, NT], F32); X1F = scr("X1F", [D, NT], F32)
    HH = scr("HH", [max(DFF, NE * DFFE), NT], BF16)
    H2 = scr("H2", [D, NT], F32); X2F = scr("X2F", [D, NT], F32)

    WB = {}
    WCONV = {l_: [] for l_ in range(DEPTH)}

    def wconv(l_, key, src, NW=256, KPp=8, blk=None):
        K_, N_ = src.shape
        blk = blk or K_
        nblk = K_ // blk
        npan = (N_ + NW - 1) // NW
        npc = (blk // 128 + KPp - 1) // KPp
        dst = nc.dram_tensor(f"wb_{key}_{l_}", [nblk * npan * npc * 128, KPp * NW], BF16, kind="Internal").ap()
        dv = dst.rearrange("(i p) (k n) -> i p k n", p=128, n=NW)

        def getter(b_):
            def g(k0, kn, c0, ncols):
                idx = (b_ * npan + c0 // NW) * npc + k0 // KPp
                return dv[idx][:, 0:kn, 0:ncols]
            return g
        WB[(key, l_)] = [getter(b_) for b_ in range(nblk)]
        for b_ in range(nblk):
            sv = src[b_ * blk:(b_ + 1) * blk, :].rearrange("(kc p) n -> p kc n", p=128)
            for pi in range(npan):
                c0 = pi * NW
                ncols = min(NW, N_ - c0)
                for qi in range(npc):
                    k0 = qi * KPp
                    kn = min(KPp, blk // 128 - k0)
                    idx = (b_ * npan + pi) * npc + qi
                    WCONV[l_].append((sv[:, k0:k0 + kn, c0:c0 + ncols], dv[idx][:, 0:kn, 0:ncols]))
    for l_ in range(DEPTH):
        j_ = l_ // 2
        wconv(l_, "w_out", I["w_out"][l_ * D:(l_ + 1) * D, :])
        wconv(l_, "w_pgate", I["w_pgate"][l_ * D:(l_ + 1) * D, :])
        if l_ % 2 == 0:
            wconv(l_, "ffg", I["w_ff_gate"][j_ * D:(j_ + 1) * D, :])
            wconv(l_, "ffu", I["w_ff_up"][j_ * D:(j_ + 1) * D, :])
            wconv(l_, "ffd", I["w_ff_down"][j_ * DFF:(j_ + 1) * DFF, :], NW=512, KPp=4)
        else:
            wconv(l_, "ffg", I["we_gate"][j_ * NE * D:(j_ + 1) * NE * D, :], blk=D)
            wconv(l_, "ffu", I["we_up"][j_ * NE * D:(j_ + 1) * NE * D, :], blk=D)
            wconv(l_, "ffd", I["we_down"][j_ * NE * DFFE:(j_ + 1) * NE * DFFE, :], NW=512, KPp=4)
        if l_ + 1 < DEPTH:
            wconv(l_, "w_in_next", I["w_in"][l_ * D:(l_ + 1) * D, :])

    gst = contextlib.ExitStack()
    with gst:
        S = Sched(nc, gst)

        uid = [0]

        def sb(st, name, shape, dt=F32):
            uid[0] += 1
            nm = f"{name}_{uid[0]}"
            return Tile(st.enter_context(nc.sbuf_tensor(nm, list(shape), dt)), name)
        PS = [Tile(gst.enter_context(nc.psum_tensor(f"psb{i}", [128, 512], F32)), f"psb{i}") for i in range(8)]

        ident_f = sb(gst, "ident_f", [128, 128]); ident_b = sb(gst, "ident_b", [128, 128], BF16)
        ones_f = sb(gst, "ones_f", [128, 128]); onesbd = sb(gst, "onesbd", [128, 128])
        identbd_b = ident_b
        linkt = sb(gst, "linkt", [128, 1])
        M01 = sb(gst, "M01", [128, 512])
        S.dma("sp", linkt[:], I["link"], w=[linkt.b])
        S.pool(lambda e: e.memset(ident_f[:], 0.0), w=[ident_f.b])
        S.pool(lambda e: e.affine_select(out=ident_f[:], in_=ident_f[:], pattern=[[-1, 128]], compare_op=ALU.not_equal,
                                         fill=1.0, base=0, channel_multiplier=1), r=[ident_f.b], w=[ident_f.b])
        S.pool(lambda e: e.tensor_copy(out=ident_b[:], in_=ident_f[:]), r=[ident_f.b], w=[ident_b.b])
        S.pool(lambda e: e.memset(ones_f[:], 1.0), w=[ones_f.b])
        S.pool(lambda e: e.memset(onesbd[:], 0.0), w=[onesbd.b])
        S.pool(lambda e: e.memset(onesbd[0:64, 0:64], 1.0), w=[onesbd.b])
        S.pool(lambda e: e.memset(onesbd[64:128, 64:128], 1.0), w=[onesbd.b])
        S.pool(lambda e: e.memset(M01[:], 1.0), w=[M01.b])
        S.pool(lambda e: e.memset(M01[:].rearrange("p (c t) -> p c t", t=64)[:, :, 0:1], 0.0), w=[M01.b])
        Lst = sb(gst, "Lst", [128, 128]); Ust = sb(gst, "Ust", [128, 128]); Uin = sb(gst, "Uin", [128, 128])
        for (T_, op_, st_, cm_) in ((Lst, ALU.is_gt, -1, 1), (Ust, ALU.is_gt, 1, -1), (Uin, ALU.is_ge, 1, -1)):
            S.pool(lambda e, T_=T_: e.memset(T_[:], 1.0), w=[T_.b])
            S.pool(lambda e, T_=T_, op_=op_, st_=st_, cm_=cm_: e.affine_select(out=T_[:], in_=T_[:], pattern=[[st_, 128]], compare_op=op_,
                                                                               fill=0.0, base=0, channel_multiplier=cm_), r=[T_.b], w=[T_.b])
            S.pool(lambda e, T_=T_: e.memset(T_[64:128, 0:64], 0.0), w=[T_.b])
            S.pool(lambda e, T_=T_: e.memset(T_[0:64, 64:128], 0.0), w=[T_.b])
        MASK = {}
        mdefs = {0: {"X1": (Lst, -1.0), "X1t": (Ust, -1.0), "NAybT": (Uin, -1.0), "AukT": (Ust, 1.0), "AykT": (Uin, 1.0)},
                 1: {"X1": (Ust, -1.0), "X1t": (Lst, -1.0), "NAybT": (Lst, -1.0), "AukT": (Lst, 1.0), "AykT": (Lst, 1.0)}}
        negs = {}
        for T_ in (Lst, Ust, Uin):
            n_ = sb(gst, "neg_" + T_.b.name, [128, 128])
            S.pool(lambda e, T_=T_, n_=n_: e.tensor_scalar(out=n_[:], in0=T_[:], scalar1=-1.0, scalar2=None, op0=ALU.mult),
                   r=[T_.b], w=[n_.b])
            negs[T_.b.name] = n_
        for d in range(2):
            for k_, (T_, sg_) in mdefs[d].items():
                MASK[(k_, d)] = T_ if sg_ > 0 else negs[T_.b.name]

        colt = sb(gst, "colt", [128, c.NCOLS])
        negw0 = sb(gst, "negw0", [128, 2 * P]); omka = sb(gst, "omka", [128, P])
        nega0 = sb(gst, "nega0", [128, 2 * P]); negv0 = sb(gst, "negv0", [128, P])
        MEAN = sb(gst, "MEAN", [128, TB]); RSTD = sb(gst, "RSTD", [128, TB])
        S1 = sb(gst, "S1acc", [128, TB]); S2 = sb(gst, "S2acc", [128, TB])

        def col(key):
            i = c.cols[key]
            return colt[:, i:i + 1]

        def colrows(key, n):
            i = c.cols[key]
            return colt[0:n, i:i + 1]

        def gemm(st, name, panels, nk, rhs, TBw, epi, banks, nsets, NW=256, KP=8, WNB=6, PF=4):
            ntb = TBw // 512
            nwt = NW // 128
            per = nwt * ntb
            assert per * nsets <= len(banks)
            wring = [sb(st, f"{name}_w{i}", [128, KP, NW], BF16) for i in range(WNB)]
            pieces = []
            for pi, (wsrc, c0, ncols, tag) in enumerate(panels):
                for k0 in range(0, nk, KP):
                    pieces.append((pi, k0, min(KP, nk - k0)))
            loaded = 0

            def load(i):
                pi, k0, kn = pieces[i]
                wsrc, c0, ncols, tag = panels[pi]
                slot = wring[i % WNB]
                if callable(wsrc):
                    src = wsrc(k0, kn, c0, ncols)
                else:
                    src = wsrc.rearrange("(kc p) n -> p kc n", p=128)[:, k0:k0 + kn, c0:c0 + ncols]
                S.dma("pool", slot[:, 0:kn, 0:ncols], src, w=[slot.b])
            for i, (pi, k0, kn) in enumerate(pieces):
                while loaded < min(len(pieces), i + PF + 1):
                    load(loaded)
                    loaded += 1
                wsrc, c0, ncols, tag = panels[pi]
                slot = wring[i % WNB]
                bset = banks[(pi % nsets) * per:(pi % nsets) * per + per]
                nots = (ncols + 127) // 128
                last_piece = (k0 + kn >= nk)
                for kc in range(kn):
                    for ot in range(nots):
                        m = min(128, ncols - ot * 128)
                        for tb in range(ntb):
                            bank = bset[ot * ntb + tb]
                            lastmm = (kc == kn - 1 and ot == nots - 1 and tb == ntb - 1)
                            S.pe(lambda e, bank=bank, slot=slot, kc=kc, ot=ot, m=m, tb=tb, k0=k0:
                                 e.matmul(bank[0:m, :], lhsT=slot[:, kc, ot * 128:ot * 128 + m],
                                          rhs=rhs[:, k0 + kc, tb * 512:(tb + 1) * 512],
                                          start=(k0 + kc == 0), stop=(k0 + kc == nk - 1)),
                                 r=[slot.b, rhs.b], w=[bank.b], inc=lastmm)
                if last_piece:
                    for ot in range(nots):
                        m = min(128, ncols - ot * 128)
                        for tb in range(ntb):
                            epi(tag, c0, ot, m, tb, bset[ot * ntb + tb])

        def wview(name, row0, K):
            return I[name][row0:row0 + K, :]

        def load_rhs(st, tname, src2d, nk, t0, TBw, cast=False):
            T_ = sb(st, tname, [128, nk, TBw], BF16)
            v = src2d.rearrange("(kc p) t -> p kc t", p=128)
            step = 8
            for k0 in range(0, nk, step):
                kn = min(step, nk - k0)
                S.dma("pool" if cast else "sp", T_[:, k0:k0 + kn, :], v[:, k0:k0 + kn, t0:t0 + TBw], w=[T_.b])
            return T_

        def ring(st, name, n, shape, dt=F32):
            tiles = [sb(st, f"{name}{i}", shape, dt) for i in range(n)]
            state = [0]

            def nxt():
                t = tiles[state[0] % n]
                state[0] += 1
                return t
            return nxt

        def layer_setup(l):
            S.dma("sp", colt[:], I["cols"][l * 128:(l + 1) * 128, :], w=[colt.b])
            for d in range(2):
                for p in range(P):
                    S.dve(lambda e, d=d, p=p: e.tensor_scalar(out=negw0[:, d * P + p:d * P + p + 1], in0=col(("w0", d, p)),
                                                              scalar1=-1.0, scalar2=None, op0=ALU.mult), r=[colt.b], w=[negw0.b])
            for d in range(2):
                for p in range(P):
                    S.dve(lambda e, d=d, p=p: e.tensor_scalar(out=nega0[:, d * P + p:d * P + p + 1], in0=col(("a0", d, p)),
                                                              scalar1=-1.0, scalar2=None, op0=ALU.mult), r=[colt.b], w=[nega0.b])
            for p in range(P):
                S.dve(lambda e, p=p: e.tensor_scalar(out=omka[:, p:p + 1], in0=col(("k_a", p)), scalar1=-1.0, scalar2=1.0,
                                                     op0=ALU.mult, op1=ALU.add), r=[colt.b], w=[omka.b])
                S.dve(lambda e, p=p: e.tensor_scalar(out=negv0[:, p:p + 1], in0=col(("v0", p)), scalar1=-1.0, scalar2=None,
                                                     op0=ALU.mult), r=[colt.b], w=[negv0.b])

        def phase_A(l):
            cin = c.CIN0 if l == 0 else c.CIN
            wsrc = I["w_in0"] if l == 0 else WB[("w_in_next", l - 1)][0]
            for t0 in range(0, NT, TB):
                with contextlib.ExitStack() as st:
                    if l == 0:
                        XT = load_rhs(st, "A_xt", I["xT"], NFT, t0, TB, cast=True)
                    else:
                        XT = load_rhs(st, "A_xt", XB1, NFT, t0, TB)
                    stg_f = ring(st, "A_sf", 4, [128, 512], F32)
                    stg_b = ring(st, "A_sb", 4, [128, 512], BF16)
                    panels = [(wsrc, c0, min(256, cin - c0), None) for c0 in range(0, cin, 256)]

                    def epi(tag, c0, ot, m, tb, bank):
                        cc = c0 + ot * 128
                        tok = slice(t0 + tb * 512, t0 + tb * 512 + 512)
                        if cc < DG:
                            o = stg_b()
                            S.act(lambda e: e.activation(out=o[0:m, :], in_=bank[0:m, :], func=AF.Gelu_apprx_tanh), r=[bank.b], w=[o.b])
                            S.dma("sp", UG[cc:cc + m, tok], o[0:m, :], r=[o.b])
                        elif cc < 2 * DG:
                            o = stg_f()
                            S.act(lambda e: e.activation(out=o[0:m, :], in_=bank[0:m, :], func=AF.Gelu_apprx_tanh), r=[bank.b], w=[o.b])
                            S.dma("sp", VG[cc - DG:cc - DG + m, tok], o[0:m, :], r=[o.b])
                        else:
                            o = stg_f()
                            S.dve(lambda e: e.tensor_copy(out=o[0:m, :], in_=bank[0:m, :]), r=[bank.b], w=[o.b])
                            S.dma("sp", ZR[cc - 2 * DG:cc - 2 * DG + m, tok], o[0:m, :], r=[o.b])
                    gemm(st, "A", panels, NFT, XT, TB, epi, PS, 2)
                    S.barrier()

        def phase_B1(l):
            with contextlib.ExitStack() as st:
                Gb = sb(st, "B1_g", [128, DG]); Bb = sb(st, "B1_b", [128, DG]); BSb = sb(st, "B1_bs", [128, NHG * 128])
                WST = sb(st, "B1_wst", [128, NHG * 128], BF16)
                S.dma("sp", Gb[:], I["sgu_ln_g"][l:l + 1, :].partition_broadcast(128), w=[Gb.b])
                S.dma("sp", Bb[:], I["sgu_ln_b"][l:l + 1, :].partition_broadcast(128), w=[Bb.b])
                S.dma("sp", BSb[:], I["b_s"][l:l + 1, :].partition_broadcast(128), w=[BSb.b])
                S.dma("pool", WST[:], I["w_sT"][l * 128:(l + 1) * 128, :], w=[WST.b])
                nb = (NHG + 3) // 4
                VGc_r = ring(st, "B1_vg", 2, [128, NHG, 128], F32)
                UGc_r = ring(st, "B1_ug", 2, [128, NHG, 128], BF16)
                VN = sb(st, "B1_vn", [128, DG]); VNB = sb(st, "B1_vnb", [128, DG], BF16)
                STt = sb(st, "B1_st", [128, nb, 6]); MV = sb(st, "B1_mv", [128, 2]); RS = sb(st, "B1_rs", [128, 1])
                TMP = sb(st, "B1_tmp", [128, NHG * 128]); YG_r = ring(st, "B1_yg", 2, [128, NHG, 128], BF16)
                for ch in range(NT // 128):
                    tok = slice(ch * 128, ch * 128 + 128)
                    VGc = VGc_r(); UGc = UGc_r(); YG = YG_r()
                    S.dma("sp", VGc[:], VG.rearrange("(h p) t -> p h t", p=128)[:, :, tok], w=[VGc.b])
                    S.dma("sp", UGc[:], UG.rearrange("(h p) t -> p h t", p=128)[:, :, tok], w=[UGc.b])
                    for h in range(NHG):
                        bank = PS[h // 4]
                        S.pe(lambda e, h=h, bank=bank: e.matmul(bank[:, (h % 4) * 128:(h % 4) * 128 + 128], lhsT=VGc[:, h, :], rhs=ident_f[:],
                                                                start=True, stop=True), r=[VGc.b, ident_f.b], w=[bank.b],
                             inc=(h % 4 == 3 or h == NHG - 1))
                    for b in range(nb):
                        w_ = min(512, DG - b * 512)
                        S.dve(lambda e, b=b, w_=w_: e.bn_stats(out=STt[:, b, :], in_=PS[b][:, 0:w_]), r=[PS[b].b], w=[STt.b])
                    S.dve(lambda e: e.bn_aggr(out=MV[:], in_=STt[:]), r=[STt.b], w=[MV.b])
                    S.act(lambda e: e.activation(out=RS[:], in_=MV[:, 1:2], func=AF.Sqrt, bias=c.LN_EPS, scale=1.0), r=[MV.b], w=[RS.b])
                    S.dve(lambda e: e.reciprocal(out=RS[:], in_=RS[:]), r=[RS.b], w=[RS.b])
                    for b in range(nb):
                        w_ = min(512, DG - b * 512)
                        S.dve(lambda e, b=b, w_=w_: e.tensor_scalar(out=VN[:, b * 512:b * 512 + w_], in0=PS[b][:, 0:w_], scalar1=MV[:, 0:1],
                                                                    scalar2=RS[:, 0:1], op0=ALU.subtract, op1=ALU.mult),
                              r=[PS[b].b, MV.b, RS.b], w=[VN.b])
                    S.dve(lambda e: e.tensor_tensor(out=VN[:], in0=VN[:], in1=Gb[:], op=ALU.mult), r=[VN.b, Gb.b], w=[VN.b])
                    S.dve(lambda e: e.tensor_tensor(out=VNB[:], in0=VN[:], in1=Bb[:], op=ALU.add), r=[VN.b, Bb.b], w=[VNB.b])
                    for h in range(NHG):
                        bank = PS[4 + h // 4]
                        S.pe(lambda e, h=h, bank=bank: e.matmul(bank[:, (h % 4) * 128:(h % 4) * 128 + 128], lhsT=VNB[:, h * 128:(h + 1) * 128],
                                                                rhs=WST[:, h * 128:(h + 1) * 128], start=True, stop=True),
                             r=[VNB.b, WST.b], w=[bank.b], inc=(h % 4 == 3 or h == NHG - 1))
                    for b in range(nb):
                        w_ = min(512, DG - b * 512)
                        S.dve(lambda e, b=b, w_=w_: e.tensor_tensor(out=TMP[:, b * 512:b * 512 + w_], in0=PS[4 + b][:, 0:w_],
                                                                    in1=BSb[:, b * 512:b * 512 + w_], op=ALU.add),
                              r=[PS[4 + b].b, BSb.b], w=[TMP.b])
                    S.dve(lambda e: e.tensor_tensor(out=YG[:].rearrange("p h t -> p (h t)"), in0=TMP[:],
                                                     in1=UGc[:].rearrange("p h t -> p (h t)"), op=ALU.mult), r=[TMP.b, UGc.b], w=[YG.b])
                    S.dma("sp", MIX[0:DG, :].rearrange("(h p) t -> p h t", p=128)[:, :, tok], YG[:], r=[YG.b])
                S.barrier()

        TN = 512

        def phase_B2(l):
            nchb = TN // 64
            with contextlib.ExitStack() as st:
                W2 = [sb(st, f"B2_w2{d}", [96, DR], BF16) for d in range(2)]
                A2 = [sb(st, f"B2_a2{d}", [96, DR], BF16) for d in range(2)]
                G2 = sb(st, "B2_g2", [128, 2, DR], BF16)
                for d in range(2):
                    S.dma("pool", W2[d][:], I["w2"][(l * 2 + d) * 96:(l * 2 + d + 1) * 96, :], w=[W2[d].b])
                    S.dma("pool", A2[d][:], I["a2"][(l * 2 + d) * 96:(l * 2 + d + 1) * 96, :], w=[A2[d].b])
                S.dma("pool", G2[:], I["g2"][l * 256:(l + 1) * 256, :].rearrange("(k p) n -> p k n", p=128), w=[G2.b])
                if l > 0:
                    V2 = sb(st, "B2_v2", [64, DR], BF16)
                    S.dma("pool", V2[:], I["v2"][(l - 1) * 64:l * 64, :], w=[V2.b])

                def fsb(name, dt=F32):
                    return sb(st, "B2_" + name, [128, TN], dt)
                TW = [fsb("tw0", BF16), fsb("tw1", BF16)]; AD = [fsb("ad0", BF16), fsb("ad1", BF16)]
                SG = [fsb("sg0", BF16), fsb("sg1", BF16)]; MVt = fsb("mv", BF16)
                OUTS = [ring(st, f"B2_o{i}", 2, [128, TN], BF16) for i in range(6)]
                psb = [0]

                def nbank():
                    b = PS[psb[0] % 8]
                    psb[0] += 1
                    return b

                class TS:
                    pass
                SETS = []
                for si in range(2):
                    t = TS()
                    for nm in ("R_", "K_", "V_", "KK", "EW", "A_", "T1", "KD", "B_", "E_", "DM", "F_", "G_", "XA", "XB_", "XC", "XD", "TMP", "E1"):
                        setattr(t, nm, fsb(f"{nm}{si}"))
                    t.VBt = fsb(f"vb{si}", BF16)
                    t.WLt = sb(st, f"B2_wl{si}", [128, nchb])
                    t.Zr = ring(st, f"B2_z{si}", 3, [128, TN + 2], F32)
                    SETS.append(t)

                def load_z(Zring, nrows, row0, t0):
                    Z = Zring()
                    lo = t0 - 1
                    hi = t0 + TN + 1
                    c_lo, c_hi = 0, TN + 2
                    if t0 == 0:
                        S.dve(lambda e: e.memset(Z[0:nrows, 0:1], 0.0), w=[Z.b])
                        lo, c_lo = 0, 1
                    if t0 + TN == NT:
                        S.dve(lambda e: e.memset(Z[0:nrows, TN + 1:TN + 2], 0.0), w=[Z.b])
                        hi, c_hi = NT, TN + 1
                    S.dma("sp", Z[0:nrows, c_lo:c_hi], ZR[row0:row0 + nrows, lo:hi], w=[Z.b])
                    if t0 == SEG:
                        S.dve(lambda e: e.tensor_scalar(out=Z[0:nrows, 0:1], in0=Z[0:nrows, 0:1], scalar1=linkt[0:nrows, 0:1], scalar2=None,
                                                        op0=ALU.mult), r=[Z.b, linkt.b], w=[Z.b])
                    if t0 + TN == SEG:
                        S.dve(lambda e: e.tensor_scalar(out=Z[0:nrows, TN + 1:TN + 2], in0=Z[0:nrows, TN + 1:TN + 2], scalar1=linkt[0:nrows, 0:1],
                                                        scalar2=None, op0=ALU.mult), r=[Z.b, linkt.b], w=[Z.b])
                    return Z

                def conv3(eng, Z, dst, TMP, nrows, cvkey):
                    w = [colt[0:nrows, c.cols[cvkey + (j,)]:c.cols[cvkey + (j,)] + 1] for j in range(3)]
                    eng(lambda e: e.tensor_scalar(out=TMP[0:nrows, :], in0=Z[0:nrows, 0:TN], scalar1=w[0], scalar2=None, op0=ALU.mult),
                        r=[Z.b, colt.b], w=[TMP.b])
                    S.dve(lambda e: e.scalar_tensor_tensor(out=TMP[0:nrows, :], in0=Z[0:nrows, 1:TN + 1], scalar=w[1], in1=TMP[0:nrows, :],
                                                           op0=ALU.mult, op1=ALU.add), r=[Z.b, colt.b, TMP.b], w=[TMP.b])
                    S.dve(lambda e: e.scalar_tensor_tensor(out=dst[0:nrows, :], in0=Z[0:nrows, 2:TN + 2], scalar=w[2], in1=TMP[0:nrows, :],
                                                           op0=ALU.mult, op1=ALU.add), r=[Z.b, colt.b, TMP.b], w=[dst.b])

                def pair_gen(p, t0, T):
                    tok = slice(t0, t0 + TN)
                    rows = slice(128 * p, 128 * p + 128)
                    R_, K_, V_, KK, EW, A_, T1, KD, B_, E_, DM, F_, G_, XA, XB_, XC, XD, TMP, E1 = (
                        T.R_, T.K_, T.V_, T.KK, T.EW, T.A_, T.T1, T.KD, T.B_, T.E_, T.DM, T.F_, T.G_, T.XA, T.XB_, T.XC, T.XD, T.TMP, T.E1)
                    Zr_ = load_z(T.Zr, 128, 128 * p, t0)
                    Zk_ = load_z(T.Zr, 128, DR + 128 * p, t0)
                    Zv_ = load_z(T.Zr, 128, 2 * DR + 128 * p, t0)
                    if l > 0:
                        S.dma("sp", XD[:], VF[rows, tok], w=[XD.b])
                    yield
                    conv3(S.dve, Zr_, R_, TMP, 128, ("cv_r", p))
                    conv3(S.dve, Zk_, K_, TMP, 128, ("cv_k", p))
                    yield
                    conv3(S.dve, Zv_, V_, TMP, 128, ("cv_v", p))
                    if l == 0:
                        S.dma("sp", VF[rows, tok], V_[:], r=[V_.b])
                    else:
                        bk = nbank()
                        S.pe(lambda e: e.matmul(bk[:, :], lhsT=V2[:, rows], rhs=MVt[0:64, :], start=True, stop=True), r=[V2.b, MVt.b], w=[bk.b])
                        S.act(lambda e: e.activation(out=XC[:], in_=bk[:, :], func=AF.Exp, bias=negv0[:, p:p + 1], scale=-1.0), r=[bk.b, negv0.b], w=[XC.b])
                        S.act(lambda e: e.activation(out=XC[:], in_=XC[:], func=AF.Ln, bias=1.0, scale=1.0), r=[XC.b], w=[XC.b])
                        S.act(lambda e: e.activation(out=XC[:], in_=XC[:], func=AF.Exp, scale=-1.0), r=[XC.b], w=[XC.b])
                        S.dve(lambda e: e.tensor_tensor(out=XD[:], in0=XD[:], in1=V_[:], op=ALU.subtract), r=[XD.b, V_.b], w=[XD.b])
                        S.dve(lambda e: e.tensor_tensor(out=XD[:], in0=XD[:], in1=XC[:], op=ALU.mult), r=[XD.b, XC.b], w=[XD.b])
                        S.dve(lambda e: e.tensor_tensor(out=V_[:], in0=V_[:], in1=XD[:], op=ALU.add), r=[V_.b, XD.b], w=[V_.b])
                    S.act(lambda e: e.copy(out=T.VBt[:], in_=V_[:]), r=[V_.b], w=[T.VBt.b])
                    S.dma("sp", VB[rows, tok], T.VBt[:], r=[T.VBt.b])
                    yield
                    bk = nbank()
                    for k2 in range(2):
                        S.pe(lambda e, k2=k2: e.matmul(bk[:, :], lhsT=G2[:, k2, rows], rhs=SG[k2][:], start=(k2 == 0), stop=(k2 == 1)),
                             r=[G2.b, SG[k2].b], w=[bk.b], inc=(k2 == 1))
                    S.dve(lambda e: e.tensor_copy(out=F_[:], in_=bk[:, :]), r=[bk.b], w=[F_.b])
                    S.dma("sp", GG[rows, tok], F_[:], r=[F_.b])
                    S.dve(lambda e: e.tensor_scalar(out=KK[:], in0=K_[:], scalar1=col(("k_k", p)), scalar2=None, op0=ALU.mult),
                          r=[K_.b, colt.b], w=[KK.b])
                    S.dve(lambda e: e.tensor_tensor(out=XA[:], in0=KK[:], in1=KK[:], op=ALU.mult), r=[KK.b], w=[XA.b])
                    bk = nbank()
                    S.pe(lambda e: e.matmul(bk[:, :], lhsT=onesbd[:], rhs=XA[:], start=True, stop=True), r=[onesbd.b, XA.b], w=[bk.b])
                    S.act(lambda e: e.activation(out=XB_[:], in_=bk[:, :], func=AF.Ln, bias=c.L2_EPS, scale=1.0), r=[bk.b], w=[XB_.b])
                    S.act(lambda e: e.activation(out=XB_[:], in_=XB_[:], func=AF.Exp, scale=-0.5), r=[XB_.b], w=[XB_.b])
                    S.dve(lambda e: e.tensor_tensor(out=KK[:], in0=KK[:], in1=XB_[:], op=ALU.mult), r=[KK.b, XB_.b], w=[KK.b])
                    yield
                    for d in range(2):
                        bk = nbank()
                        S.pe(lambda e, d=d: e.matmul(bk[:, :], lhsT=W2[d][:, rows], rhs=TW[d][0:96, :], start=True, stop=True),
                             r=[W2[d].b, TW[d].b], w=[bk.b])
                        S.act(lambda e, d=d: e.activation(out=E1[:], in_=bk[:, :], func=AF.Exp, bias=negw0[:, d * P + p:d * P + p + 1], scale=-1.0),
                              r=[bk.b, negw0.b], w=[E1.b])
                        S.act(lambda e: e.activation(out=E1[:], in_=E1[:], func=AF.Ln, bias=1.0, scale=1.0), r=[E1.b], w=[E1.b])
                        S.act(lambda e: e.activation(out=EW[:], in_=E1[:], func=AF.Exp, bias=-0.5, scale=-1.0), r=[E1.b], w=[EW.b])
                        bk = nbank()
                        S.pe(lambda e, d=d: e.matmul(bk[:, :], lhsT=A2[d][:, rows], rhs=AD[d][0:96, :], start=True, stop=True),
                             r=[A2[d].b, AD[d].b], w=[bk.b])
                        S.act(lambda e, d=d: e.activation(out=A_[:], in_=bk[:, :], func=AF.Exp, bias=nega0[:, d * P + p:d * P + p + 1], scale=-1.0),
                              r=[bk.b, nega0.b], w=[A_.b])
                        S.act(lambda e: e.activation(out=A_[:], in_=A_[:], func=AF.Ln, bias=1.0, scale=1.0), r=[A_.b], w=[A_.b])
                        S.act(lambda e: e.activation(out=A_[:], in_=A_[:], func=AF.Exp, scale=-1.0), r=[A_.b], w=[A_.b])
                        yield
                        S.dve(lambda e: e.tensor_scalar(out=T1[:], in0=A_[:], scalar1=col(("k_a", p)), scalar2=omka[:, p:p + 1],
                                                        op0=ALU.mult, op1=ALU.add), r=[A_.b, colt.b, omka.b], w=[T1.b])
                        S.dve(lambda e: e.tensor_tensor(out=KD[:], in0=K_[:], in1=T1[:], op=ALU.mult), r=[K_.b, T1.b], w=[KD.b])
                        S.dve(lambda e: e.tensor_tensor(out=B_[:], in0=KK[:], in1=A_[:], op=ALU.mult), r=[KK.b, A_.b], w=[B_.b])
                        if d == 0:
                            S.dve(lambda e: e.scalar_tensor_tensor(out=T1[:], in0=R_[:], scalar=col(("r_k", p)), in1=KD[:],
                                                                   op0=ALU.mult, op1=ALU.mult), r=[R_.b, colt.b, KD.b], w=[T1.b])
                            bk = nbank()
                            S.pe(lambda e: e.matmul(bk[:, :], lhsT=onesbd[:], rhs=T1[:], start=True, stop=True), r=[onesbd.b, T1.b], w=[bk.b])
                            S.dve(lambda e: e.tensor_tensor(out=G_[:], in0=bk[:, :], in1=V_[:], op=ALU.mult), r=[bk.b, V_.b], w=[G_.b])
                            S.dma("sp", BON[rows, tok], G_[:], r=[G_.b])
                        S.dve(lambda e: e.tensor_tensor_scan(out=E_[:], data0=M01[:, 0:TN], data1=EW[:], initial=0.0, op0=ALU.mult, op1=ALU.add),
                              r=[M01.b, EW.b], w=[E_.b])
                        E3 = E_[:].rearrange("p (c t) -> p c t", t=64)
                        S.dve(lambda e: e.tensor_tensor(out=DM[:].rearrange("p (c t) -> p c t", t=64), in0=E3,
                                                        in1=E3[:, :, 63:64].to_broadcast([128, nchb, 64]), op=ALU.subtract), r=[E_.b], w=[DM.b])
                        S.dve(lambda e: e.tensor_tensor(out=F_[:], in0=EW[:], in1=E_[:], op=ALU.subtract), r=[EW.b, E_.b], w=[F_.b])
                        yield
                        S.act(lambda e: e.activation(out=T.WLt[:].rearrange("p (c o) -> p c o", o=1), in_=E3[:, :, 63:64], func=AF.Exp, scale=-1.0),
                              r=[E_.b], w=[T.WLt.b])
                        S.dma("sp", SC[("WL", d)][rows, t0 // 64:t0 // 64 + nchb], T.WLt[:], r=[T.WLt.b])
                        S.act(lambda e: e.activation(out=XB_[:], in_=F_[:], func=AF.Exp), r=[F_.b], w=[XB_.b])
                        S.act(lambda e: e.activation(out=XD[:], in_=DM[:], func=AF.Exp), r=[DM.b], w=[XD.b])
                        if d == 0:
                            S.act(lambda e: e.activation(out=XA[:], in_=E_[:], func=AF.Exp, scale=-1.0), r=[E_.b], w=[XA.b])
                            S.act(lambda e: e.activation(out=XC[:], in_=E_[:], func=AF.Exp), r=[E_.b], w=[XC.b])
                            prods = [("RT", R_, XA, 1), ("KKT", KK, XB_, 1), ("KH", KD, XC, 1), ("BH", B_, XC, 1),
                                     ("KEND", KD, XD, 1), ("NBEND", B_, XD, -1)]
                        else:
                            S.dve(lambda e: e.tensor_tensor(out=G_[:], in0=EW[:], in1=DM[:], op=ALU.subtract), r=[EW.b, DM.b], w=[G_.b])
                            S.act(lambda e: e.activation(out=XC[:], in_=G_[:], func=AF.Exp), r=[G_.b], w=[XC.b])
                            prods = [("RT", R_, XD, 1), ("KKT", KK, XD, 1), ("KH", KD, XC, 1), ("BH", B_, XC, 1),
                                     ("KEND", KD, XB_, 1), ("NBEND", B_, XB_, -1)]
                        yield
                        for i_, (nm, a_, x_, sgn) in enumerate(prods):
                            o = OUTS[i_]()
                            eng = S.dve
                            if sgn > 0:
                                eng(lambda e, o=o, a_=a_, x_=x_: e.tensor_tensor(out=o[:], in0=a_[:], in1=x_[:], op=ALU.mult),
                                    r=[a_.b, x_.b], w=[o.b])
                            else:
                                eng(lambda e, o=o, a_=a_, x_=x_: e.scalar_tensor_tensor(out=o[:], in0=a_[:], scalar=-1.0, in1=x_[:],
                                                                                        op0=ALU.mult, op1=ALU.mult), r=[a_.b, x_.b], w=[o.b])
                            S.dma("sp", SC[(nm, d)][rows, tok], o[:], r=[o.b])
                        yield

                E1g = SETS[0].E1
                TMPg = SETS[0].TMP
                Zg = SETS[0].Zr
                for t0 in range(0, NT, TN):
                    base = 3 * DR
                    for d, nm in enumerate(("cv_wdf", "cv_wdb")):
                        Z = load_z(Zg, 96, base + 96 * d, t0)
                        conv3(S.dve, Z, E1g, TMPg, 96, (nm,))
                        S.act(lambda e, d=d: e.activation(out=TW[d][0:96, :], in_=E1g[0:96, :], func=AF.Tanh), r=[E1g.b], w=[TW[d].b])
                    for k2, nm in enumerate(("cv_gd0", "cv_gd1")):
                        Z = load_z(Zg, 128, base + 384 + 128 * k2, t0)
                        conv3(S.dve, Z, E1g, TMPg, 128, (nm,))
                        S.act(lambda e, k2=k2: e.activation(out=SG[k2][:], in_=E1g[:], func=AF.Sigmoid), r=[E1g.b], w=[SG[k2].b])
                    for d, nm in enumerate(("cv_adf", "cv_adb")):
                        Z = load_z(Zg, 96, base + 192 + 96 * d, t0)
                        conv3(S.dve, Z, E1g, TMPg, 96, (nm,))
                        S.dve(lambda e, d=d: e.tensor_copy(out=AD[d][0:96, :], in_=E1g[0:96, :]), r=[E1g.b], w=[AD[d].b])
                    if l > 0:
                        Z = load_z(Zg, 64, base + 640, t0)
                        conv3(S.dve, Z, E1g, TMPg, 64, ("cv_mv",))
                        S.dve(lambda e: e.tensor_copy(out=MVt[0:64, :], in_=E1g[0:64, :]), r=[E1g.b], w=[MVt.b])
                    for p0 in range(0, P, 2):
                        interleave([pair_gen(p, t0, SETS[p - p0]) for p in range(p0, min(P, p0 + 2))])
                S.barrier()

        PB = c.PB if getattr(c, "PB", None) else min(4, P)
        GC = 2
        OPN = ("KKT", "RT", "BH", "KH", "KEND", "NBEND", "V")

        def interleave(gens):
            gens = list(gens)
            while gens:
                for g_ in list(gens):
                    try:
                        next(g_)
                    except StopIteration:
                        gens.remove(g_)

        def convert_weights(l):
            for (src, dst) in WCONV[l]:
                S.dma("pool", dst, src)

        def phase_B3(l):
            with contextlib.ExitStack() as st:
                MR = {}
                for d in range(2):
                    for k_ in ("X1", "X1t", "NAybT", "AukT", "AykT"):
                        src = MASK[(k_, d)]
                        key = src.b.name
                        if key not in MR:
                            t_ = sb(st, "B3_m_" + key, [128, PB, 128])
                            for q in range(PB):
                                S.pool(lambda e, q=q, t_=t_, src=src: e.tensor_copy(out=t_[:, q, :], in_=src[:]), r=[src.b], w=[t_.b])
                            MR[key] = t_
                IDR = sb(st, "B3_idr", [128, PB, 128], BF16)
                for q in range(PB):
                    S.pool(lambda e, q=q: e.tensor_copy(out=IDR[:, q, :], in_=ident_b[:]), r=[ident_b.b], w=[IDR.b])
                WLa = sb(st, "B3_wl", [128, P, NCH])
                bank_i = [0]
                evac_i = [0]

                def nbank():
                    b = PS[bank_i[0] % 8]
                    bank_i[0] += 1
                    return b

                def stage(nq, mms):
                    bank = nbank()
                    for q in range(nq):
                        lst = mms(q)
                        for i, (l_, r_, bufs) in enumerate(lst):
                            S.pe(lambda e, l_=l_, r_=r_, q=q, i=i, n=len(lst): e.matmul(bank[:, q * 128:(q + 1) * 128], lhsT=l_, rhs=r_,
                                                                                     start=(i == 0), stop=(i == n - 1)),
                                 r=bufs, w=[bank.b], inc=(q == nq - 1 and i == len(lst) - 1))
                    return bank

                def evac_copy(bank, dst, nq):
                    evac_i[0] += 1
                    o = dst[:, 0:nq, :].rearrange("p q t -> p (q t)")
                    if evac_i[0] % 3 == 0:
                        S.dve(lambda e: e.tensor_copy(out=o, in_=bank[:, 0:nq * 128]), r=[bank.b], w=[dst.b])
                    else:
                        S.act(lambda e: e.copy(out=o, in_=bank[:, 0:nq * 128]), r=[bank.b], w=[dst.b])

                def evac_mask(bank, dst, nq, mask):
                    o = dst[:, 0:nq, :].rearrange("p q t -> p (q t)")
                    m_ = mask[:, 0:nq, :].rearrange("p q t -> p (q t)")
                    S.dve(lambda e: e.tensor_tensor(out=o, in0=bank[:, 0:nq * 128], in1=m_, op=ALU.mult), r=[bank.b, mask.b], w=[dst.b])

                class Res:
                    pass
                RES = []
                for ri in range(2):
                    R_ = Res()
                    R_.OPS = []
                    for i in range(2):
                        dct = {}
                        for nm in OPN:
                            t_ = sb(st, f"B3_op{ri}{i}_{nm}", [128, PB, GC, 128], BF16)
                            S.pool(lambda e, t_=t_: e.memset(t_[:], 0.0), w=[t_.b])
                            dct[nm] = t_
                        R_.OPS.append(dct)

                    def tb(name, dt=BF16, ri=ri):
                        return sb(st, f"B3_{name}{ri}", [128, PB, 128], dt)
                    R_.X = [tb("x0"), tb("x1")]; R_.Xt = [tb("xt0"), tb("xt1")]; R_.Tt = [tb("tt0"), tb("tt1")]
                    R_.KKTT = tb("kktt"); R_.KENDT = tb("kendt"); R_.NBENDT = tb("nbendt"); R_.VT = tb("vt")
                    R_.NAybT = tb("naybt"); R_.AukT = tb("aukt"); R_.AykT = tb("aykt"); R_.KTT = tb("ktt"); R_.AV = tb("av"); R_.U = tb("u")
                    R_.Hf = tb("hf", F32); R_.Hb = tb("hb")
                    R_.Ys = [tb("ys0", F32), tb("ys1", F32)]
                    RES.append(R_)

                def batch_gen(d, p0, nq, R_):
                    X, Xt, Tt = R_.X, R_.Xt, R_.Tt
                    KKTT, KENDT, NBENDT, VT = R_.KKTT, R_.KENDT, R_.NBENDT, R_.VT
                    NAybT, AukT, AykT, KTT, AV, U, Hf, Hb, Ys, OPS = R_.NAybT, R_.AukT, R_.AykT, R_.KTT, R_.AV, R_.U, R_.Hf, R_.Hb, R_.Ys, R_.OPS
                    Yd = SC[("Y", d)]
                    ngroups = NCH // GC
                    gorder = list(range(ngroups)) if d == 0 else list(range(ngroups - 1, -1, -1))

                    def load_ops(gi, slot):
                        g = gorder[gi]
                        for nm in OPN:
                            src = VB if nm == "V" else SC[(nm, d)]
                            t_ = OPS[slot][nm]
                            for q in range(nq):
                                for h in range(2):
                                    r0 = 128 * (p0 + q) + 64 * h
                                    S.dma("sp", t_[64 * h:64 * h + 64, q, :, 64 * h:64 * h + 64],
                                          src[r0:r0 + 64, g * GC * 64:(g + 1) * GC * 64].rearrange("p (c t) -> p c t", t=64), w=[t_.b])
                    load_ops(0, 0)
                    S.dve(lambda e: e.memset(Hf[:], 0.0), w=[Hf.b])
                    S.dve(lambda e: e.memset(Hb[:], 0.0), w=[Hb.b])
                    yield
                    for gi in range(ngroups):
                        if gi + 1 < ngroups:
                            load_ops(gi + 1, (gi + 1) % 2)
                        O = OPS[gi % 2]
                        g = gorder[gi]
                        corder = list(range(GC)) if d == 0 else list(range(GC - 1, -1, -1))
                        for cl in corder:
                            ch = g * GC + cl
                            if (d == 0 and ch * 64 == SEG) or (d == 1 and (ch + 1) * 64 == SEG):
                                S.dve(lambda e: e.tensor_scalar(out=Hf[:].rearrange("p q t -> p (q t)"), in0=Hf[:].rearrange("p q t -> p (q t)"),
                                                                scalar1=linkt[:, 0:1], scalar2=None, op0=ALU.mult), r=[Hf.b, linkt.b], w=[Hf.b])
                                S.act(lambda e: e.copy(out=Hb[:].rearrange("p q t -> p (q t)"), in_=Hf[:].rearrange("p q t -> p (q t)")),
                                      r=[Hf.b], w=[Hb.b])

                            def op(nm, q):
                                return O[nm][:, q, cl, :]
                            mk = lambda k_: MR[MASK[(k_, d)].b.name]
                            bk = stage(nq, lambda q: [(op("KKT", q), op("BH", q), [O["KKT"].b, O["BH"].b])])
                            evac_mask(bk, X[0], nq, mk("X1"))
                            bk = stage(nq, lambda q: [(op("BH", q), op("KKT", q), [O["KKT"].b, O["BH"].b])])
                            evac_mask(bk, Xt[0], nq, mk("X1t"))
                            yield
                            bk = stage(nq, lambda q: [(op("BH", q), op("RT", q), [O["RT"].b, O["BH"].b])])
                            evac_mask(bk, NAybT, nq, mk("NAybT"))
                            bk = stage(nq, lambda q: [(op("KH", q), op("KKT", q), [O["KKT"].b, O["KH"].b])])
                            evac_mask(bk, AukT, nq, mk("AukT"))
                            yield
                            bk = stage(nq, lambda q: [(op("KH", q), op("RT", q), [O["RT"].b, O["KH"].b])])
                            evac_mask(bk, AykT, nq, mk("AykT"))
                            for (nm, dst) in (("KKT", KKTT), ("KEND", KENDT), ("NBEND", NBENDT), ("V", VT)):
                                bk = stage(nq, lambda q, nm=nm: [(op(nm, q), ident_b[:], [O[nm].b, ident_b.b])])
                                evac_copy(bk, dst, nq)
                                yield
                            S.dve(lambda e: e.tensor_tensor(out=Tt[0][:, 0:nq, :], in0=Xt[0][:, 0:nq, :], in1=IDR[:, 0:nq, :], op=ALU.add),
                                  r=[Xt[0].b, IDR.b], w=[Tt[0].b])
                            cur = 0
                            for k in range(1, 6):
                                nx = 1 - cur
                                bk = stage(nq, lambda q: [(Xt[cur][:, q, :], X[cur][:, q, :], [Xt[cur].b, X[cur].b])])
                                evac_copy(bk, X[nx], nq)
                                if k < 5:
                                    bk = stage(nq, lambda q: [(X[cur][:, q, :], Xt[cur][:, q, :], [Xt[cur].b, X[cur].b])])
                                    evac_copy(bk, Xt[nx], nq)
                                yield
                                tcur = (k - 1) % 2
                                bk = stage(nq, lambda q: [(X[nx][:, q, :], Tt[tcur][:, q, :], [X[nx].b, Tt[tcur].b])])
                                S.dve(lambda e, bk=bk, tcur=tcur: e.tensor_tensor(out=Tt[1 - tcur][:, 0:nq, :].rearrange("p q t -> p (q t)"),
                                                                                  in0=bk[:, 0:nq * 128],
                                                                                  in1=Tt[tcur][:, 0:nq, :].rearrange("p q t -> p (q t)"), op=ALU.add),
                                      r=[bk.b, Tt[tcur].b], w=[Tt[1 - tcur].b])
                                cur = nx
                                yield
                            TT = Tt[1]
                            bk = stage(nq, lambda q: [(KKTT[:, q, :], TT[:, q, :], [KKTT.b, TT.b])])
                            evac_copy(bk, KTT, nq)
                            bk = stage(nq, lambda q: [(AukT[:, q, :], VT[:, q, :], [AukT.b, VT.b])])
                            evac_copy(bk, AV, nq)
                            yield
                            bk = stage(nq, lambda q: [(TT[:, q, :], AV[:, q, :], [TT.b, AV.b]), (KTT[:, q, :], Hb[:, q, :], [KTT.b, Hb.b])])
                            evac_copy(bk, U, nq)
                            yield
                            bk = stage(nq, lambda q: [(Hb[:, q, :], op("RT", q), [Hb.b, O["RT"].b]), (VT[:, q, :], AykT[:, q, :], [VT.b, AykT.b]),
                                                      (U[:, q, :], NAybT[:, q, :], [U.b, NAybT.b])])
                            Y_ = Ys[ch % 2]
                            evac_copy(bk, Y_, nq)
                            for h in range(2):
                                S.dma("sp", Yd.rearrange("(pp r) t -> r pp t", r=128)[64 * h:64 * h + 64, p0:p0 + nq, ch * 64:ch * 64 + 64],
                                      Y_[64 * h:64 * h + 64, 0:nq, 64 * h:64 * h + 64], r=[Y_.b])
                            bk = stage(nq, lambda q: [(KENDT[:, q, :], VT[:, q, :], [KENDT.b, VT.b]), (NBENDT[:, q, :], U[:, q, :], [NBENDT.b, U.b])])
                            for q in range(nq):
                                S.dve(lambda e, q=q, bk=bk: e.scalar_tensor_tensor(out=Hf[:, q, :], in0=Hf[:, q, :], scalar=WLa[:, p0 + q, ch:ch + 1],
                                                                                   in1=bk[:, q * 128:(q + 1) * 128], op0=ALU.mult, op1=ALU.add),
                                      r=[Hf.b, WLa.b, bk.b], w=[Hf.b])
                            S.act(lambda e: e.copy(out=Hb[:, 0:nq, :].rearrange("p q t -> p (q t)"), in_=Hf[:, 0:nq, :].rearrange("p q t -> p (q t)")),
                                  r=[Hf.b], w=[Hb.b])
                            yield

                convert_weights(l)
                for d in range(2):
                    S.dma("sp", WLa[:], SC[("WL", d)].rearrange("(pp p) c -> p pp c", p=128), w=[WLa.b])
                    batches = list(range(0, P, PB))
                    for bi in range(0, len(batches), 2):
                        gens = []
                        for k_, p0 in enumerate(batches[bi:bi + 2]):
                            gens.append(batch_gen(d, p0, min(PB, P - p0), RES[k_]))
                        interleave(gens)
                S.barrier()

        def phase_B4(l):
            with contextlib.ExitStack() as st:
                def fsb(name, n=2, dt=F32):
                    return ring(st, "B4_" + name, n, [128, TN], dt)
                YA = fsb("ya"); YB_ = fsb("yb"); SQ = fsb("sq", 1); MEANt = fsb("mean", 1); MSQ = fsb("msq", 1); VAR = fsb("var", 1)
                BONt = fsb("bon"); GGt = fsb("gg"); OUT = fsb("out", 2, BF16)
                for t0 in range(0, NT, TN):
                    tok = slice(t0, t0 + TN)
                    for p in range(P):
                        rows = slice(128 * p, 128 * p + 128)
                        ya = YA(); yb = YB_(); sq = SQ(); mean = MEANt(); msq = MSQ(); var = VAR(); bon = BONt(); gg = GGt(); out = OUT()
                        S.dma("sp", ya[:], SC[("Y", 0)][rows, tok], w=[ya.b])
                        S.dma("sp", yb[:], SC[("Y", 1)][rows, tok], w=[yb.b])
                        S.dma("sp", bon[:], BON[rows, tok], w=[bon.b])
                        S.dma("sp", gg[:], GG[rows, tok], w=[gg.b])
                        S.pool(lambda e: e.tensor_tensor(out=ya[:], in0=ya[:], in1=yb[:], op=ALU.add), r=[ya.b, yb.b], w=[ya.b])
                        S.act(lambda e: e.activation(out=sq[:], in_=ya[:], func=AF.Square), r=[ya.b], w=[sq.b])
                        S.pe(lambda e: e.matmul(PS[0][:, :], lhsT=onesbd[:], rhs=ya[:], start=True, stop=True), r=[onesbd.b, ya.b], w=[PS[0].b])
                        S.pe(lambda e: e.matmul(PS[1][:, :], lhsT=onesbd[:], rhs=sq[:], start=True, stop=True), r=[onesbd.b, sq.b], w=[PS[1].b])
                        S.act(lambda e: e.activation(out=mean[:], in_=PS[0][:, :], func=AF.Copy, scale=1.0 / 64.0), r=[PS[0].b], w=[mean.b])
                        S.dve(lambda e: e.tensor_tensor(out=msq[:], in0=mean[:], in1=mean[:], op=ALU.mult), r=[mean.b], w=[msq.b])
                        S.dve(lambda e: e.scalar_tensor_tensor(out=var[:], in0=PS[1][:, :], scalar=1.0 / 64.0, in1=msq[:], op0=ALU.mult, op1=ALU.subtract),
                              r=[PS[1].b, msq.b], w=[var.b])
                        S.act(lambda e: e.activation(out=var[:], in_=var[:], func=AF.Sqrt, bias=c.GN_EPS, scale=1.0), r=[var.b], w=[var.b])
                        S.dve(lambda e: e.reciprocal(out=var[:], in_=var[:]), r=[var.b], w=[var.b])
                        S.dve(lambda e: e.tensor_tensor(out=ya[:], in0=ya[:], in1=mean[:], op=ALU.subtract), r=[ya.b, mean.b], w=[ya.b])
                        S.dve(lambda e: e.tensor_tensor(out=ya[:], in0=ya[:], in1=var[:], op=ALU.mult), r=[ya.b, var.b], w=[ya.b])
                        S.dve(lambda e: e.tensor_scalar(out=ya[:], in0=ya[:], scalar1=col(("gn_g", p)), scalar2=col(("gn_b", p)), op0=ALU.mult, op1=ALU.add),
                              r=[ya.b, colt.b], w=[ya.b])
                        S.dve(lambda e: e.tensor_tensor(out=ya[:], in0=ya[:], in1=bon[:], op=ALU.add), r=[ya.b, bon.b], w=[ya.b])
                        S.dve(lambda e: e.tensor_tensor(out=out[:], in0=ya[:], in1=gg[:], op=ALU.mult), r=[ya.b, gg.b], w=[out.b])
                        S.dma("sp", MIX[DG + 128 * p:DG + 128 * p + 128, tok], out[:], r=[out.b])
                S.barrier()

        def ln_stats(st, TBw):
            msq = sb(st, "ln_msq", [128, TBw])
            for tb in range(TBw // 512):
                cs = slice(tb * 512, tb * 512 + 512)
                S.pe(lambda e: e.matmul(PS[0][:, :], lhsT=ones_f[:], rhs=S1[:, cs], start=True, stop=True), r=[ones_f.b, S1.b], w=[PS[0].b])
                S.pe(lambda e: e.matmul(PS[1][:, :], lhsT=ones_f[:], rhs=S2[:, cs], start=True, stop=True), r=[ones_f.b, S2.b], w=[PS[1].b])
                S.act(lambda e: e.activation(out=MEAN[:, cs], in_=PS[0][:, :], func=AF.Copy, scale=1.0 / D), r=[PS[0].b], w=[MEAN.b])
                S.dve(lambda e: e.tensor_tensor(out=msq[:, cs], in0=MEAN[:, cs], in1=MEAN[:, cs], op=ALU.mult), r=[MEAN.b], w=[msq.b])
                S.dve(lambda e: e.scalar_tensor_tensor(out=RSTD[:, cs], in0=PS[1][:, :], scalar=1.0 / D, in1=msq[:, cs], op0=ALU.mult, op1=ALU.subtract),
                      r=[PS[1].b, msq.b], w=[RSTD.b])
                S.act(lambda e: e.activation(out=RSTD[:, cs], in_=RSTD[:, cs], func=AF.Sqrt, bias=c.LN_EPS, scale=1.0), r=[RSTD.b], w=[RSTD.b])
                S.dve(lambda e: e.reciprocal(out=RSTD[:, cs], in_=RSTD[:, cs]), r=[RSTD.b], w=[RSTD.b])

        def resid_epi(st, name, xsrc, hdst, t0, cs_off=0):
            xr = ring(st, name + "_x", 2, [128, 512]); hr = ring(st, name + "_h", 2, [128, 512]); sqr = ring(st, name + "_sq", 1, [128, 512])

            def epi(tag, c0, ot, m, tb, bank):
                cc = c0 + ot * 128
                tok = slice(t0 + tb * 512, t0 + tb * 512 + 512)
                cs = slice(cs_off + tb * 512, cs_off + tb * 512 + 512)
                x = xr(); h = hr(); sq = sqr()
                S.dma("sp", x[:], xsrc[cc:cc + 128, tok], w=[x.b])
                S.dve(lambda e: e.scalar_tensor_tensor(out=h[:], in0=x[:], scalar=float(c.ALPHA), in1=bank[:, :], op0=ALU.mult, op1=ALU.add),
                      r=[x.b, bank.b], w=[h.b])
                S.dma("sp", hdst[cc:cc + 128, tok], h[:], r=[h.b])
                S.act(lambda e: e.activation(out=sq[:], in_=h[:], func=AF.Square), r=[h.b], w=[sq.b])
                S.pool(lambda e: e.tensor_tensor(out=S1[:, cs], in0=S1[:, cs], in1=h[:], op=ALU.add), r=[S1.b, h.b], w=[S1.b])
                S.pool(lambda e: e.tensor_tensor(out=S2[:, cs], in0=S2[:, cs], in1=sq[:], op=ALU.add), r=[S2.b, sq.b], w=[S2.b])
            return epi

        def zero_stats():
            S.pool(lambda e: e.memset(S1[:], 0.0), w=[S1.b])
            S.pool(lambda e: e.memset(S2[:], 0.0), w=[S2.b])

        def ln_apply(st, name, hsrc, t0, TBw, gkey, bkey, fdst, XT):
            hr = ring(st, name + "_h", 2, [128, TBw]); xr = ring(st, name + "_xo", 2, [128, TBw])
            for ft in range(NFT):
                rows = slice(128 * ft, 128 * ft + 128)
                h = hr(); x = xr()
                S.dma("sp", h[:], hsrc[rows, t0:t0 + TBw], w=[h.b])
                S.dve(lambda e: e.tensor_tensor(out=h[:], in0=h[:], in1=MEAN[:, 0:TBw], op=ALU.subtract), r=[h.b, MEAN.b], w=[h.b])
                S.dve(lambda e: e.tensor_tensor(out=h[:], in0=h[:], in1=RSTD[:, 0:TBw], op=ALU.mult), r=[h.b, RSTD.b], w=[h.b])
                S.act(lambda e: e.activation(out=x[:], in_=h[:], func=AF.Identity, bias=col((bkey, ft)), scale=col((gkey, ft))),
                      r=[h.b, colt.b], w=[x.b])
                S.dma("sp", fdst[rows, t0:t0 + TBw], x[:], r=[x.b])
                S.act(lambda e: e.activation(out=XT[:, ft, :], in_=h[:], func=AF.Identity, bias=col((bkey, ft)), scale=col((gkey, ft))),
                      r=[h.b, colt.b], w=[XT.b])

        def phase_C(l, t0):
            xsrc = I["xT"] if l == 0 else XF1
            last = (l == DEPTH - 1)
            j = l // 2
            moe = (l % 2 == 1)
            with contextlib.ExitStack() as st:
                XT = load_rhs(st, "C1_xt", MIX, NFT, t0, TB)
                zero_stats()
                epi = resid_epi(st, "C1", xsrc, H1, t0)
                wsrc = WB[("w_out", l)][0]
                gemm(st, "C1", [(wsrc, c0, 256, None) for c0 in range(0, D, 256)], NFT, XT, TB, epi, PS, 2)
                ln_stats(st, TB)
                S.barrier()
            with contextlib.ExitStack() as st:
                XT = sb(st, "D1_xt", [128, NFT, TB], BF16)
                with contextlib.ExitStack() as st2:
                    ln_apply(st2, "C2", H1, t0, TB, "ln1_g", "ln1_b", X1F, XT)
                    S.barrier()
                GT = None
                if moe:
                    GT = sb(st, "D1_gt", [128, NE, TB])
                    st_outer = st
                    st = st_outer.enter_context(contextlib.ExitStack())
                    RT_ = sb(st, "D1_rt", [128, NFT, NE]); XF_r = ring(st, "D1_xf", 1, [128, NFT, 128])
                    LG = sb(st, "D1_lg", [128, NE]); M1 = sb(st, "D1_m1", [128, 1]); M2 = sb(st, "D1_m2", [128, 1])
                    EQ1 = sb(st, "D1_eq1", [128, NE]); EQ2 = sb(st, "D1_eq2", [128, NE]); LG2 = sb(st, "D1_lg2", [128, NE])
                    G1 = sb(st, "D1_g1", [128, 1]); G2_ = sb(st, "D1_g2", [128, 1]); GA = sb(st, "D1_ga", [128, NE])
                    S.dma("sp", RT_[:], I["router"][j * D:(j + 1) * D, :].rearrange("(k p) e -> p k e", p=128), w=[RT_.b])
                    for tt in range(TB // 128):
                        xf = XF_r()
                        S.dma("sp", xf[:], X1F.rearrange("(k p) t -> p k t", p=128)[:, :, t0 + tt * 128:t0 + tt * 128 + 128], w=[xf.b])
                        for k in range(NFT):
                            S.pe(lambda e, k=k: e.matmul(PS[0][:, 0:NE], lhsT=xf[:, k, :], rhs=RT_[:, k, :], start=(k == 0), stop=(k == NFT - 1)),
                                 r=[xf.b, RT_.b], w=[PS[0].b], inc=(k == NFT - 1))
                        S.dve(lambda e: e.tensor_copy(out=LG[:], in_=PS[0][:, 0:NE]), r=[PS[0].b], w=[LG.b])
                        S.dve(lambda e: e.tensor_reduce(out=M1[:], in_=LG[:], axis=AX.X, op=ALU.max), r=[LG.b], w=[M1.b])
                        S.dve(lambda e: e.tensor_scalar(out=EQ1[:], in0=LG[:], scalar1=M1[:, 0:1], scalar2=None, op0=ALU.is_equal), r=[LG.b, M1.b], w=[EQ1.b])
                        S.dve(lambda e: e.scalar_tensor_tensor(out=LG2[:], in0=EQ1[:], scalar=-1.0e30, in1=LG[:], op0=ALU.mult, op1=ALU.add),
                              r=[EQ1.b, LG.b], w=[LG2.b])
                        S.dve(lambda e: e.tensor_reduce(out=M2[:], in_=LG2[:], axis=AX.X, op=ALU.max), r=[LG2.b], w=[M2.b])
                        S.dve(lambda e: e.tensor_scalar(out=EQ2[:], in0=LG2[:], scalar1=M2[:, 0:1], scalar2=None, op0=ALU.is_equal), r=[LG2.b, M2.b], w=[EQ2.b])
                        S.dve(lambda e: e.tensor_tensor(out=G1[:], in0=M1[:], in1=M2[:], op=ALU.subtract), r=[M1.b, M2.b], w=[G1.b])
                        S.act(lambda e: e.activation(out=G1[:], in_=G1[:], func=AF.Sigmoid), r=[G1.b], w=[G1.b])
                        S.dve(lambda e: e.tensor_scalar(out=G2_[:], in0=G1[:], scalar1=-1.0, scalar2=1.0, op0=ALU.mult, op1=ALU.add), r=[G1.b], w=[G2_.b])
                        S.dve(lambda e: e.tensor_scalar(out=GA[:], in0=EQ1[:], scalar1=G1[:, 0:1], scalar2=None, op0=ALU.mult), r=[EQ1.b, G1.b], w=[GA.b])
                        S.dve(lambda e: e.scalar_tensor_tensor(out=GA[:], in0=EQ2[:], scalar=G2_[:, 0:1], in1=GA[:], op0=ALU.mult, op1=ALU.add),
                              r=[EQ2.b, G2_.b, GA.b], w=[GA.b])
                        for e_ in range(NE):
                            bank = PS[1 + e_ // 4]
                            S.pe(lambda e, e_=e_, bank=bank: e.matmul(bank[:, (e_ % 4) * 128:(e_ % 4) * 128 + 128], lhsT=GA[:, e_:e_ + 1].to_broadcast([128, 128]),
                                                                      rhs=ident_f[:], start=True, stop=True), r=[GA.b, ident_f.b], w=[bank.b],
                                 inc=(e_ % 4 == 3))
                        for hb_ in range(NE // 4):
                            S.act(lambda e, hb_=hb_: e.copy(out=GT[:, hb_ * 4:hb_ * 4 + 4, tt * 128:tt * 128 + 128],
                                                            in_=PS[1 + hb_][:, :].rearrange("p (a t) -> p a t", t=128)), r=[PS[1 + hb_].b], w=[GT.b])
                    S.barrier()
                    st.close()
                    st = st_outer
                Gbuf = sb(st, "D1_g", [128, 2, TB], BF16)
                hr = ring(st, "D1_h", 3, [128, 512], BF16)
                panels = []
                if not moe:
                    wg = WB[("ffg", l)][0]; wu = WB[("ffu", l)][0]
                    for f0 in range(0, DFF, 256):
                        panels.append((wg, f0, min(256, DFF - f0), ("g", f0, None)))
                        panels.append((wu, f0, min(256, DFF - f0), ("u", f0, None)))
                else:
                    for e_ in range(NE):
                        wg = WB[("ffg", l)][e_]; wu = WB[("ffu", l)][e_]
                        for f0 in range(0, DFFE, 256):
                            panels.append((wg, f0, min(256, DFFE - f0), ("g", e_ * DFFE + f0, e_)))
                            panels.append((wu, f0, min(256, DFFE - f0), ("u", e_ * DFFE + f0, e_)))

                def epi_ff(tag, c0, ot, m, tb, bank):
                    kind, hrow0, e_ = tag
                    cs = slice(tb * 512, tb * 512 + 512)
                    if kind == "g":
                        S.act(lambda e: e.activation(out=Gbuf[0:m, ot, cs], in_=bank[0:m, :], func=AF.Silu), r=[bank.b], w=[Gbuf.b])
                    else:
                        h = hr()
                        S.dve(lambda e: e.tensor_tensor(out=h[0:m, :], in0=bank[0:m, :], in1=Gbuf[0:m, ot, cs], op=ALU.mult), r=[bank.b, Gbuf.b], w=[h.b])
                        if e_ is not None:
                            S.pool(lambda e: e.tensor_tensor(out=h[0:m, :], in0=h[0:m, :], in1=GT[0:m, e_, cs], op=ALU.mult), r=[h.b, GT.b], w=[h.b])
                        r0 = hrow0 + ot * 128
                        S.dma("sp", HH[r0:r0 + m, t0 + tb * 512:t0 + tb * 512 + 512], h[0:m, :], r=[h.b])
                gemm(st, "D1", panels, NFT, XT, TB, epi_ff, PS, 2)
                S.barrier()
            kff = (DFF if not moe else NE * DFFE)
            nkf = kff // 128
            wd = WB[("ffd", l)][0]
            zero_stats()
            for sbk in range(TB // 512):
                with contextlib.ExitStack() as st:
                    t1 = t0 + sbk * 512
                    HT = load_rhs(st, "D2_ht", HH[0:kff, :], nkf, t1, 512)
                    epi2 = resid_epi(st, "D2", X1F, H2, t1, cs_off=sbk * 512)
                    gemm(st, "D2", [(wd, c0, 512, None) for c0 in range(0, D, 512)], nkf, HT, 512, epi2, PS, 2, NW=512, KP=4, WNB=7, PF=5)
                    S.barrier()
            with contextlib.ExitStack() as st:
                ln_stats(st, TB)
                XT = sb(st, "E_xt", [128, NFT, TB], BF16)
                with contextlib.ExitStack() as st2:
                    ln_apply(st2, "E1", H2, t0, TB, "ln2_g", "ln2_b", X2F, XT)
                    S.barrier()
                nkp = PLE // 128
                PTt = load_rhs(st, "E_pt", I["pT"][l * PLE:(l + 1) * PLE, :], nkp, t0, TB, cast=True)
                WPP = sb(st, "E_wpp", [128, nkp, D], BF16)
                S.dma("pool", WPP[:], I["w_pproj"][l * PLE:(l + 1) * PLE, :].rearrange("(k p) n -> p k n", p=128), w=[WPP.b])
                xr = ring(st, "E_x", 3, [128, 512]); sr = ring(st, "E_s", 2, [128, 512]); orr = ring(st, "E_o", 3, [128, 512])
                obr = ring(st, "E_ob", 3, [128, 512], BF16)
                ppb = [0]
                dstF = yT if last else XF1

                def epi_e(tag, c0, ot, m, tb, bank):
                    cc = c0 + ot * 128
                    tok = slice(t0 + tb * 512, t0 + tb * 512 + 512)
                    pb = PS[4 + ppb[0] % 4]
                    ppb[0] += 1
                    x = xr(); s = sr(); o = orr()
                    S.dma("sp", x[:], X2F[cc:cc + 128, tok], w=[x.b])
                    for k in range(nkp):
                        S.pe(lambda e, k=k: e.matmul(pb[:, :], lhsT=WPP[:, k, cc:cc + 128], rhs=PTt[:, k, tb * 512:tb * 512 + 512], start=(k == 0), stop=(k == nkp - 1)),
                             r=[WPP.b, PTt.b], w=[pb.b], inc=(k == nkp - 1))
                    S.act(lambda e: e.activation(out=s[:], in_=bank[:, :], func=AF.Sigmoid), r=[bank.b], w=[s.b])
                    S.dve(lambda e: e.tensor_tensor(out=s[:], in0=s[:], in1=pb[:, :], op=ALU.mult), r=[s.b, pb.b], w=[s.b])
                    S.dve(lambda e: e.tensor_tensor(out=o[:], in0=s[:], in1=x[:], op=ALU.add), r=[s.b, x.b], w=[o.b])
                    S.dma("sp", dstF[cc:cc + 128, tok], o[:], r=[o.b])
                    if not last:
                        ob = obr()
                        S.pool(lambda e: e.tensor_copy(out=ob[:], in_=o[:]), r=[o.b], w=[ob.b])
                        S.dma("sp", XB1[cc:cc + 128, tok], ob[:], r=[ob.b])
                wsrc = WB[("w_pgate", l)][0]
                gemm(st, "E", [(wsrc, c0, 256, None) for c0 in range(0, D, 256)], NFT, XT, TB, epi_e, PS[0:4], 1)
                S.barrier()

        for l in range(DEPTH):
            layer_setup(l)
            S.barrier()
            phase_A(l)
            phase_B1(l)
            phase_B2(l)
            phase_B3(l)
            phase_B4(l)
            for t0 in range(0, NT, TB):
                phase_C(l, t0)
        S.barrier()
        nc._sched_stats = (S.n_ops, S.n_waits, dict(S.count), dict(S.dma_n), len(S.semmap))
    return nc, dbg


def shared_inputs(cfg, W):
    c = cfg
    DEPTH, D = c.DEPTH, c.D
    f = lambda a: np.ascontiguousarray(a, dtype=np.float32)
    sh = {}
    sh["w_in0"] = f(W["w_in0"])
    if DEPTH > 1:
        sh["w_in"] = f(W["w_in"]).reshape((DEPTH - 1) * D, c.CIN)
        sh["v2"] = f(W["v2"]).reshape((DEPTH - 1) * 64, c.DR)
    sh["w_out"] = f(W["w_out"]).reshape(DEPTH * D, D)
    sh["w_pgate"] = f(W["w_pgate"]).reshape(DEPTH * D, D)
    sh["w_pproj"] = f(W["w_pproj"]).reshape(DEPTH * c.PLE, D)
    sh["w_ff_gate"] = f(W["w_ff_gate"]).reshape(-1, c.DFF)
    sh["w_ff_up"] = f(W["w_ff_up"]).reshape(-1, c.DFF)
    sh["w_ff_down"] = f(W["w_ff_down"]).reshape(-1, D)
    if DEPTH // 2:
        sh["router"] = f(W["router"]).reshape(-1, c.NE)
        sh["we_gate"] = f(W["we_gate"]).reshape(-1, c.DFFE)
        sh["we_up"] = f(W["we_up"]).reshape(-1, c.DFFE)
        sh["we_down"] = f(W["we_down"]).reshape(-1, D)
    sh["sgu_ln_g"] = f(W["sgu_ln_g"]); sh["sgu_ln_b"] = f(W["sgu_ln_b"])
    sh["b_s"] = f(W["b_s"]).reshape(DEPTH, c.NHG * 128)
    sh["w_sT"] = f(np.transpose(np.asarray(W["w_s"]), (0, 3, 1, 2))).reshape(DEPTH * 128, c.NHG * 128)
    sh["w2"] = f(W["w2"]).reshape(DEPTH * 2 * 96, c.DR)
    sh["a2"] = f(W["a2"]).reshape(DEPTH * 2 * 96, c.DR)
    sh["g2"] = f(W["g2"]).reshape(DEPTH * 256, c.DR)
    Wn = {k: np.asarray(v) for k, v in W.items()}
    sh["cols"] = np.concatenate([pack_cols(c, l, Wn) for l in range(DEPTH)], axis=0)
    return sh


def core_tokens(cfg, x_prompt, x_sample, p_prompt, p_sample, core):
    nb = x_prompt.shape[0]
    if core < nb:
        return x_prompt[core], p_prompt[:, core], 1.0
    k = core - nb
    x = np.concatenate([x_sample[2 * k], x_sample[2 * k + 1]], axis=0)
    p = np.concatenate([p_sample[:, 2 * k], p_sample[:, 2 * k + 1]], axis=1)
    return x, p, 0.0


def run(cfg, inputs, debug_outs=()):
    c = cfg
    nc, dbg = build(c, debug_outs)
    W = {k: v for k, v in inputs.items() if k not in ("x_prompt", "x_sample", "p_prompt", "p_sample")}
    sh = shared_inputs(c, W)
    xp, xs = np.asarray(inputs["x_prompt"]), np.asarray(inputs["x_sample"])
    pp, psm = np.asarray(inputs["p_prompt"]), np.asarray(inputs["p_sample"])
    in_maps = []
    for core in range(c.n_cores):
        x, p, link = core_tokens(c, xp, xs, pp, psm, core)
        m = dict(sh)
        m["xT"] = np.ascontiguousarray(x.T, dtype=np.float32)
        m["pT"] = np.ascontiguousarray(np.transpose(p, (0, 2, 1)), dtype=np.float32).reshape(c.DEPTH * c.PLE, c.NT)
        m["link"] = np.full((128, 1), link, np.float32)
        in_maps.append(m)
    res = run_bass_kernel_spmd(nc, in_maps, core_ids=list(range(c.n_cores)))
    nb = xp.shape[0]
    y_prompt = np.empty(xp.shape, np.float32)
    y_sample = np.empty(xs.shape, np.float32)
    for core in range(c.n_cores):
        y = np.ascontiguousarray(res.results[core]["yT"].T)
        if core < nb:
            y_prompt[core] = y
        else:
            k = core - nb
            y_sample[2 * k] = y[:c.SEG]
            y_sample[2 * k + 1] = y[c.SEG:]
    return (y_prompt, y_sample), res


def kernel(x_prompt, x_sample, p_prompt, p_sample, w_in0, conv0, w_in, conv, sgu_ln_g, sgu_ln_b,
           w_s, b_s, w0, w2, a0, a2, g2, k_k, k_a, r_k, gn_g, gn_b, v0, v2, w_out,
           ln1_g, ln1_b, ln2_g, ln2_b, w_ff_gate, w_ff_up, w_ff_down, router, we_gate, we_up,
           we_down, w_pproj, w_pgate):
    inputs = dict(x_prompt=x_prompt, x_sample=x_sample, p_prompt=p_prompt, p_sample=p_sample, w_in0=w_in0, conv0=conv0,
                  w_in=w_in, conv=conv, sgu_ln_g=sgu_ln_g, sgu_ln_b=sgu_ln_b, w_s=w_s, b_s=b_s, w0=w0, w2=w2, a0=a0, a2=a2,
                  g2=g2, k_k=k_k, k_a=k_a, r_k=r_k, gn_g=gn_g, gn_b=gn_b, v0=v0, v2=v2, w_out=w_out, ln1_g=ln1_g, ln1_b=ln1_b,
                  ln2_g=ln2_g, ln2_b=ln2_b, w_ff_gate=w_ff_gate, w_ff_up=w_ff_up, w_ff_down=w_ff_down, router=router,
                  we_gate=we_gate, we_up=we_up, we_down=we_down, w_pproj=w_pproj, w_pgate=w_pgate)
    cfg = Cfg()
    (y_prompt, y_sample), _ = run(cfg, inputs)
    return (y_prompt, y_sample)
```
